# Optimizing a Trainium2 kernel written in Bass

```python
import math
import jax, jax.numpy as jnp
from jax import lax
import numpy as np

D_MODEL = 1024
BATCH = 8
SEQ = 4096
DEPTH = 1

CTX_LEN = 256
GRID_W = 64
MIX_WIDTH = D_MODEL
ATTN_WIDTH = MIX_WIDTH // 2
HYENA_WIDTH = MIX_WIDTH - ATTN_WIDTH
N_HEADS = 4
V_DIM = ATTN_WIDTH // N_HEADS
QK_DIM = V_DIM // 2
QK_COLS = N_HEADS * 2 * QK_DIM
V_COLS = N_HEADS * V_DIM
HYENA_ORDER = 2
HYENA_COLS = (HYENA_ORDER + 1) * HYENA_WIDTH
IN_COLS = 2 * QK_COLS + V_COLS + HYENA_COLS
SHORT_CONV = 3
N_BANDS = 8
FEAT_DIM = 1 + 2 * N_BANDS
FILTER_HIDDEN = 64
DECAY_TARGET = 1e-2
FAST_DECAY_PCT = 0.3
SLOW_DECAY_PCT = 1.5
N_EXPERTS = 32
TOP_K = 4
D_EXPERT = D_MODEL
SWIGLU_LIMIT = 7.0
SWIGLU_ALPHA = 1.702
EXPERT_BLOCK = 128
Q_BLOCK = 128
ROPE_BASE = 10000.0
EPS = 1e-6
N_MOD = 6

kernel_name = "hybrid_diffattn_hyena_moe_dit"

F32 = jnp.float32


def rms_norm(x, g):
    x32 = x.astype(F32)
    y = x32 * lax.rsqrt(jnp.mean(x32 * x32, axis=-1, keepdims=True) + EPS)
    return (y * g.astype(F32)).astype(x.dtype)


def modulate(h, shift, scale):
    return h * (1 + scale) + shift


def rope_2d(x, row, col):
    half = QK_DIM // 2
    nf = half // 2
    inv = ROPE_BASE ** (-jnp.arange(nf, dtype=F32) / nf)

    def rot(xp, pos):
        ang = pos.astype(F32)[:, None] * inv[None]
        cos = jnp.cos(ang)[None, :, None, None, :].astype(x.dtype)
        sin = jnp.sin(ang)[None, :, None, None, :].astype(x.dtype)
        x1, x2 = xp[..., :nf], xp[..., nf:]
        return jnp.concatenate([x1 * cos - x2 * sin, x2 * cos + x1 * sin], axis=-1)

    return jnp.concatenate([rot(x[..., :half], row), rot(x[..., half:], col)], axis=-1)


def short_conv(u, w, b):
    L = u.shape[1]
    pad = SHORT_CONV // 2
    up = jnp.pad(u, ((0, 0), (pad, pad), (0, 0)))
    y = b
    for j in range(SHORT_CONV):
        y = y + up[:, j:j + L] * w[j]
    return y


def hyena_filters(L, w1, b1, f1, w2, b2, f2, w3):
    pos = jnp.arange(L, dtype=F32)
    tn = pos / L
    bands = jnp.linspace(1e-4, N_BANDS - 1, N_BANDS, dtype=F32)
    ang = (2.0 * math.pi / L) * pos[:, None] * bands[None]
    feats = jnp.concatenate([tn[:, None], jnp.sin(ang), jnp.cos(ang)], axis=-1)
    h = jnp.sin(f1 * (feats @ w1 + b1))
    h = jnp.sin(f2 * (h @ w2 + b2))
    h = (h @ w3).astype(F32)
    deltas = jnp.abs(jnp.linspace(math.log(DECAY_TARGET) / SLOW_DECAY_PCT,
                                  math.log(DECAY_TARGET) / FAST_DECAY_PCT,
                                  HYENA_WIDTH, dtype=F32))
    window = jnp.exp(-tn[:, None] * deltas[None])
    return h.reshape(L, HYENA_ORDER, 2, HYENA_WIDTH) * window[:, None, None, :]


def long_conv_bidir(u, h_fwd, h_bwd, skip):
    L, C = u.shape[1], u.shape[2]
    h_circ = jnp.concatenate([h_fwd[:1] + h_bwd[:1], h_fwd[1:],
                              jnp.zeros((1, C), F32), h_bwd[:0:-1]], axis=0)
    hf = jnp.fft.rfft(h_circ, n=2 * L, axis=0)
    u32 = u.astype(F32)
    uf = jnp.fft.rfft(u32, n=2 * L, axis=1)
    y = jnp.fft.irfft(uf * hf[None], n=2 * L, axis=1)[:, :L]
    return (y + u32 * skip.astype(F32)).astype(u.dtype)


def hyena_mixer(u, conv_w, conv_b, filters, skip):
    u = short_conv(u, conv_w, conv_b)
    v, x1, x2 = jnp.split(u, 3, axis=-1)
    z = v
    for n, gate in enumerate((x1, x2)):
        z = gate * long_conv_bidir(z, filters[:, n, 0], filters[:, n, 1], skip[n])
    return z


def diff_attend(q, k, v, lam):
    s = jnp.einsum('bqhmd,bkhmd->bhmqk', q, k).astype(F32) * (QK_DIM ** -0.5)
    p = jax.nn.softmax(s, axis=-1)
    a = p[:, :, 0] - lam * p[:, :, 1]
    return jnp.einsum('bhqk,bkhd->bqhd', a.astype(v.dtype), v)


def diff_attention_blocked(q, k_all, v_all, lam):
    B, L = q.shape[:2]
    nb = L // Q_BLOCK
    qb = q.reshape(B, nb, Q_BLOCK, N_HEADS, 2, QK_DIM).swapaxes(0, 1)
    out = lax.map(lambda qi: diff_attend(qi, k_all, v_all, lam), qb)
    return out.swapaxes(0, 1).reshape(B, L, N_HEADS, V_DIM)


def moe(h, router_w, router_b, w1, b1, w2, b2):
    T, D = h.shape
    logits = (h @ router_w + router_b).astype(F32)
    top_val, top_idx = lax.top_k(logits, TOP_K)
    gates = jax.nn.softmax(top_val, axis=-1)
    n_assign = T * TOP_K
    flat_e = top_idx.reshape(-1)
    flat_tok = jnp.arange(n_assign) // TOP_K
    order = jnp.argsort(flat_e)
    e_sorted = flat_e[order]
    tok_sorted = flat_tok[order]
    gate_sorted = gates.reshape(-1)[order]
    counts = jnp.bincount(flat_e, length=N_EXPERTS)
    padded = ((counts + EXPERT_BLOCK - 1) // EXPERT_BLOCK) * EXPERT_BLOCK
    start = jnp.cumsum(counts) - counts
    pend = jnp.cumsum(padded)
    pstart = pend - padded
    dest = pstart[e_sorted] + (jnp.arange(n_assign) - start[e_sorted])
    n_blocks = -(-n_assign // EXPERT_BLOCK) + N_EXPERTS
    buf = jnp.zeros((n_blocks * EXPERT_BLOCK, D), h.dtype).at[dest].set(h[tok_sorted])
    block_e = jnp.clip(jnp.searchsorted(pend, jnp.arange(n_blocks) * EXPERT_BLOCK, side='right'),
                       0, N_EXPERTS - 1)

    def expert_block(args):
        xb, e = args
        gl = xb @ w1[e] + b1[e]
        g, lin = gl[:, :D_EXPERT], gl[:, D_EXPERT:]
        g = jnp.minimum(g, SWIGLU_LIMIT)
        lin = jnp.clip(lin, -SWIGLU_LIMIT, SWIGLU_LIMIT)
        glu = g * jax.nn.sigmoid(SWIGLU_ALPHA * g)
        return ((lin + 1) * glu) @ w2[e] + b2[e]

    out_buf = lax.map(expert_block, (buf.reshape(n_blocks, EXPERT_BLOCK, D), block_e))
    y = out_buf.reshape(-1, D)[dest] * gate_sorted[:, None].astype(h.dtype)
    return jax.ops.segment_sum(y, tok_sorted, num_segments=T)


def trunk_layer(x, ctx, c, c_ctx, p, lam_init, update_ctx):
    B, S, D = x.shape
    Lc = ctx.shape[1]
    ROWS = S // GRID_W
    row = jnp.repeat(jnp.arange(ROWS), GRID_W)
    col = jnp.tile(jnp.arange(GRID_W), ROWS)

    mod_x = (jax.nn.silu(c) @ p['w_mod'] + p['b_mod']).reshape(B, N_MOD, 1, D)
    mod_c = (jax.nn.silu(c_ctx) @ p['w_mod'] + p['b_mod']).reshape(1, N_MOD, 1, D)
    sh1, sc1, g1, sh2, sc2, g2 = [mod_x[:, i] for i in range(N_MOD)]
    csh1, csc1, cg1, csh2, csc2, cg2 = [mod_c[:, i] for i in range(N_MOD)]

    w_in = p['w_in']
    lam = (jnp.exp(jnp.sum(p['lam_q1'].astype(F32) * p['lam_k1'].astype(F32)))
           - jnp.exp(jnp.sum(p['lam_q2'].astype(F32) * p['lam_k2'].astype(F32))) + lam_init)

    h = modulate(rms_norm(x, p['norm1_g']), sh1, sc1)
    hc = modulate(rms_norm(ctx, p['norm1_g']), csh1, csc1)
    proj = h @ w_in
    q = proj[..., :QK_COLS].reshape(B, S, N_HEADS, 2, QK_DIM)
    k = proj[..., QK_COLS:2 * QK_COLS].reshape(B, S, N_HEADS, 2, QK_DIM)
    v = proj[..., 2 * QK_COLS:2 * QK_COLS + V_COLS].reshape(B, S, N_HEADS, V_DIM)
    u_hy = proj[..., 2 * QK_COLS + V_COLS:]
    q = rope_2d(rms_norm(q, p['q_norm_g']), row, col)
    k = rope_2d(rms_norm(k, p['k_norm_g']), row, col)

    kv_c = hc @ w_in[:, QK_COLS:2 * QK_COLS + V_COLS]
    k_c = rms_norm(kv_c[..., :QK_COLS].reshape(B, Lc, N_HEADS, 2, QK_DIM), p['k_norm_g'])
    v_c = kv_c[..., QK_COLS:].reshape(B, Lc, N_HEADS, V_DIM)

    k_all = jnp.concatenate([k_c, k], axis=1)
    v_all = jnp.concatenate([v_c, v], axis=1)
    attn = diff_attention_blocked(q, k_all, v_all, lam)
    attn = (rms_norm(attn, p['subln_g']) * (1.0 - lam_init)).reshape(B, S, ATTN_WIDTH)

    filt_x = hyena_filters(S, p['hy_w1'], p['hy_b1'], p['hy_f1'], p['hy_w2'], p['hy_b2'], p['hy_f2'], p['hy_w3'])
    hy = hyena_mixer(u_hy, p['hy_conv_w'], p['hy_conv_b'], filt_x, p['hy_skip'])
    mix = jnp.concatenate([attn, rms_norm(hy, p['hy_out_g'])], axis=-1) @ p['w_out']
    x_new = x + g1 * mix

    h2 = modulate(rms_norm(x_new, p['norm2_g']), sh2, sc2)
    ffn = moe(h2.reshape(B * S, D), p['router_w'], p['router_b'],
              p['exp_w1'], p['exp_b1'], p['exp_w2'], p['exp_b2']).reshape(B, S, D)
    x_new = x_new + g2 * ffn

    if update_ctx:
        q_c = rms_norm((hc @ w_in[:, :QK_COLS]).reshape(B, Lc, N_HEADS, 2, QK_DIM), p['q_norm_g'])
        attn_c = diff_attend(q_c, k_c, v_c, lam)
        attn_c = (rms_norm(attn_c, p['subln_g']) * (1.0 - lam_init)).reshape(B, Lc, ATTN_WIDTH)
        filt_c = hyena_filters(Lc, p['hy_w1'], p['hy_b1'], p['hy_f1'], p['hy_w2'], p['hy_b2'], p['hy_f2'], p['hy_w3'])
        hy_c = hyena_mixer(hc @ w_in[:, 2 * QK_COLS + V_COLS:], p['hy_conv_w'], p['hy_conv_b'], filt_c, p['hy_skip'])
        mix_c = jnp.concatenate([attn_c, rms_norm(hy_c, p['hy_out_g'])], axis=-1) @ p['w_out']
        ctx = ctx + cg1 * mix_c
        hc2 = modulate(rms_norm(ctx, p['norm2_g']), csh2, csc2)
        ffn_c = moe(hc2.reshape(B * Lc, D), p['router_w'], p['router_b'],
                    p['exp_w1'], p['exp_b1'], p['exp_w2'], p['exp_b2']).reshape(B, Lc, D)
        ctx = ctx + cg2 * ffn_c
    return x_new, ctx


def setup_inputs(seed: int = 0) -> dict:
    key = jax.random.key(seed)
    ks = iter(jax.random.split(key, 40))

    def nrm(shape, scale):
        return jax.random.normal(next(ks), shape, F32) * scale

    def gain(shape):
        return 1.0 + nrm(shape, 0.01)

    L = DEPTH
    return {
        'x': nrm((BATCH, SEQ, D_MODEL), 1.0),
        'c': nrm((BATCH, D_MODEL), 1.0),
        'ctx': nrm((BATCH, CTX_LEN, D_MODEL), 1.0),
        'c_ctx': nrm((D_MODEL,), 1.0),
        'w_mod': nrm((L, D_MODEL, N_MOD * D_MODEL), 0.5 * D_MODEL ** -0.5),
        'b_mod': nrm((L, N_MOD * D_MODEL), 0.01),
        'norm1_g': gain((L, D_MODEL)),
        'norm2_g': gain((L, D_MODEL)),
        'w_in': nrm((L, D_MODEL, IN_COLS), D_MODEL ** -0.5),
        'q_norm_g': gain((L, QK_DIM)),
        'k_norm_g': gain((L, QK_DIM)),
        'lam_q1': nrm((L, QK_DIM), 0.1),
        'lam_k1': nrm((L, QK_DIM), 0.1),
        'lam_q2': nrm((L, QK_DIM), 0.1),
        'lam_k2': nrm((L, QK_DIM), 0.1),
        'subln_g': gain((L, V_DIM)),
        'hy_conv_w': nrm((L, SHORT_CONV, HYENA_COLS), SHORT_CONV ** -0.5),
        'hy_conv_b': nrm((L, HYENA_COLS), 0.01),
        'hy_w1': nrm((L, FEAT_DIM, FILTER_HIDDEN), FEAT_DIM ** -0.5),
        'hy_b1': nrm((L, FILTER_HIDDEN), 0.01),
        'hy_f1': gain((L, FILTER_HIDDEN)),
        'hy_w2': nrm((L, FILTER_HIDDEN, FILTER_HIDDEN), FILTER_HIDDEN ** -0.5),
        'hy_b2': nrm((L, FILTER_HIDDEN), 0.01),
        'hy_f2': gain((L, FILTER_HIDDEN)),
        'hy_w3': nrm((L, FILTER_HIDDEN, HYENA_ORDER * 2 * HYENA_WIDTH), 0.07 * FILTER_HIDDEN ** -0.5),
        'hy_skip': nrm((L, HYENA_ORDER, HYENA_WIDTH), 0.1),
        'hy_out_g': gain((L, HYENA_WIDTH)),
        'w_out': nrm((L, MIX_WIDTH, D_MODEL), MIX_WIDTH ** -0.5),
        'router_w': nrm((L, D_MODEL, N_EXPERTS), D_MODEL ** -0.5),
        'router_b': nrm((L, N_EXPERTS), 0.01),
        'exp_w1': nrm((L, N_EXPERTS, D_MODEL, 2 * D_EXPERT), D_MODEL ** -0.5),
        'exp_b1': nrm((L, N_EXPERTS, 2 * D_EXPERT), 0.01),
        'exp_w2': nrm((L, N_EXPERTS, D_EXPERT, D_MODEL), D_EXPERT ** -0.5),
        'exp_b2': nrm((L, N_EXPERTS, D_MODEL), 0.01),
    }


def reference(x, c, ctx, c_ctx, w_mod, b_mod, norm1_g, norm2_g, w_in, q_norm_g, k_norm_g,
              lam_q1, lam_k1, lam_q2, lam_k2, subln_g, hy_conv_w, hy_conv_b, hy_w1, hy_b1,
              hy_f1, hy_w2, hy_b2, hy_f2, hy_w3, hy_skip, hy_out_g, w_out, router_w, router_b,
              exp_w1, exp_b1, exp_w2, exp_b2):
    for l in range(DEPTH):
        p = {
            'w_mod': w_mod[l], 'b_mod': b_mod[l], 'norm1_g': norm1_g[l], 'norm2_g': norm2_g[l],
            'w_in': w_in[l], 'q_norm_g': q_norm_g[l], 'k_norm_g': k_norm_g[l],
            'lam_q1': lam_q1[l], 'lam_k1': lam_k1[l], 'lam_q2': lam_q2[l], 'lam_k2': lam_k2[l],
            'subln_g': subln_g[l], 'hy_conv_w': hy_conv_w[l], 'hy_conv_b': hy_conv_b[l],
            'hy_w1': hy_w1[l], 'hy_b1': hy_b1[l], 'hy_f1': hy_f1[l], 'hy_w2': hy_w2[l],
            'hy_b2': hy_b2[l], 'hy_f2': hy_f2[l], 'hy_w3': hy_w3[l], 'hy_skip': hy_skip[l],
            'hy_out_g': hy_out_g[l], 'w_out': w_out[l], 'router_w': router_w[l],
            'router_b': router_b[l], 'exp_w1': exp_w1[l], 'exp_b1': exp_b1[l],
            'exp_w2': exp_w2[l], 'exp_b2': exp_b2[l],
        }
        lam_init = 0.8 - 0.6 * math.exp(-0.3 * l)
        x, ctx = trunk_layer(x, ctx, c, c_ctx, p, lam_init, update_ctx=(l < DEPTH - 1))
    return x
```

```python
import math
import os
from contextlib import ExitStack
KDBG = os.environ.get('KDBG', '')

import numpy as np
import ml_dtypes
import concourse.bass as bass
import concourse.mybir as mybir
from concourse.bass_utils import run_bass_kernel_spmd

F32 = mybir.dt.float32
BF16 = mybir.dt.bfloat16
AF = mybir.ActivationFunctionType
ALU = mybir.AluOpType
AX = mybir.AxisListType

D = 1024
S = 4096
LC = 256
NKT = 34
NT = 32
EPS = 1e-6
NE = 32
LAM_INIT = 0.8 - 0.6 * math.exp(0.0)
NFFT = 8192


class Buf:
    __slots__ = ("w", "r", "sem", "dcnt", "name")

    def __init__(self, name):
        self.name = name
        self.w = None
        self.r = {}
        self.sem = None
        self.dcnt = 0


class KB:
    def __init__(self, nc, es):
        self.nc = nc
        self.es = es
        self.eng = {"pe": nc.tensor, "act": nc.scalar, "dve": nc.vector, "pool": nc.gpsimd, "sp": nc.sync}
        self.sem = {n: es.enter_context(nc.semaphore("sem_" + n)) for n in self.eng}
        self.cnt = {n: 0 for n in self.eng}
        self.waited = {n: {} for n in self.eng}
        self.dma_bufs = []

    def buf(self, name, dma=False):
        b = Buf(name)
        if dma:
            b.sem = self.es.enter_context(self.nc.semaphore("d_" + name))
            self.dma_bufs.append(b)
        return b

    def _wait(self, e, ev):
        if ev is None:
            return
        key, sem, val, src = ev
        if src == "pe" and e == "pe":
            return
        if self.waited[e].get(key, 0) >= val:
            return
        self.eng[e].wait_ge(sem, val)
        self.waited[e][key] = val

    def _deps(self, e, reads, writes):
        for b in reads:
            self._wait(e, b.w)
        for b in writes:
            self._wait(e, b.w)
            for ev in list(b.r.values()):
                self._wait(e, ev)

    def op(self, e, fn, reads=(), writes=(), sig=True):
        self._deps(e, reads, writes)
        ins = fn(self.eng[e])
        if sig:
            self.cnt[e] += 1
            ins.then_inc(self.sem[e], 1)
            val = self.cnt[e]
        else:
            val = self.cnt[e] + 1
        ev = (e, self.sem[e], val, e)
        for b in reads:
            b.r[e] = ev
        for b in writes:
            b.w = ev
            b.r = {}
        return ev

    def dma(self, e, out, in_, sb, reads=(), writes=()):
        self._deps(e, reads, writes)
        ins = self.eng[e].dma_start(out=out, in_=in_)
        sb.dcnt += 16
        ins.then_inc(sb.sem, 16)
        ev = (id(sb), sb.sem, sb.dcnt, "dma")
        for b in reads:
            b.r[id(sb)] = ev
        for b in writes:
            b.w = ev
            b.r = {}
        return ev

    def barrier(self):
        evs = [(n, self.sem[n], self.cnt[n], "x") for n in self.eng if self.cnt[n] > 0]
        evs += [(id(b), b.sem, b.dcnt, "dma") for b in self.dma_bufs if b.dcnt > 0]
        for e in self.eng:
            for ev in evs:
                if ev[0] == e:
                    continue
                self._wait(e, ev)


def build_program(stop_after="E", debug=False):
    nc = bass.Bass("TRN2", target_bir_lowering=False)
    LV = {"0": 0, "Ah": 1, "Aq": 2, "A": 3, "B": 4, "C": 5, "D": 6, "E": 7}[stop_after]

    def din(name, shape, dt=F32):
        return nc.dram_tensor(name, list(shape), dt, kind="ExternalInput").ap()

    def dscr(name, shape, dt=BF16):
        return nc.dram_tensor(name, list(shape), dt, kind=("ExternalOutput" if debug else "Internal")).ap()

    x_d = din("x", [S, D])
    ctx_d = din("ctx", [LC, D])
    csil_d = din("csil", [128, 16])
    wmod_d = din("w_mod", [D, 6 * D])
    bmodc_d = din("bmod_col", [128, 48])
    bmodr_d = din("bmod_row", [1, 6 * D])
    n1g_d = din("n1g_col", [128, 8])
    n2g_d = din("n2g_col", [128, 8])
    win_d = din("w_in", [D, 3072])
    qkg_d = din("qkg_row", [1, 1024])
    lamv_d = din("lam_row", [1, 256])
    subg_d = din("subg_row", [1, 128])
    cw_d = din("convw", [1, 4608])
    cb_d = din("convb", [1, 1536])
    hw1_d = din("hy_w1", [17, 64])
    hcol_d = din("hy_cols", [64, 4])
    hw2_d = din("hy_w2", [64, 64])
    hw3_d = din("hy_w3", [64, 2048])
    hskip_d = din("hy_skip", [1, 1024])
    hyg_d = din("hyg_row", [1, 512])
    wout_d = din("w_out", [D, D])
    rw_d = din("router_w", [D, NE])
    rb_d = din("router_b", [1, NE])
    ew1_d = din("exp_w1", [NE, D, 2 * D])
    eb1_d = din("eb1_col", [NE, 128, 16])
    ew2_d = din("exp_w2", [NE, D, D])
    eb2_d = din("exp_b2", [NE, D])
    ident_d = din("ident", [128, 128])
    ropec_d = din("rope_c", [128, NKT * 64])
    ropea_d = din("rope_sa", [128, NKT * 32])
    ropeb_d = din("rope_sb", [128, NKT * 32])
    featsT_d = din("featsT", [17, S])
    window_d = din("window", [S, 512])
    ctk_d = din("dft_ctk", [NT, 128, 4096], BF16)
    stk_d = din("dft_stk", [NT, 128, 4096], BF16)
    ckt_d = din("dft_ckt", [NT, 128, 4096], BF16)
    skt_d = din("dft_skt", [NT, 128, 4096], BF16)

    out_d = nc.dram_tensor("out", [S, D], F32, kind="ExternalOutput").ap()

    QT = dscr("QT", [4, 128, S])
    KT = dscr("KT", [4, 128, NKT * 128])
    VV = dscr("VV", [NKT * 128, 512])
    UU = dscr("UU", [S, 1536])
    AT = dscr("AT", [S, 512])
    Z2 = dscr("Z2", [S, 512])
    H2T = dscr("H2T", [8, 128, S])
    GATES = dscr("GATES", [S, NE], F32)

    with ExitStack() as es:
        kb = KB(nc, es)

        def sb(st, name, shape, dt=F32):
            return st.enter_context(nc.sbuf_tensor("sb_" + name, list(shape), dt))

        def ps(st, name, shape, dt=F32):
            return st.enter_context(nc.psum_tensor("ps_" + name, list(shape), dt))

        ident_f = sb(es, "ident_f", [128, 128]); b_identf = kb.buf("identf", dma=True)
        ident_b = sb(es, "ident_b", [128, 128], BF16); b_identb = kb.buf("identb", dma=True)
        ones_f = sb(es, "ones_f", [128, 128]); b_onesf = kb.buf("onesf")
        ones_b = sb(es, "ones_b", [128, 128], BF16); b_onesb = kb.buf("onesb")
        modcol = sb(es, "modcol", [128, 96]); b_modcol = kb.buf("modcol")
        AB = sb(es, "AB", [128, 64]); b_AB = kb.buf("AB")
        G1row = sb(es, "G1row", [128, D]); b_G1 = kb.buf("G1row")
        G2row = sb(es, "G2row", [128, D]); b_G2 = kb.buf("G2row")
        nlam = sb(es, "nlam", [128, 1]); b_nlam = kb.buf("nlam")

        kb.dma("sp", ident_f[:], ident_d, b_identf, writes=[b_identf])
        kb.dma("pool", ident_b[:], ident_d, b_identb, writes=[b_identb])
        kb.op("dve", lambda e: e.memset(ones_f[:], 1.0), writes=[b_onesf])
        kb.op("dve", lambda e: e.memset(ones_b[:], 1.0), writes=[b_onesb])

        with ExitStack() as st:
            csil = sb(st, "csil", [128, 16]); b_csil = kb.buf("csil", dma=True)
            scb = sb(st, "scb", [128, 16], BF16); b_scb = kb.buf("scb")
            bmodc = sb(st, "bmodc", [128, 48]); b_bmodc = kb.buf("bmodc", dma=True)
            bmodr = sb(st, "bmodr", [1, 6 * D]); b_bmodr = kb.buf("bmodr", dma=True)
            n1g = sb(st, "n1g", [128, 8]); b_n1g = kb.buf("n1g", dma=True)
            n2g = sb(st, "n2g", [128, 8]); b_n2g = kb.buf("n2g", dma=True)
            lamv = sb(st, "lamv", [128, 256]); b_lamv = kb.buf("lamv", dma=True)
            lamt = sb(st, "lamt", [128, 128]); b_lamt = kb.buf("lamt")
            lams = sb(st, "lams", [128, 2]); b_lams = kb.buf("lams")
            grow = sb(st, "grow", [1, 2 * D]); b_grow = kb.buf("grow")
            tmp8 = sb(st, "tmp8", [128, 16]); b_tmp8 = kb.buf("tmp8")
            wm = [sb(st, f"wm{i}", [128, 8, D], BF16) for i in range(2)]
            b_wm = [kb.buf(f"wm{i}", dma=True) for i in range(2)]
            p_col = ps(st, "p_col", [128, 512]); b_pcol = kb.buf("pcol")
            p_row = ps(st, "p_row", [128, 512]); b_prow = kb.buf("prow")
            p_bc = [ps(st, f"p_bc{i}", [128, 512]) for i in range(2)]
            b_pbc = [kb.buf(f"pbc{i}") for i in range(2)]

            kb.dma("sp", csil[:], csil_d, b_csil, writes=[b_csil])
            kb.dma("sp", bmodc[:], bmodc_d, b_bmodc, writes=[b_bmodc])
            kb.dma("sp", bmodr[:], bmodr_d, b_bmodr, writes=[b_bmodr])
            kb.dma("sp", n1g[:], n1g_d, b_n1g, writes=[b_n1g])
            kb.dma("sp", n2g[:], n2g_d, b_n2g, writes=[b_n2g])
            kb.dma("sp", lamv[:], lamv_d.broadcast_to([128, 256]), b_lamv, writes=[b_lamv])
            kb.op("act", lambda e: e.activation(out=scb[:], in_=csil[:], func=AF.Silu), reads=[b_csil], writes=[b_scb])
            scv = scb[:].rearrange("p (c w) -> p c w", w=2)
            wmod_v = wmod_d.rearrange("(c p) f -> p c f", p=128)
            for i in range(6):
                sl = i % 2
                kb.dma("pool", wm[sl][:], wmod_v[:, :, i * D:(i + 1) * D], b_wm[sl], writes=[b_wm[sl]])
                if i in (0, 1, 3, 4):
                    for fc in range(8):
                        col = (i * 8 + fc) * 2
                        for c in range(8):
                            kb.op("pe", lambda e, c=c, fc=fc, col=col: e.matmul(
                                p_col[:, col:col + 2], wm[sl][:, c, fc * 128:(fc + 1) * 128], scv[:, c, :],
                                start=(c == 0), stop=(c == 7)),
                                reads=[b_wm[sl], b_scb], writes=[b_pcol], sig=(c == 7))
                else:
                    gi = 0 if i == 2 else 1
                    for hh in range(2):
                        for c in range(8):
                            kb.op("pe", lambda e, c=c, hh=hh: e.matmul(
                                p_row[0:1, :], scv[:, c, 0:1], wm[sl][:, c, hh * 512:(hh + 1) * 512],
                                start=(c == 0), stop=(c == 7)),
                                reads=[b_wm[sl], b_scb], writes=[b_prow], sig=(c == 7))
                        kb.op("dve", lambda e, hh=hh, gi=gi, i=i: e.tensor_tensor(
                            out=grow[0:1, gi * D + hh * 512: gi * D + (hh + 1) * 512], in0=p_row[0:1, :],
                            in1=bmodr[0:1, i * D + hh * 512: i * D + (hh + 1) * 512], op=ALU.add),
                            reads=[b_prow, b_bmodr], writes=[b_grow])
            kb.op("dve", lambda e: e.tensor_tensor(
                out=modcol[:, :].rearrange("p (a w) -> p a w", w=2), in0=p_col[:, 0:96].rearrange("p (a w) -> p a w", w=2),
                in1=bmodc[:, :].unsqueeze(2).to_broadcast([128, 48, 2]), op=ALU.add),
                reads=[b_pcol, b_bmodc], writes=[b_modcol])
            mc = modcol[:, :].rearrange("p (i c w) -> p i c w", i=6, c=8)
            ABv = AB[:, :].rearrange("p (j c) -> p j c", c=8)
            for j, (i, w, g) in enumerate([(1, 0, n1g), (1, 1, n1g), (None, None, None), (None, None, None), (4, 0, n2g)]):
                if i is None:
                    continue
                kb.op("dve", lambda e, i=i, w=w: e.tensor_scalar(tmp8[:, 0:8], mc[:, i, :, w], 1.0, None, ALU.add),
                      reads=[b_modcol], writes=[b_tmp8])
                kb.op("dve", lambda e, j=j, g=g: e.tensor_tensor(out=ABv[:, j, :], in0=tmp8[:, 0:8], in1=g[:, :], op=ALU.mult),
                      reads=[b_tmp8, b_n1g, b_n2g], writes=[b_AB])
            kb.op("dve", lambda e: e.tensor_copy(out=ABv[:, 2, :], in_=mc[:, 0, :, 0]), reads=[b_modcol], writes=[b_AB])
            kb.op("dve", lambda e: e.tensor_copy(out=ABv[:, 3, :], in_=mc[:, 0, :, 1]), reads=[b_modcol], writes=[b_AB])
            kb.op("dve", lambda e: e.tensor_copy(out=ABv[:, 5, :], in_=mc[:, 3, :, 0]), reads=[b_modcol], writes=[b_AB])
            for gi, (G, bG) in enumerate(((G1row, b_G1), (G2row, b_G2))):
                for hh in range(2):
                    k = hh
                    kb.op("pe", lambda e, gi=gi, hh=hh, k=k: e.matmul(
                        p_bc[k][:, :], ones_f[0:1, :], grow[0:1, gi * D + hh * 512: gi * D + (hh + 1) * 512],
                        start=True, stop=True), reads=[b_onesf, b_grow], writes=[b_pbc[k]])
                    kb.op("act", lambda e, G=G, hh=hh, k=k: e.activation(
                        out=G[:, hh * 512:(hh + 1) * 512], in_=p_bc[k][:, :], func=AF.Copy),
                        reads=[b_pbc[k]], writes=[bG])
            for j in range(2):
                kb.op("dve", lambda e, j=j: e.tensor_tensor(
                    out=lamt[:, j * 64:(j + 1) * 64], in0=lamv[:, j * 128: j * 128 + 64],
                    in1=lamv[:, j * 128 + 64: j * 128 + 128], op=ALU.mult), reads=[b_lamv], writes=[b_lamt])
            kb.op("dve", lambda e: e.tensor_reduce(out=lams[:, :], in_=lamt[:, :].rearrange("p (j d) -> p j d", d=64),
                                                   axis=AX.X, op=ALU.add), reads=[b_lamt], writes=[b_lams])
            kb.op("act", lambda e: e.activation(out=lams[:, :], in_=lams[:, :], func=AF.Exp), reads=[b_lams], writes=[b_lams])
            kb.op("dve", lambda e: e.tensor_tensor(out=nlam[:, :], in0=lams[:, 1:2], in1=lams[:, 0:1], op=ALU.subtract),
                  reads=[b_lams], writes=[b_nlam])
            kb.op("dve", lambda e: e.tensor_scalar(nlam[:, :], nlam[:, :], -LAM_INIT, None, ALU.add),
                  reads=[b_nlam], writes=[b_nlam])
            kb.barrier()

        ABv = AB[:, :].rearrange("p (j c) -> p j c", c=8)

        def rms_rstd(e_act_reads, src_ap, junk_ap, ssq_ap, rstd_ap, n, bufs_r, b_junk, b_ssq, b_rstd):
            kb.op("dve", lambda e: e.memset(ssq_ap, 0.0), writes=[b_ssq])
            kb.op("act", lambda e: e.activation(out=junk_ap, in_=src_ap, func=AF.Square, accum_out=ssq_ap),
                  reads=bufs_r, writes=[b_junk, b_ssq])
            kb.op("act", lambda e: e.activation(out=rstd_ap, in_=ssq_ap, func=AF.Sqrt, bias=EPS, scale=1.0 / n),
                  reads=[b_ssq], writes=[b_rstd])
            kb.op("dve", lambda e: e.reciprocal(out=rstd_ap, in_=rstd_ap), reads=[b_rstd], writes=[b_rstd])

        if LV >= 1:
          with ExitStack() as st:
            HW = NKT * 128 + 2
            hT = sb(st, "hT", [128, 8, HW], BF16)
            b_hTa = [kb.buf(f"hTa{i}") for i in range(NKT)]
            b_hTb = [kb.buf(f"hTb{i}") for i in range(NKT)]
            b_hpad = kb.buf("hpad")
            XOFF = LC + 1

            def hcol(i):
                return i * 128 if i < 2 else XOFF + (i - 2) * 128

            kb.op("pool", lambda e: e.memset(hT[:, :, LC:LC + 1], 0.0), writes=[b_hpad])
            kb.op("pool", lambda e: e.memset(hT[:, :, HW - 1:HW], 0.0), writes=[b_hpad])
            with ExitStack() as s2:
                xt = [sb(s2, f"xt{i}", [128, D]) for i in range(3)]
                b_xt = [kb.buf(f"xt{i}", dma=True) for i in range(3)]
                junk = sb(s2, "junkA", [128, D], BF16); b_junk = kb.buf("junkA")
                ssq = [sb(s2, f"ssqA{i}", [128, 2]) for i in range(3)]
                b_ssq = [kb.buf(f"ssqA{i}") for i in range(3)]
                xn = [sb(s2, f"xnA{i}", [128, D], BF16) for i in range(2)]
                b_xn = [kb.buf(f"xnA{i}") for i in range(2)]
                pT = [ps(s2, f"pTA{i}", [128, D], BF16) for i in range(2)]
                b_pT = [kb.buf(f"pTA{i}") for i in range(2)]
                srcs = [ctx_d[0:128, :], ctx_d[128:256, :]] + [x_d[i * 128:(i + 1) * 128, :] for i in range(NT)]
                for i in range(NKT if 't1' not in KDBG else 1):
                    s3 = i % 3
                    s2_ = i % 2
                    kb.dma("sp", xt[s3][:], srcs[i], b_xt[s3], writes=[b_xt[s3]])
                    rms_rstd(None, xt[s3][:], junk[:], ssq[s3][:, 0:1], ssq[s3][:, 1:2], D, [b_xt[s3]], b_junk, b_ssq[s3], b_ssq[s3])
                    kb.op("dve", lambda e, s3=s3, s2_=s2_: e.tensor_scalar(xn[s2_][:], xt[s3][:], ssq[s3][:, 1:2], None, ALU.mult),
                          reads=[b_xt[s3], b_ssq[s3]], writes=[b_xn[s2_]])
                    if 'notr' in KDBG:
                        continue
                    for c in range(8):
                        kb.op("pe", lambda e, c=c, s2_=s2_: e.transpose(pT[s2_][:, c * 128:(c + 1) * 128],
                                                                          xn[s2_][:, c * 128:(c + 1) * 128], ident_b[:]),
                              reads=[b_xn[s2_], b_identb], writes=[b_pT[s2_]], sig=(c == 7))
                    ja, jb = (1, 3) if i < 2 else (0, 2)
                    c0 = hcol(i)
                    if 'noevac' in KDBG:
                        continue
                    for c in range(8):
                        if False:
                            kb.op("act", lambda e, c=c, s2_=s2_, ja=ja, jb=jb, c0=c0: e.activation(
                                out=hT[:, c, c0:c0 + 128], in_=pT[s2_][:, c * 128:(c + 1) * 128], func=AF.Identity,
                                scale=ABv[:, ja, c:c + 1], bias=ABv[:, jb, c:c + 1]),
                                reads=[b_pT[s2_], b_AB], writes=[b_hTa[i]])
                        else:
                            kb.op("dve", lambda e, c=c, s2_=s2_, ja=ja, jb=jb, c0=c0: e.tensor_scalar(
                                hT[:, c, c0:c0 + 128], pT[s2_][:, c * 128:(c + 1) * 128],
                                ABv[:, ja, c:c + 1], ABv[:, jb, c:c + 1], ALU.mult, ALU.add),
                                reads=[b_pT[s2_], b_AB], writes=[b_hTb[i]])
                kb.barrier()

            if LV >= 2:
             with ExitStack() as s2:
                 wq = sb(s2, "wq", [128, 8, 1536], BF16); b_wq = kb.buf("wq", dma=True)
                 qkg = sb(s2, "qkg", [128, 1024]); b_qkg = kb.buf("qkg", dma=True)
                 ropec = sb(s2, "ropec", [128, NKT * 64]); b_ropec = kb.buf("ropec", dma=True)
                 ropea = sb(s2, "ropea", [128, NKT * 32]); b_ropea = kb.buf("ropea", dma=True)
                 ropeb = sb(s2, "ropeb", [128, NKT * 32]); b_ropeb = kb.buf("ropeb", dma=True)
                 sq = sb(s2, "sqB", [128, 1024]); b_sq = kb.buf("sqB")
                 s16 = [sb(s2, f"s16_{i}", [128, 16]) for i in range(2)]
                 b_s16 = [kb.buf(f"s16_{i}") for i in range(2)]
                 t0 = sb(s2, "t0B", [128, 1024]); b_t0 = kb.buf("t0B")
                 t1 = sb(s2, "t1B", [128, 1024]); b_t1 = kb.buf("t1B")
                 t2 = sb(s2, "t2B", [128, 1024]); b_t2 = kb.buf("t2B")
                 qkb = [sb(s2, f"qkb{i}", [128, 1024], BF16) for i in range(2)]
                 b_qkb = [kb.buf(f"qkb{i}") for i in range(2)]
                 qkT = [sb(s2, f"qkT{i}", [128, 1024], BF16) for i in range(2)]
                 b_qkT = [kb.buf(f"qkT{i}", dma=True) for i in range(2)]
                 vt = [sb(s2, f"vt{i}", [128, 512], BF16) for i in range(2)]
                 b_vt = [kb.buf(f"vt{i}", dma=True) for i in range(2)]
                 p_qk = [ps(s2, f"p_qk{i}", [128, 1024]) for i in range(2)]
                 b_pqk = [kb.buf(f"pqk{i}") for i in range(2)]
                 p_v = [ps(s2, f"p_v{i}", [128, 512]) for i in range(2)]
                 b_pv = [kb.buf(f"pv{i}") for i in range(2)]
                 p_t = ps(s2, "p_tB", [128, 1024], BF16); b_pt = kb.buf("ptB")

                 win_v = win_d.rearrange("(c p) f -> p c f", p=128)
                 for c in range(8):
                     kb.dma("pool", wq[:, c, :], win_v[:, c, 0:1536], b_wq, writes=[b_wq])
                 kb.dma("sp", qkg[:], qkg_d.broadcast_to([128, 1024]), b_qkg, writes=[b_qkg])
                 kb.dma("sp", ropec[:], ropec_d, b_ropec, writes=[b_ropec])
                 kb.dma("sp", ropea[:], ropea_d, b_ropea, writes=[b_ropea])
                 kb.dma("sp", ropeb[:], ropeb_d, b_ropeb, writes=[b_ropeb])

                 for i in range(NKT):
                     sl = i % 2
                     c0 = hcol(i)
                     for half in range(2):
                         for c in range(8):
                             kb.op("pe", lambda e, c=c, half=half, sl=sl, c0=c0: e.matmul(
                                 p_qk[sl][:, half * 512:(half + 1) * 512], hT[:, c, c0:c0 + 128],
                                 wq[:, c, half * 512:(half + 1) * 512], start=(c == 0), stop=(c == 7)),
                                 reads=[b_hTa[i], b_hTb[i], b_wq], writes=[b_pqk[sl]], sig=(c == 7 and half == 1))
                     for c in range(8):
                         kb.op("pe", lambda e, c=c, sl=sl, c0=c0: e.matmul(
                             p_v[sl][:, :], hT[:, c, c0:c0 + 128], wq[:, c, 1024:1536], start=(c == 0), stop=(c == 7)),
                             reads=[b_hTa[i], b_hTb[i], b_wq], writes=[b_pv[sl]], sig=(c == 7))
                     kb.op("act", lambda e, sl=sl: e.activation(out=vt[sl][:], in_=p_v[sl][:, :], func=AF.Copy),
                           reads=[b_pv[sl]], writes=[b_vt[sl]])
                     kb.dma("sp", VV[i * 128:(i + 1) * 128, :], vt[sl][:], b_vt[sl], reads=[b_vt[sl]])
                     kb.op("act", lambda e, sl=sl: e.activation(out=sq[:], in_=p_qk[sl][:, :], func=AF.Square),
                           reads=[b_pqk[sl]], writes=[b_sq])
                     kb.op("dve", lambda e, sl=sl: e.tensor_reduce(out=s16[sl][:], in_=sq[:].rearrange("p (g d) -> p g d", d=64),
                                                                   axis=AX.X, op=ALU.add), reads=[b_sq], writes=[b_s16[sl]])
                     kb.op("act", lambda e, sl=sl: e.activation(out=s16[sl][:], in_=s16[sl][:], func=AF.Sqrt, bias=EPS, scale=1.0 / 64),
                           reads=[b_s16[sl]], writes=[b_s16[sl]])
                     kb.op("dve", lambda e, sl=sl: e.reciprocal(out=s16[sl][:], in_=s16[sl][:]), reads=[b_s16[sl]], writes=[b_s16[sl]])
                     kb.op("dve", lambda e, sl=sl: e.tensor_tensor(
                         out=t0[:].rearrange("p (g d) -> p g d", d=64), in0=p_qk[sl][:, :].rearrange("p (g d) -> p g d", d=64),
                         in1=s16[sl][:].unsqueeze(2).to_broadcast([128, 16, 64]), op=ALU.mult),
                         reads=[b_pqk[sl], b_s16[sl]], writes=[b_t0])
                     kb.op("pool", lambda e: e.tensor_tensor(out=t0[:], in0=t0[:], in1=qkg[:], op=ALU.mult),
                           reads=[b_t0, b_qkg], writes=[b_t0])
                     kb.op("dve", lambda e, i=i: e.tensor_tensor(
                         out=t1[:].rearrange("p (g d) -> p g d", d=64), in0=t0[:].rearrange("p (g d) -> p g d", d=64),
                         in1=ropec[:, i * 64:(i + 1) * 64].unsqueeze(1).to_broadcast([128, 16, 64]), op=ALU.mult),
                         reads=[b_t0, b_ropec], writes=[b_t1])
                     t0v = t0[:].rearrange("p (g r h d) -> p g r h d", g=16, r=2, h=2)
                     t2v = t2[:].rearrange("p (g r h d) -> p g r h d", g=16, r=2, h=2)
                     kb.op("pool", lambda e, i=i: e.tensor_tensor(
                         out=t2v[:, :, :, 0, :], in0=t0v[:, :, :, 1, :],
                         in1=ropea[:, i * 32:(i + 1) * 32].rearrange("p (r d) -> p r d", r=2).unsqueeze(1).to_broadcast([128, 16, 2, 16]),
                         op=ALU.mult), reads=[b_t0, b_ropea], writes=[b_t2])
                     kb.op("pool", lambda e, i=i: e.tensor_tensor(
                         out=t2v[:, :, :, 1, :], in0=t0v[:, :, :, 0, :],
                         in1=ropeb[:, i * 32:(i + 1) * 32].rearrange("p (r d) -> p r d", r=2).unsqueeze(1).to_broadcast([128, 16, 2, 16]),
                         op=ALU.mult), reads=[b_t0, b_ropeb], writes=[b_t2])
                     kb.op("dve", lambda e, sl=sl: e.tensor_tensor(out=qkb[sl][:], in0=t1[:], in1=t2[:], op=ALU.add),
                           reads=[b_t1, b_t2], writes=[b_qkb[sl]])
                     for c in range(8):
                         kb.op("pe", lambda e, c=c, sl=sl: e.transpose(p_t[:, c * 128:(c + 1) * 128],
                                                                        qkb[sl][:, c * 128:(c + 1) * 128], ident_b[:]),
                               reads=[b_qkb[sl], b_identb], writes=[b_pt], sig=(c == 7))
                     kb.op("act", lambda e, sl=sl: e.activation(out=qkT[sl][:], in_=p_t[:, :], func=AF.Copy),
                           reads=[b_pt], writes=[b_qkT[sl]])
                     qv = qkT[sl][:].rearrange("p (a h t) -> p a h t", a=2, h=4)
                     if i >= 2:
                         kb.dma("sp", QT[:, :, (i - 2) * 128:(i - 1) * 128].rearrange("h p t -> p h t"), qv[:, 0, :, :],
                                b_qkT[sl], reads=[b_qkT[sl]])
                     kb.dma("sp", KT[:, :, i * 128:(i + 1) * 128].rearrange("h p t -> p h t"), qv[:, 1, :, :],
                            b_qkT[sl], reads=[b_qkT[sl]])
                 kb.barrier()

            if LV >= 3:
              with ExitStack() as s2:
                wu = [sb(s2, f"wu{j}", [128, 8, 1536], BF16) for j in range(3)]
                b_wu = kb.buf("wu")
                wst = [sb(s2, f"wst{i}", [128, 1536]) for i in range(2)]
                b_wst = [kb.buf(f"wst{i}", dma=True) for i in range(2)]
                cw = sb(s2, "cw", [128, 3 * 1536]); b_cw = kb.buf("cw", dma=True)
                cb = sb(s2, "cb", [128, 1536]); b_cb = kb.buf("cb", dma=True)
                ut = [sb(s2, f"ut{i}", [128, 1536], BF16) for i in range(2)]
                b_ut = [kb.buf(f"ut{i}", dma=True) for i in range(2)]
                p_u = [ps(s2, f"p_u{i}", [128, 512]) for i in range(6)]
                b_pu = [kb.buf(f"pu{i}") for i in range(6)]
                win_v = win_d.rearrange("(c p) f -> p c f", p=128)
                kb.dma("sp", cw[:], cw_d.broadcast_to([128, 4608]), b_cw, writes=[b_cw])
                kb.dma("sp", cb[:], cb_d.broadcast_to([128, 1536]), b_cb, writes=[b_cb])
                for c in range(8):
                    sl = c % 2
                    kb.dma("sp", wst[sl][:], win_v[:, c, 1536:3072], b_wst[sl], writes=[b_wst[sl]])
                    for j in range(3):
                        kb.op("dve" if j < 2 else "pool", lambda e, j=j, c=c, sl=sl: e.tensor_tensor(
                            out=wu[j][:, c, :], in0=wst[sl][:], in1=cw[:, j * 1536:(j + 1) * 1536], op=ALU.mult),
                            reads=[b_wst[sl], b_cw], writes=[b_wu])
                for i in range(NT):
                    sl = i % 2
                    c0 = XOFF + i * 128
                    rd = [b_hTa[i + 2], b_hTb[i + 2], b_wu, b_hpad]
                    if i > 0:
                        rd += [b_hTa[i + 1], b_hTb[i + 1]]
                    if i < NT - 1:
                        rd += [b_hTa[i + 3], b_hTb[i + 3]]
                    for g in range(3):
                        pb = sl * 3 + g
                        n = 0
                        for j in range(3):
                            for c in range(8):
                                kb.op("pe", lambda e, c=c, j=j, g=g, pb=pb, c0=c0, n=n: e.matmul(
                                    p_u[pb][:, :], hT[:, c, c0 + j - 1:c0 + j - 1 + 128], wu[j][:, c, g * 512:(g + 1) * 512],
                                    start=(n == 0), stop=(n == 23)), reads=rd, writes=[b_pu[pb]], sig=(n == 23))
                                n += 1
                        kb.op("dve", lambda e, g=g, pb=pb, sl=sl: e.tensor_tensor(
                            out=ut[sl][:, g * 512:(g + 1) * 512], in0=p_u[pb][:, :], in1=cb[:, g * 512:(g + 1) * 512], op=ALU.add),
                            reads=[b_pu[pb], b_cb], writes=[b_ut[sl]])
                    kb.dma("sp", UU[i * 128:(i + 1) * 128, :], ut[sl][:], b_ut[sl], reads=[b_ut[sl]])
                kb.barrier()

        if LV >= 4:
          with ExitStack() as st:
            qh = [sb(st, f"qh{i}", [128, S], BF16) for i in range(2)]
            b_qh = [kb.buf(f"qh{i}", dma=True) for i in range(2)]
            kh = [sb(st, f"kh{i}", [128, NKT * 128], BF16) for i in range(2)]
            b_kh = [kb.buf(f"kh{i}", dma=True) for i in range(2)]
            vh = [sb(st, f"vh{i}", [128, NKT, 130], BF16) for i in range(2)]
            b_vh = [kb.buf(f"vh{i}", dma=True) for i in range(2)]
            subg = sb(st, "subg", [128, 128]); b_subg = kb.buf("subg", dma=True)
            NPT = 4
            pt_ = [sb(st, f"ptile{i}", [128, 512], BF16) for i in range(NPT)]
            b_ptile = [kb.buf(f"ptile{i}") for i in range(NPT)]
            p_s = [ps(st, f"p_s{i}", [128, 512]) for i in range(3)]
            b_ps = [kb.buf(f"ps{i}") for i in range(3)]
            p_o = [ps(st, f"p_o{i}", [128, 512]) for i in range(4)]
            b_po = [kb.buf(f"po{i}") for i in range(4)]
            rr = [sb(st, f"rr{i}", [128, 4]) for i in range(2)]
            b_rr = [kb.buf(f"rr{i}") for i in range(2)]
            ta = [sb(st, f"ta{i}", [128, 128]) for i in range(2)]
            b_ta = [kb.buf(f"ta{i}") for i in range(2)]
            aa = [sb(st, f"aa{i}", [128, 128]) for i in range(2)]
            b_aa = [kb.buf(f"aa{i}") for i in range(2)]
            junkb = sb(st, "junkBB", [128, 128]); b_junkb = kb.buf("junkBB")
            ao = [sb(st, f"ao{i}", [128, 128], BF16) for i in range(2)]
            b_ao = [kb.buf(f"ao{i}", dma=True) for i in range(2)]

            kb.dma("sp", subg[:], subg_d.broadcast_to([128, 128]), b_subg, writes=[b_subg])
            for sl in range(2):
                kb.op("pool", lambda e, sl=sl: e.memset(vh[sl][:, :, 128:130], 1.0), writes=[b_vh[sl]])
            VVv = VV.rearrange("(t p) (h d) -> h p t d", p=128, h=4)
            nps = 0
            npt = 0
            nq = 0
            for h in range(4):
                sl = h % 2
                kb.dma("sp", qh[sl][:], QT[h], b_qh[sl], writes=[b_qh[sl]])
                kb.dma("sp", kh[sl][:], KT[h], b_kh[sl], writes=[b_kh[sl]])
                kb.dma("sp", vh[sl][:, :, 0:128], VVv[h], b_vh[sl], writes=[b_vh[sl]])
                for qc in range(8):
                    for m in range(2):
                        for kt in range(NKT):
                            a = nps % 3
                            nps += 1
                            kb.op("pe", lambda e, a=a, m=m, kt=kt, qc=qc, sl=sl: e.matmul(
                                p_s[a][:, :], kh[sl][m * 64:(m + 1) * 64, kt * 128:(kt + 1) * 128],
                                qh[sl][m * 64:(m + 1) * 64, qc * 512:(qc + 1) * 512], start=True, stop=True),
                                reads=[b_kh[sl], b_qh[sl]], writes=[b_ps[a]])
                            b = npt % NPT
                            npt += 1
                            kb.op("act", lambda e, a=a, b=b: e.activation(out=pt_[b][:], in_=p_s[a][:, :], func=AF.Exp, scale=0.125),
                                  reads=[b_ps[a]], writes=[b_ptile[b]])
                            for qt in range(4):
                                pb = m * 2 + qt // 2
                                o0 = (qt % 2) * 130
                                kb.op("pe", lambda e, b=b, qt=qt, pb=pb, o0=o0, kt=kt, sl=sl: e.matmul(
                                    p_o[pb][:, o0:o0 + 129], pt_[b][:, qt * 128:(qt + 1) * 128], vh[sl][:, kt, 0:129],
                                    start=(kt == 0 and qt % 2 == 0), stop=(kt == NKT - 1)),
                                    reads=[b_ptile[b], b_vh[sl]], writes=[b_po[pb]], sig=(kt == NKT - 1 and qt % 2 == 1) or qt == 3)
                    for qt in range(4):
                        z = nq % 2
                        nq += 1
                        pb0 = qt // 2
                        pb1 = 2 + qt // 2
                        o0 = (qt % 2) * 130
                        kb.op("dve", lambda e, z=z, pb0=pb0, o0=o0: e.reciprocal(out=rr[z][:, 0:1], in_=p_o[pb0][:, o0 + 128:o0 + 129]),
                              reads=[b_po[pb0]], writes=[b_rr[z]])
                        kb.op("dve", lambda e, z=z, pb1=pb1, o0=o0: e.reciprocal(out=rr[z][:, 1:2], in_=p_o[pb1][:, o0 + 128:o0 + 129]),
                              reads=[b_po[pb1]], writes=[b_rr[z]])
                        kb.op("dve", lambda e, z=z: e.tensor_tensor(out=rr[z][:, 1:2], in0=rr[z][:, 1:2], in1=nlam[:, 0:1], op=ALU.mult),
                              reads=[b_rr[z], b_nlam], writes=[b_rr[z]])
                        kb.op("dve", lambda e, z=z, pb1=pb1, o0=o0: e.tensor_scalar(ta[z][:], p_o[pb1][:, o0:o0 + 128], rr[z][:, 1:2], None, ALU.mult),
                              reads=[b_po[pb1], b_rr[z]], writes=[b_ta[z]])
                        kb.op("dve", lambda e, z=z, pb0=pb0, o0=o0: e.scalar_tensor_tensor(
                            out=aa[z][:], in0=p_o[pb0][:, o0:o0 + 128], scalar=rr[z][:, 0:1], in1=ta[z][:], op0=ALU.mult, op1=ALU.add),
                            reads=[b_po[pb0], b_rr[z], b_ta[z]], writes=[b_aa[z]])
                        rms_rstd(None, aa[z][:], junkb[:], rr[z][:, 2:3], rr[z][:, 3:4], 128, [b_aa[z]], b_junkb, b_rr[z], b_rr[z])
                        kb.op("dve", lambda e, z=z: e.tensor_scalar(rr[z][:, 3:4], rr[z][:, 3:4], 1.0 - LAM_INIT, None, ALU.mult),
                              reads=[b_rr[z]], writes=[b_rr[z]])
                        kb.op("dve", lambda e, z=z: e.scalar_tensor_tensor(
                            out=ao[z][:], in0=aa[z][:], scalar=rr[z][:, 3:4], in1=subg[:], op0=ALU.mult, op1=ALU.mult),
                            reads=[b_aa[z], b_rr[z], b_subg], writes=[b_ao[z]])
                        tok0 = qc * 512 + qt * 128
                        kb.dma("sp", AT[tok0:tok0 + 128, h * 128:(h + 1) * 128], ao[z][:], b_ao[z], reads=[b_ao[z]])
            kb.barrier()

        if LV >= 5:
          with ExitStack() as st:
            h2T = sb(st, "h2T_f", [64, S]); b_h2T = kb.buf("h2Tf")
            w3pm = sb(st, "w3pm", [64, 2048]); b_w3pm = kb.buf("w3pm")
            skipb = sb(st, "skipb", [128, 1024]); b_skipb = kb.buf("skipb", dma=True)
            with ExitStack() as s2:
                featsT = sb(s2, "featsT", [17, S]); b_feats = kb.buf("feats", dma=True)
                hw1 = sb(s2, "hw1", [17, 64]); b_hw1 = kb.buf("hw1", dma=True)
                hw2 = sb(s2, "hw2", [64, 64]); b_hw2 = kb.buf("hw2", dma=True)
                hw3 = sb(s2, "hw3", [64, 2048]); b_hw3 = kb.buf("hw3", dma=True)
                hcol_ = sb(s2, "hcols", [64, 4]); b_hcols = kb.buf("hcols", dma=True)
                fb = sb(s2, "fb", [64, 2]); b_fb = kb.buf("fb")
                negpi = sb(s2, "negpi", [64, 1]); b_negpi = kb.buf("negpi")
                h1T = sb(s2, "h1T_f", [64, S]); b_h1T = kb.buf("h1Tf")
                tmpm = [sb(s2, f"tmpm{i}", [64, 512]) for i in range(2)]
                b_tmpm = [kb.buf(f"tmpm{i}") for i in range(2)]
                wrp = [sb(s2, f"wrp{i}", [64, 512]) for i in range(2)]
                b_wrp = [kb.buf(f"wrp{i}") for i in range(2)]
                p_m = [ps(s2, f"p_m{i}", [128, 512]) for i in range(2)]
                b_pm = [kb.buf(f"pm{i}") for i in range(2)]
                kb.dma("sp", featsT[:], featsT_d, b_feats, writes=[b_feats])
                kb.dma("sp", hw1[:], hw1_d, b_hw1, writes=[b_hw1])
                kb.dma("sp", hw2[:], hw2_d, b_hw2, writes=[b_hw2])
                kb.dma("sp", hw3[:], hw3_d, b_hw3, writes=[b_hw3])
                kb.dma("sp", hcol_[:], hcol_d, b_hcols, writes=[b_hcols])
                kb.dma("sp", skipb[:], hskip_d.broadcast_to([128, 1024]), b_skipb, writes=[b_skipb])
                kb.op("dve", lambda e: e.memset(negpi[:], -math.pi), writes=[b_negpi])
                OFFS = 0.0
                for l in range(2):
                    kb.op("dve", lambda e, l=l: e.tensor_tensor(out=fb[:, l:l + 1], in0=hcol_[:, 2 * l:2 * l + 1],
                                                                in1=hcol_[:, 2 * l + 1:2 * l + 2], op=ALU.mult),
                          reads=[b_hcols], writes=[b_fb])
                    kb.op("dve", lambda e, l=l: e.tensor_scalar(fb[:, l:l + 1], fb[:, l:l + 1], OFFS, None, ALU.add),
                          reads=[b_fb], writes=[b_fb])
                for l in range(2):
                    src, b_src = (featsT, b_feats) if l == 0 else (h1T, b_h1T)
                    dst, b_dst = (h1T, b_h1T) if l == 0 else (h2T, b_h2T)
                    wl, b_wl = (hw1, b_hw1) if l == 0 else (hw2, b_hw2)
                    kdim = 17 if l == 0 else 64
                    for tcn in range(8):
                        a = tcn % 2
                        kb.op("pe", lambda e, a=a, tcn=tcn, src=src, wl=wl, kdim=kdim: e.matmul(
                            p_m[a][0:64, :], wl[0:kdim, :], src[0:kdim, tcn * 512:(tcn + 1) * 512], start=True, stop=True),
                            reads=[b_src, b_wl], writes=[b_pm[a]])
                        kb.op("dve", lambda e, a=a, l=l: e.tensor_scalar(
                            tmpm[a][:], p_m[a][0:64, :], hcol_[:, 2 * l + 1:2 * l + 2], fb[:, l:l + 1], ALU.mult, ALU.add),
                            reads=[b_pm[a], b_hcols, b_fb], writes=[b_tmpm[a]])
                        for (thr, per, cmp_) in ((-math.pi, 2.0 * math.pi, ALU.is_lt), (math.pi, -2.0 * math.pi, ALU.is_gt)):
                            kb.op("dve", lambda e, a=a, thr=thr, per=per, cmp_=cmp_: e.tensor_scalar(wrp[a][:], tmpm[a][:], thr, per, cmp_, ALU.mult),
                                  reads=[b_tmpm[a]], writes=[b_wrp[a]])
                            kb.op("dve", lambda e, a=a: e.tensor_tensor(out=tmpm[a][:], in0=tmpm[a][:], in1=wrp[a][:], op=ALU.add),
                                  reads=[b_tmpm[a], b_wrp[a]], writes=[b_tmpm[a]])
                        kb.op("act", lambda e, a=a, tcn=tcn, dst=dst: e.activation(
                            out=dst[:, tcn * 512:(tcn + 1) * 512], in_=tmpm[a][:], func=AF.Sin),
                            reads=[b_tmpm[a]], writes=[b_dst])
                w3v = hw3[:, :].rearrange("j (n r c) -> j n r c", n=2, r=2)
                w3o = w3pm[:, :].rearrange("j (n r c) -> j n r c", n=2, r=2)
                for n_ in range(2):
                    kb.op("dve", lambda e, n_=n_: e.tensor_tensor(out=w3o[:, n_, 0, :], in0=w3v[:, n_, 0, :], in1=w3v[:, n_, 1, :], op=ALU.add),
                          reads=[b_hw3], writes=[b_w3pm])
                    kb.op("dve", lambda e, n_=n_: e.tensor_tensor(out=w3o[:, n_, 1, :], in0=w3v[:, n_, 0, :], in1=w3v[:, n_, 1, :], op=ALU.subtract),
                          reads=[b_hw3], writes=[b_w3pm])
                kb.barrier()
            w3o = w3pm[:, :].rearrange("j (n r c) -> j n r c", n=2, r=2)

            HC = 256
            vz = sb(st, "vz", [128, NT, HC], BF16); b_vz = kb.buf("vz", dma=True)
            gp = sb(st, "gp", [128, NT, HC], BF16); b_gp = kb.buf("gp")
            gm = sb(st, "gm", [128, NT, HC], BF16); b_gm = kb.buf("gm")
            Yr = sb(st, "Yr", [128, NT, HC], BF16); b_Yr = kb.buf("Yr")
            Yn = sb(st, "Yn", [128, NT, HC], BF16); b_Yn = kb.buf("Yn")
            tabC = [sb(st, f"tabC{i}", [128, NT, 128], BF16) for i in range(2)]
            b_tabC = [kb.buf(f"tabC{i}", dma=True) for i in range(2)]
            tabS = [sb(st, f"tabS{i}", [128, NT, 128], BF16) for i in range(2)]
            b_tabS = [kb.buf(f"tabS{i}", dma=True) for i in range(2)]
            wnd = [sb(st, f"wnd{i}", [128, HC]) for i in range(2)]
            b_wnd = [kb.buf(f"wnd{i}", dma=True) for i in range(2)]
            xg = [sb(st, f"xg{i}", [128, HC], BF16) for i in range(2)]
            b_xg = [kb.buf(f"xg{i}", dma=True) for i in range(2)]
            zo = [sb(st, f"zo{i}", [128, HC], BF16) for i in range(2)]
            b_zo = [kb.buf(f"zo{i}", dma=True) for i in range(2)]
            Hr = sb(st, "Hr", [128, HC]); b_Hr = kb.buf("Hr")
            Hs = sb(st, "Hs", [128, HC]); b_Hs = kb.buf("Hs")
            tt = [sb(st, f"ttC{i}", [128, HC]) for i in range(4)]
            b_tt = [kb.buf(f"ttC{i}") for i in range(4)]
            p_g = [ps(st, f"p_g{i}", [128, 512]) for i in range(2)]
            b_pg = [kb.buf(f"pgC{i}") for i in range(2)]
            p_x = [ps(st, f"p_x{i}", [128, 512]) for i in range(4)]
            b_px = [kb.buf(f"pxC{i}") for i in range(4)]
            p_y = [ps(st, f"p_y{i}", [128, 512]) for i in range(2)]
            b_py = [kb.buf(f"pyC{i}") for i in range(2)]
            ntab = 0
            UUv = UU.rearrange("(t p) f -> p t f", p=128)
            windv = window_d.rearrange("(t p) c -> t p c", p=128)
            for half in range(2):
                hc0 = half * HC
                kb.dma("sp", vz[:], UUv[:, :, hc0:hc0 + HC], b_vz, writes=[b_vz])
                for order in range(2):
                    for tt_ in range(NT):
                        a = tt_ % 2
                        kb.dma("sp", wnd[a][:], windv[tt_][:, hc0:hc0 + HC], b_wnd[a], writes=[b_wnd[a]])
                        for r in range(2):
                            kb.op("pe", lambda e, a=a, r=r, tt_=tt_, order=order, hc0=hc0: e.matmul(
                                p_g[a][:, r * HC:(r + 1) * HC], h2T[:, tt_ * 128:(tt_ + 1) * 128], w3o[:, order, r, hc0:hc0 + HC],
                                start=True, stop=True), reads=[b_h2T, b_w3pm], writes=[b_pg[a]], sig=(r == 1))
                        kb.op("dve", lambda e, a=a, tt_=tt_: e.tensor_tensor(out=gp[:, tt_, :], in0=p_g[a][:, 0:HC], in1=wnd[a][:], op=ALU.mult),
                              reads=[b_pg[a], b_wnd[a]], writes=[b_gp])
                        kb.op("dve", lambda e, a=a, tt_=tt_: e.tensor_tensor(out=gm[:, tt_, :], in0=p_g[a][:, HC:2 * HC], in1=wnd[a][:], op=ALU.mult),
                              reads=[b_pg[a], b_wnd[a]], writes=[b_gm])
                    for kt in range(NT):
                        a = ntab % 2
                        ntab += 1
                        kb.dma("sp", tabC[a][:].rearrange("p c k -> p (c k)"), ctk_d[kt], b_tabC[a], writes=[b_tabC[a]])
                        kb.dma("sp", tabS[a][:].rearrange("p c k -> p (c k)"), stk_d[kt], b_tabS[a], writes=[b_tabS[a]])
                        pc, psn = p_x[2 * (kt % 2)], p_x[2 * (kt % 2) + 1]
                        bpc, bps = b_px[2 * (kt % 2)], b_px[2 * (kt % 2) + 1]
                        for c in range(NT):
                            fl = dict(start=(c == 0), stop=(c == NT - 1))
                            kb.op("pe", lambda e, c=c, a=a, pc=pc, fl=fl: e.matmul(pc[:, 0:HC], tabC[a][:, c, :], vz[:, c, :], **fl),
                                  reads=[b_tabC[a], b_vz], writes=[bpc], sig=False)
                            fl = dict(start=False, stop=(c == NT - 1))
                            kb.op("pe", lambda e, c=c, a=a, pc=pc, fl=fl: e.matmul(pc[:, HC:2 * HC], tabC[a][:, c, :], gp[:, c, :], **fl),
                                  reads=[b_tabC[a], b_gp], writes=[bpc], sig=(c == NT - 1))
                            fl = dict(start=(c == 0), stop=(c == NT - 1))
                            kb.op("pe", lambda e, c=c, a=a, psn=psn, fl=fl: e.matmul(psn[:, 0:HC], tabS[a][:, c, :], vz[:, c, :], **fl),
                                  reads=[b_tabS[a], b_vz], writes=[bps], sig=False)
                            fl = dict(start=False, stop=(c == NT - 1))
                            kb.op("pe", lambda e, c=c, a=a, psn=psn, fl=fl: e.matmul(psn[:, HC:2 * HC], tabS[a][:, c, :], gm[:, c, :], **fl),
                                  reads=[b_tabS[a], b_gm], writes=[bps], sig=(c == NT - 1))
                        sk0 = order * 512 + hc0
                        kb.op("dve", lambda e, pc=pc, sk0=sk0: e.tensor_tensor(out=Hr[:], in0=pc[:, HC:2 * HC], in1=skipb[:, sk0:sk0 + HC], op=ALU.add),
                              reads=[bpc, b_skipb], writes=[b_Hr])
                        kb.op("act", lambda e, psn=psn: e.activation(out=Hs[:], in_=psn[:, HC:2 * HC], func=AF.Copy),
                              reads=[bps], writes=[b_Hs])
                        kb.op("dve", lambda e, pc=pc: e.tensor_tensor(out=tt[0][:], in0=pc[:, 0:HC], in1=Hr[:], op=ALU.mult),
                              reads=[bpc, b_Hr], writes=[b_tt[0]])
                        kb.op("dve", lambda e, psn=psn: e.tensor_tensor(out=tt[1][:], in0=psn[:, 0:HC], in1=Hs[:], op=ALU.mult),
                              reads=[bps, b_Hs], writes=[b_tt[1]])
                        kb.op("pool", lambda e, kt=kt: e.tensor_tensor(out=Yr[:, kt, :], in0=tt[0][:], in1=tt[1][:], op=ALU.subtract),
                              reads=[b_tt[0], b_tt[1]], writes=[b_Yr])
                        kb.op("dve", lambda e, pc=pc: e.tensor_tensor(out=tt[2][:], in0=pc[:, 0:HC], in1=Hs[:], op=ALU.mult),
                              reads=[bpc, b_Hs], writes=[b_tt[2]])
                        kb.op("dve", lambda e, psn=psn: e.tensor_tensor(out=tt[3][:], in0=psn[:, 0:HC], in1=Hr[:], op=ALU.mult),
                              reads=[bps, b_Hr], writes=[b_tt[3]])
                        kb.op("pool", lambda e, kt=kt: e.tensor_tensor(out=Yn[:, kt, :], in0=tt[2][:], in1=tt[3][:], op=ALU.add),
                              reads=[b_tt[2], b_tt[3]], writes=[b_Yn])
                    xoff = 512 * (order + 1) + hc0
                    for t_ in range(NT):
                        a = ntab % 2
                        ntab += 1
                        kb.dma("sp", tabC[a][:].rearrange("p c k -> p (c k)"), ckt_d[t_], b_tabC[a], writes=[b_tabC[a]])
                        kb.dma("sp", tabS[a][:].rearrange("p c k -> p (c k)"), skt_d[t_], b_tabS[a], writes=[b_tabS[a]])
                        z = t_ % 2
                        kb.dma("sp", xg[z][:], UU[t_ * 128:(t_ + 1) * 128, xoff:xoff + HC], b_xg[z], writes=[b_xg[z]])
                        for c in range(NT):
                            kb.op("pe", lambda e, c=c, a=a, z=z: e.matmul(p_y[z][:, 0:HC], tabC[a][:, c, :], Yr[:, c, :], start=(c == 0), stop=False),
                                  reads=[b_tabC[a], b_Yr], writes=[b_py[z]], sig=False)
                            kb.op("pe", lambda e, c=c, a=a, z=z: e.matmul(p_y[z][:, 0:HC], tabS[a][:, c, :], Yn[:, c, :], start=False, stop=(c == NT - 1)),
                                  reads=[b_tabS[a], b_Yn], writes=[b_py[z]], sig=(c == NT - 1))
                        if order == 0:
                            kb.op("dve", lambda e, z=z, t_=t_: e.scalar_tensor_tensor(
                                out=vz[:, t_, :], in0=p_y[z][:, 0:HC], scalar=2.0 / NFFT, in1=xg[z][:], op0=ALU.mult, op1=ALU.mult),
                                reads=[b_py[z], b_xg[z]], writes=[b_vz])
                        else:
                            kb.op("dve", lambda e, z=z: e.scalar_tensor_tensor(
                                out=zo[z][:], in0=p_y[z][:, 0:HC], scalar=2.0 / NFFT, in1=xg[z][:], op0=ALU.mult, op1=ALU.mult),
                                reads=[b_py[z], b_xg[z]], writes=[b_zo[z]])
                            kb.dma("sp", Z2[t_ * 128:(t_ + 1) * 128, hc0:hc0 + HC], zo[z][:], b_zo[z], reads=[b_zo[z]])
            kb.barrier()

        if LV >= 6:
          with ExitStack() as st:
            wo = sb(st, "wo", [128, 8, D], BF16); b_wo = kb.buf("wo")
            wost = [sb(st, f"wost{i}", [128, D]) for i in range(2)]
            b_wost = [kb.buf(f"wost{i}", dma=True) for i in range(2)]
            rw = sb(st, "rw", [128, 8, NE], BF16); b_rw = kb.buf("rw", dma=True)
            rbb = sb(st, "rbb", [128, NE]); b_rbb = kb.buf("rbb", dma=True)
            hyg = sb(st, "hyg", [128, 512]); b_hyg = kb.buf("hyg", dma=True)
            cat = [sb(st, f"cat{i}", [128, D], BF16) for i in range(2)]
            b_cat = [kb.buf(f"cat{i}", dma=True) for i in range(2)]
            z2t = [sb(st, f"z2t{i}", [128, 512], BF16) for i in range(2)]
            b_z2t = [kb.buf(f"z2t{i}", dma=True) for i in range(2)]
            xt = [sb(st, f"xtD{i}", [128, D]) for i in range(2)]
            b_xt = [kb.buf(f"xtD{i}", dma=True) for i in range(2)]
            xnw = [sb(st, f"xnw{i}", [128, D]) for i in range(2)]
            b_xnw = [kb.buf(f"xnw{i}", dma=True) for i in range(2)]
            junk = sb(st, "junkD", [128, D], BF16); b_junk = kb.buf("junkD")
            sD = [sb(st, f"sD{i}", [128, 4]) for i in range(2)]
            b_sD = [kb.buf(f"sD{i}") for i in range(2)]
            catT = [sb(st, f"catT{i}", [128, D], BF16) for i in range(2)]
            b_catT = [kb.buf(f"catT{i}") for i in range(2)]
            xn2 = [sb(st, f"xn2{i}", [128, D], BF16) for i in range(2)]
            b_xn2 = [kb.buf(f"xn2{i}") for i in range(2)]
            h2t = [sb(st, f"h2t{i}", [128, D], BF16) for i in range(2)]
            b_h2t = [kb.buf(f"h2t{i}", dma=True) for i in range(2)]
            lg = [sb(st, f"lg{i}", [128, NE]) for i in range(2)]
            b_lg = [kb.buf(f"lg{i}") for i in range(2)]
            m8 = [sb(st, f"m8{i}", [128, 8]) for i in range(2)]
            b_m8 = [kb.buf(f"m8{i}") for i in range(2)]
            msk = [sb(st, f"msk{i}", [128, NE]) for i in range(2)]
            b_msk = [kb.buf(f"msk{i}") for i in range(2)]
            gt = [sb(st, f"gt{i}", [128, NE]) for i in range(2)]
            b_gt = [kb.buf(f"gt{i}", dma=True) for i in range(2)]
            p_t = [ps(st, f"p_tD{i}", [128, D], BF16) for i in range(2)]
            b_pt = [kb.buf(f"ptD{i}") for i in range(2)]
            p_m = [ps(st, f"p_mD{i}", [128, D]) for i in range(2)]
            b_pmx = [kb.buf(f"pmD{i}") for i in range(2)]
            p_l = ps(st, "p_lD", [128, 512]); b_pl = kb.buf("plD")

            wout_v = wout_d.rearrange("(c p) f -> p c f", p=128)
            for c in range(8):
                sl = c % 2
                kb.dma("sp", wost[sl][:], wout_v[:, c, :], b_wost[sl], writes=[b_wost[sl]])
                kb.op("dve", lambda e, c=c, sl=sl: e.tensor_tensor(out=wo[:, c, :], in0=wost[sl][:], in1=G1row[:], op=ALU.mult),
                      reads=[b_wost[sl], b_G1], writes=[b_wo])
            kb.dma("pool", rw[:], rw_d.rearrange("(c p) e -> p c e", p=128), b_rw, writes=[b_rw])
            kb.dma("sp", rbb[:], rb_d.broadcast_to([128, NE]), b_rbb, writes=[b_rbb])
            kb.dma("sp", hyg[:], hyg_d.broadcast_to([128, 512]), b_hyg, writes=[b_hyg])
            for i in range(NT):
                sl = i % 2
                r0, r1 = i * 128, (i + 1) * 128
                kb.dma("sp", cat[sl][:, 0:512], AT[r0:r1, :], b_cat[sl], writes=[b_cat[sl]])
                kb.dma("sp", z2t[sl][:], Z2[r0:r1, :], b_z2t[sl], writes=[b_z2t[sl]])
                kb.dma("sp", xt[sl][:], x_d[r0:r1, :], b_xt[sl], writes=[b_xt[sl]])
                rms_rstd(None, z2t[sl][:], junk[:, 0:512], sD[sl][:, 0:1], sD[sl][:, 1:2], 512, [b_z2t[sl]], b_junk, b_sD[sl], b_sD[sl])
                kb.op("dve", lambda e, sl=sl: e.scalar_tensor_tensor(
                    out=cat[sl][:, 512:1024], in0=z2t[sl][:], scalar=sD[sl][:, 1:2], in1=hyg[:], op0=ALU.mult, op1=ALU.mult),
                    reads=[b_z2t[sl], b_sD[sl], b_hyg], writes=[b_cat[sl]])
                for c in range(8):
                    kb.op("pe", lambda e, c=c, sl=sl: e.transpose(p_t[0][:, c * 128:(c + 1) * 128], cat[sl][:, c * 128:(c + 1) * 128], ident_b[:]),
                          reads=[b_cat[sl], b_identb], writes=[b_pt[0]], sig=(c == 7))
                kb.op("act", lambda e, sl=sl: e.activation(out=catT[sl][:], in_=p_t[0][:, :], func=AF.Copy),
                      reads=[b_pt[0]], writes=[b_catT[sl]])
                for hh in range(2):
                    for c in range(8):
                        kb.op("pe", lambda e, c=c, hh=hh, sl=sl: e.matmul(
                            p_m[sl][:, hh * 512:(hh + 1) * 512], catT[sl][:, c * 128:(c + 1) * 128], wo[:, c, hh * 512:(hh + 1) * 512],
                            start=(c == 0), stop=(c == 7)), reads=[b_catT[sl], b_wo], writes=[b_pmx[sl]], sig=(c == 7 and hh == 1))
                kb.op("dve", lambda e, sl=sl: e.tensor_tensor(out=xnw[sl][:], in0=p_m[sl][:, :], in1=xt[sl][:], op=ALU.add),
                      reads=[b_pmx[sl], b_xt[sl]], writes=[b_xnw[sl]])
                kb.dma("sp", out_d[r0:r1, :], xnw[sl][:], b_xnw[sl], reads=[b_xnw[sl]])
                rms_rstd(None, xnw[sl][:], junk[:], sD[sl][:, 2:3], sD[sl][:, 3:4], D, [b_xnw[sl]], b_junk, b_sD[sl], b_sD[sl])
                kb.op("dve", lambda e, sl=sl: e.tensor_scalar(xn2[sl][:], xnw[sl][:], sD[sl][:, 3:4], None, ALU.mult),
                      reads=[b_xnw[sl], b_sD[sl]], writes=[b_xn2[sl]])
                for c in range(8):
                    kb.op("pe", lambda e, c=c, sl=sl: e.transpose(p_t[1][:, c * 128:(c + 1) * 128], xn2[sl][:, c * 128:(c + 1) * 128], ident_b[:]),
                          reads=[b_xn2[sl], b_identb], writes=[b_pt[1]], sig=(c == 7))
                for c in range(8):
                    if False:
                        kb.op("act", lambda e, c=c, sl=sl: e.activation(
                            out=h2t[sl][:, c * 128:(c + 1) * 128], in_=p_t[1][:, c * 128:(c + 1) * 128], func=AF.Identity,
                            scale=ABv[:, 4, c:c + 1], bias=ABv[:, 5, c:c + 1]), reads=[b_pt[1], b_AB], writes=[b_h2t[sl]])
                    else:
                        kb.op("dve", lambda e, c=c, sl=sl: e.tensor_scalar(
                            h2t[sl][:, c * 128:(c + 1) * 128], p_t[1][:, c * 128:(c + 1) * 128],
                            ABv[:, 4, c:c + 1], ABv[:, 5, c:c + 1], ALU.mult, ALU.add), reads=[b_pt[1], b_AB], writes=[b_h2t[sl]])
                kb.dma("sp", H2T[:, :, r0:r1].rearrange("c p t -> p c t"), h2t[sl][:].rearrange("p (c t) -> p c t", c=8),
                       b_h2t[sl], reads=[b_h2t[sl]])
                for c in range(8):
                    kb.op("pe", lambda e, c=c, sl=sl: e.matmul(p_l[:, 0:NE], h2t[sl][:, c * 128:(c + 1) * 128], rw[:, c, :],
                                                               start=(c == 0), stop=(c == 7)),
                          reads=[b_h2t[sl], b_rw], writes=[b_pl], sig=(c == 7))
                kb.op("dve", lambda e, sl=sl: e.tensor_tensor(out=lg[sl][:], in0=p_l[:, 0:NE], in1=rbb[:], op=ALU.add),
                      reads=[b_pl, b_rbb], writes=[b_lg[sl]])
                kb.op("dve", lambda e, sl=sl: e.max(out=m8[sl][:], in_=lg[sl][:]), reads=[b_lg[sl]], writes=[b_m8[sl]])
                kb.op("dve", lambda e, sl=sl: e.tensor_scalar(msk[sl][:], lg[sl][:], m8[sl][:, 3:4], None, ALU.is_ge),
                      reads=[b_lg[sl], b_m8[sl]], writes=[b_msk[sl]])
                kb.op("dve", lambda e, sl=sl: e.tensor_scalar(lg[sl][:], lg[sl][:], m8[sl][:, 0:1], None, ALU.subtract),
                      reads=[b_lg[sl], b_m8[sl]], writes=[b_lg[sl]])
                kb.op("act", lambda e, sl=sl: e.activation(out=lg[sl][:], in_=lg[sl][:], func=AF.Exp), reads=[b_lg[sl]], writes=[b_lg[sl]])
                kb.op("dve", lambda e, sl=sl: e.tensor_tensor(out=lg[sl][:], in0=lg[sl][:], in1=msk[sl][:], op=ALU.mult),
                      reads=[b_lg[sl], b_msk[sl]], writes=[b_lg[sl]])
                kb.op("dve", lambda e, sl=sl: e.tensor_reduce(out=m8[sl][:, 4:5], in_=lg[sl][:], axis=AX.X, op=ALU.add),
                      reads=[b_lg[sl]], writes=[b_m8[sl]])
                kb.op("dve", lambda e, sl=sl: e.reciprocal(out=m8[sl][:, 4:5], in_=m8[sl][:, 4:5]), reads=[b_m8[sl]], writes=[b_m8[sl]])
                kb.op("dve", lambda e, sl=sl: e.tensor_scalar(gt[sl][:], lg[sl][:], m8[sl][:, 4:5], None, ALU.mult),
                      reads=[b_lg[sl], b_m8[sl]], writes=[b_gt[sl]])
                kb.dma("sp", GATES[r0:r1, :], gt[sl][:], b_gt[sl], reads=[b_gt[sl]])
            kb.barrier()

        if LV >= 7:
          with ExitStack() as st:
            TB = 1024
            NTB = TB // 128
            gates = sb(st, "gatesE", [128, NT, NE]); b_gates = kb.buf("gatesE", dma=True)
            h2b = sb(st, "h2b", [128, 8, TB], BF16); b_h2b = kb.buf("h2b", dma=True)
            acc = sb(st, "acc", [128, NTB, D]); b_acc = [kb.buf(f"acc{i}") for i in range(NTB)]
            w1b = [sb(st, f"w1b{i}", [128, 8, 2 * D], BF16) for i in range(2)]
            b_w1b = [kb.buf(f"w1b{i}", dma=True) for i in range(2)]
            w2b = [sb(st, f"w2b{i}", [128, 8, D], BF16) for i in range(2)]
            b_w2b = [kb.buf(f"w2b{i}", dma=True) for i in range(2)]
            b1c = [sb(st, f"b1c{i}", [128, 16]) for i in range(2)]
            b_b1c = [kb.buf(f"b1c{i}", dma=True) for i in range(2)]
            b2r = [sb(st, f"b2r{i}", [1, D], BF16) for i in range(2)]
            b_b2r = [kb.buf(f"b2r{i}", dma=True) for i in range(2)]
            actT = [sb(st, f"actT{i}", [128, 8, 512], BF16) for i in range(2)]
            b_actT = [kb.buf(f"actT{i}") for i in range(2)]
            gcl = [sb(st, f"gcl{i}", [128, 512]) for i in range(2)]
            b_gcl = [kb.buf(f"gcl{i}") for i in range(2)]
            sgm = [sb(st, f"sgm{i}", [128, 512]) for i in range(2)]
            b_sgm = [kb.buf(f"sgm{i}") for i in range(2)]
            lcl = [sb(st, f"lcl{i}", [128, 512]) for i in range(2)]
            b_lcl = [kb.buf(f"lcl{i}") for i in range(2)]
            xo = [sb(st, f"xoE{i}", [128, D]) for i in range(2)]
            b_xo = [kb.buf(f"xoE{i}", dma=True) for i in range(2)]
            p_gl = [ps(st, f"p_gl{i}", [128, 512]) for i in range(4)]
            b_pgl = [kb.buf(f"pgl{i}") for i in range(4)]
            p_o2 = [ps(st, f"p_o2{i}", [128, 512]) for i in range(2)]
            b_po2 = [kb.buf(f"po2{i}") for i in range(2)]

            kb.dma("sp", gates[:], GATES.rearrange("(t p) e -> p t e", p=128), b_gates, writes=[b_gates])
            nw = 0
            nfc = 0
            no2 = 0
            nx = 0
            ew1_v = ew1_d.rearrange("e (c p) f -> e p c f", p=128)
            ew2_v = ew2_d.rearrange("e (c p) f -> e p c f", p=128)
            for tb in range(S // TB):
                t0_ = tb * TB
                kb.dma("sp", h2b[:], H2T[:, :, t0_:t0_ + TB].rearrange("c p t -> p c t"), b_h2b, writes=[b_h2b])
                for tl in range(NTB):
                    kb.op("pool", lambda e, tl=tl: e.memset(acc[:, tl, :], 0.0), writes=[b_acc[tl]])
                for ex in range(NE):
                    ws = nw % 2
                    nw += 1
                    for c in range(8):
                        kb.dma("pool", w1b[ws][:, c, :], ew1_v[ex][:, c, :], b_w1b[ws], writes=[b_w1b[ws]])
                    for c in range(0, 8, 2):
                        kb.dma("pool", w2b[ws][:, c:c + 2, :], ew2_v[ex][:, c:c + 2, :], b_w2b[ws], writes=[b_w2b[ws]])
                    kb.dma("sp", b1c[ws][:], eb1_d[ex], b_b1c[ws], writes=[b_b1c[ws]])
                    kb.dma("pool", b2r[ws][:], eb2_d[ex:ex + 1, :], b_b2r[ws], writes=[b_b2r[ws]])
                    for grp in range(TB // 512):
                        asl = (nfc // 8) % 2
                        for fc in range(8):
                            a = nfc % 2
                            nfc += 1
                            pg_, pl_ = p_gl[2 * a], p_gl[2 * a + 1]
                            bpg, bpl = b_pgl[2 * a], b_pgl[2 * a + 1]
                            for c in range(8):
                                kb.op("pe", lambda e, c=c, fc=fc, ws=ws, grp=grp, pg_=pg_: e.matmul(
                                    pg_[:, :], w1b[ws][:, c, fc * 128:(fc + 1) * 128], h2b[:, c, grp * 512:(grp + 1) * 512],
                                    start=(c == 0), stop=(c == 7)), reads=[b_w1b[ws], b_h2b], writes=[bpg], sig=(c == 7))
                            for c in range(8):
                                kb.op("pe", lambda e, c=c, fc=fc, ws=ws, grp=grp, pl_=pl_: e.matmul(
                                    pl_[:, :], w1b[ws][:, c, D + fc * 128:D + (fc + 1) * 128], h2b[:, c, grp * 512:(grp + 1) * 512],
                                    start=(c == 0), stop=(c == 7)), reads=[b_w1b[ws], b_h2b], writes=[bpl], sig=(c == 7))
                            kb.op("dve", lambda e, a=a, fc=fc, ws=ws, pg_=pg_: e.tensor_scalar(
                                gcl[a][:], pg_[:, :], b1c[ws][:, fc:fc + 1], 7.0, ALU.add, ALU.min),
                                reads=[bpg, b_b1c[ws]], writes=[b_gcl[a]])
                            kb.op("act", lambda e, a=a: e.activation(out=sgm[a][:], in_=gcl[a][:], func=AF.Sigmoid, scale=1.702),
                                  reads=[b_gcl[a]], writes=[b_sgm[a]])
                            kb.op("dve", lambda e, a=a, fc=fc, ws=ws, pl_=pl_: e.tensor_scalar(
                                lcl[a][:], pl_[:, :], b1c[ws][:, 8 + fc:9 + fc], 7.0, ALU.add, ALU.min),
                                reads=[bpl, b_b1c[ws]], writes=[b_lcl[a]])
                            kb.op("pool", lambda e, a=a: e.tensor_scalar(lcl[a][:], lcl[a][:], -7.0, 1.0, ALU.max, ALU.add),
                                  reads=[b_lcl[a]], writes=[b_lcl[a]])
                            kb.op("pool", lambda e, a=a: e.tensor_tensor(out=gcl[a][:], in0=gcl[a][:], in1=sgm[a][:], op=ALU.mult),
                                  reads=[b_gcl[a], b_sgm[a]], writes=[b_gcl[a]])
                            kb.op("dve", lambda e, a=a, fc=fc, asl=asl: e.tensor_tensor(out=actT[asl][:, fc, :], in0=gcl[a][:], in1=lcl[a][:], op=ALU.mult),
                                  reads=[b_gcl[a], b_lcl[a]], writes=[b_actT[asl]])
                        for tq in range(4):
                            tl = grp * 4 + tq
                            tg = tb * NTB + tl
                            for dh in range(2):
                                o = no2 % 2
                                no2 += 1
                                for fc in range(8):
                                    kb.op("pe", lambda e, fc=fc, tq=tq, dh=dh, o=o, asl=asl, ws=ws: e.matmul(
                                        p_o2[o][:, :], actT[asl][:, fc, tq * 128:(tq + 1) * 128], w2b[ws][:, fc, dh * 512:(dh + 1) * 512],
                                        start=(fc == 0), stop=False), reads=[b_actT[asl], b_w2b[ws]], writes=[b_po2[o]], sig=False)
                                kb.op("pe", lambda e, dh=dh, o=o, ws=ws: e.matmul(
                                    p_o2[o][:, :], ones_b[0:1, :], b2r[ws][0:1, dh * 512:(dh + 1) * 512], start=False, stop=True),
                                    reads=[b_onesb, b_b2r[ws]], writes=[b_po2[o]])
                                kb.op("dve", lambda e, o=o, tl=tl, tg=tg, dh=dh, ex=ex: e.scalar_tensor_tensor(
                                    out=acc[:, tl, dh * 512:(dh + 1) * 512], in0=p_o2[o][:, :], scalar=gates[:, tg, ex:ex + 1],
                                    in1=acc[:, tl, dh * 512:(dh + 1) * 512], op0=ALU.mult, op1=ALU.add),
                                    reads=[b_po2[o], b_gates, b_acc[tl]], writes=[b_acc[tl]])
                for tl in range(NTB):
                    tg = tb * NTB + tl
                    z = nx % 2
                    nx += 1
                    kb.dma("sp", xo[z][:], out_d[tg * 128:(tg + 1) * 128, :], b_xo[z], writes=[b_xo[z]])
                    kb.op("dve", lambda e, tl=tl: e.tensor_tensor(out=acc[:, tl, :], in0=acc[:, tl, :], in1=G2row[:], op=ALU.mult),
                          reads=[b_acc[tl], b_G2], writes=[b_acc[tl]])
                    kb.op("dve", lambda e, tl=tl, z=z: e.tensor_tensor(out=xo[z][:], in0=acc[:, tl, :], in1=xo[z][:], op=ALU.add),
                          reads=[b_acc[tl], b_xo[z]], writes=[b_xo[z]])
                    kb.dma("sp", out_d[tg * 128:(tg + 1) * 128, :], xo[z][:], b_xo[z], reads=[b_xo[z]])
            kb.barrier()
        kb.barrier()
    return nc


_CONST = {}


def _constants():
    if _CONST:
        return _CONST
    f32 = np.float32
    pos = np.arange(S)
    row = (pos // 64).astype(f32)
    col = (pos % 64).astype(f32)
    inv = (f32(10000.0) ** (-np.arange(16, dtype=f32) / f32(16))).astype(f32)
    ar = (row[:, None] * inv[None]).astype(f32)
    ac = (col[:, None] * inv[None]).astype(f32)
    cr, sr, cc, sc = np.cos(ar), np.sin(ar), np.cos(ac), np.sin(ac)
    C = np.concatenate([cr, cr, cc, cc], axis=1)
    SA = np.concatenate([-sr, -sc], axis=1)
    SB = np.concatenate([sr, sc], axis=1)
    C = np.concatenate([np.ones((LC, 64), f32), C], 0)
    SA = np.concatenate([np.zeros((LC, 32), f32), SA], 0)
    SB = np.concatenate([np.zeros((LC, 32), f32), SB], 0)

    def pm(a):
        n = a.shape[0] // 128
        return np.ascontiguousarray(a.reshape(n, 128, -1).transpose(1, 0, 2).reshape(128, -1)).astype(f32)

    _CONST["rope_c"], _CONST["rope_sa"], _CONST["rope_sb"] = pm(C), pm(SA), pm(SB)
    posf = np.arange(S, dtype=f32)
    tn = posf / f32(S)
    bands = np.linspace(1e-4, 7, 8, dtype=f32)
    ang = (f32(2.0 * math.pi / S) * posf[:, None] * bands[None]).astype(f32)
    feats = np.concatenate([tn[:, None], np.sin(ang), np.cos(ang)], axis=-1).astype(f32)
    _CONST["featsT"] = np.ascontiguousarray(feats.T)
    deltas = np.abs(np.linspace(math.log(1e-2) / 1.5, math.log(1e-2) / 0.3, 512, dtype=f32))
    _CONST["window"] = np.exp(-tn[:, None] * deltas[None]).astype(f32)
    _CONST["ident"] = np.eye(128, dtype=f32)
    k = np.arange(S, dtype=np.float64)
    t = np.arange(S, dtype=np.float64)
    th = np.pi * (2 * k[:, None] + 1) * t[None, :] / NFFT
    bf = ml_dtypes.bfloat16
    for nm, fn in (("c", np.cos), ("s", np.sin)):
        M = fn(th).astype(f32)
        a = M.reshape(NT, 128, NT, 128).transpose(0, 3, 2, 1)
        _CONST[f"dft_{nm}tk"] = np.ascontiguousarray(a).reshape(NT, 128, S).astype(bf)
        a = M.reshape(NT, 128, NT, 128).transpose(2, 1, 0, 3)
        _CONST[f"dft_{nm}kt"] = np.ascontiguousarray(a).reshape(NT, 128, S).astype(bf)
    return _CONST


def _col(v, n):
    return np.ascontiguousarray(np.asarray(v, np.float32).reshape(n, 128).T)


_PROG = {}


def make_in_maps(inp):
    cst = _constants()
    f32 = np.float32
    g = lambda k: np.asarray(inp[k], f32)
    shared = dict(cst)
    shared["w_mod"] = g("w_mod")[0]
    bm = g("b_mod")[0]
    shared["bmod_col"] = np.ascontiguousarray(bm.reshape(6, 8, 128).transpose(2, 0, 1).reshape(128, 48))
    shared["bmod_row"] = bm.reshape(1, -1)
    shared["n1g_col"] = _col(g("norm1_g")[0], 8)
    shared["n2g_col"] = _col(g("norm2_g")[0], 8)
    shared["w_in"] = g("w_in")[0]
    shared["qkg_row"] = np.concatenate([np.tile(g("q_norm_g")[0], 8), np.tile(g("k_norm_g")[0], 8)]).reshape(1, 1024)
    shared["lam_row"] = np.concatenate([g("lam_q1")[0], g("lam_k1")[0], g("lam_q2")[0], g("lam_k2")[0]]).reshape(1, 256)
    shared["subg_row"] = g("subln_g")[0].reshape(1, 128)
    shared["convw"] = g("hy_conv_w")[0].reshape(1, 4608)
    shared["convb"] = g("hy_conv_b")[0].reshape(1, 1536)
    shared["hy_w1"] = g("hy_w1")[0]
    shared["hy_cols"] = np.ascontiguousarray(np.stack([g("hy_b1")[0], g("hy_f1")[0], g("hy_b2")[0], g("hy_f2")[0]], axis=1))
    shared["hy_w2"] = g("hy_w2")[0]
    shared["hy_w3"] = g("hy_w3")[0]
    shared["hy_skip"] = g("hy_skip")[0].reshape(1, 1024)
    shared["hyg_row"] = g("hy_out_g")[0].reshape(1, 512)
    shared["w_out"] = g("w_out")[0]
    shared["router_w"] = g("router_w")[0]
    shared["router_b"] = g("router_b")[0].reshape(1, NE)
    shared["exp_w1"] = g("exp_w1")[0]
    shared["eb1_col"] = np.ascontiguousarray(g("exp_b1")[0].reshape(NE, 16, 128).transpose(0, 2, 1))
    shared["exp_w2"] = g("exp_w2")[0]
    shared["exp_b2"] = g("exp_b2")[0]
    x = g("x")
    ctx = g("ctx")
    c = g("c")
    cc = g("c_ctx")
    maps = []
    for b in range(8):
        m = dict(shared)
        m["x"] = x[b]
        m["ctx"] = ctx[b]
        cs = np.stack([_col(c[b], 8), _col(cc, 8)], axis=2).reshape(128, 16)
        m["csil"] = np.ascontiguousarray(cs)
        maps.append(m)
    return maps


def kernel(**inputs):
    if "nc" not in _PROG:
        _PROG["nc"] = build_program()
    nc = _PROG["nc"]
    maps = make_in_maps(inputs)
    res = run_bass_kernel_spmd(nc, maps, core_ids=list(range(8)))
    return np.stack([np.asarray(r["out"], np.float32) for r in res.results], axis=0)
```

```python
import math
import os
from contextlib import ExitStack
KDBG = os.environ.get('KDBG', '')

import numpy as np
import ml_dtypes
import concourse.bass as bass
import concourse.mybir as mybir
from concourse.bass_utils import run_bass_kernel_spmd

F32 = mybir.dt.float32
BF16 = mybir.dt.bfloat16
AF = mybir.ActivationFunctionType
ALU = mybir.AluOpType
AX = mybir.AxisListType

D = 1024
S = 4096
LC = 256
NKT = 34
NT = 32
EPS = 1e-6
NE = 32
LAM_INIT = 0.8 - 0.6 * math.exp(0.0)
NFFT = 8192


class Buf:
    __slots__ = ("w", "r", "sem", "dcnt", "name")

    def __init__(self, name):
        self.name = name
        self.w = None
        self.r = {}
        self.sem = None
        self.dcnt = 0


class KB:
    def __init__(self, nc, es):
        self.nc = nc
        self.es = es
        self.eng = {"pe": nc.tensor, "act": nc.scalar, "dve": nc.vector, "pool": nc.gpsimd, "sp": nc.sync}
        self.sem = {n: es.enter_context(nc.semaphore("sem_" + n)) for n in self.eng}
        self.cnt = {n: 0 for n in self.eng}
        self.waited = {n: {} for n in self.eng}
        self.dma_bufs = []

    def buf(self, name, dma=False):
        b = Buf(name)
        if dma:
            b.sem = self.es.enter_context(self.nc.semaphore("d_" + name))
            self.dma_bufs.append(b)
        return b

    def _wait(self, e, ev):
        if ev is None:
            return
        key, sem, val, src = ev
        if src == "pe" and e == "pe":
            return
        if self.waited[e].get(key, 0) >= val:
            return
        self.eng[e].wait_ge(sem, val)
        self.waited[e][key] = val

    def _deps(self, e, reads, writes):
        for b in reads:
            self._wait(e, b.w)
        for b in writes:
            self._wait(e, b.w)
            for ev in list(b.r.values()):
                self._wait(e, ev)

    def op(self, e, fn, reads=(), writes=(), sig=True):
        self._deps(e, reads, writes)
        ins = fn(self.eng[e])
        if sig:
            self.cnt[e] += 1
            ins.then_inc(self.sem[e], 1)
            val = self.cnt[e]
        else:
            val = self.cnt[e] + 1
        ev = (e, self.sem[e], val, e)
        for b in reads:
            b.r[e] = ev
        for b in writes:
            b.w = ev
            b.r = {}
        return ev

    def dma(self, e, out, in_, sb, reads=(), writes=()):
        self._deps(e, reads, writes)
        ins = self.eng[e].dma_start(out=out, in_=in_)
        sb.dcnt += 16
        ins.then_inc(sb.sem, 16)
        ev = (id(sb), sb.sem, sb.dcnt, "dma")
        for b in reads:
            b.r[id(sb)] = ev
        for b in writes:
            b.w = ev
            b.r = {}
        return ev

    def barrier(self):
        evs = [(n, self.sem[n], self.cnt[n], "x") for n in self.eng if self.cnt[n] > 0]
        evs += [(id(b), b.sem, b.dcnt, "dma") for b in self.dma_bufs if b.dcnt > 0]
        for e in self.eng:
            for ev in evs:
                if ev[0] == e:
                    continue
                self._wait(e, ev)


def build_program(stop_after="E", debug=False):
    nc = bass.Bass("TRN2", target_bir_lowering=False)
    LV = {"0": 0, "Ah": 1, "Aq": 2, "A": 3, "B": 4, "C": 5, "D": 6, "E": 7}[stop_after]

    def din(name, shape, dt=F32):
        return nc.dram_tensor(name, list(shape), dt, kind="ExternalInput").ap()

    def dscr(name, shape, dt=BF16):
        return nc.dram_tensor(name, list(shape), dt, kind=("ExternalOutput" if debug else "Internal")).ap()

    x_d = din("x", [S, D])
    ctx_d = din("ctx", [LC, D])
    csil_d = din("csil", [128, 16])
    wmod_d = din("w_mod", [D, 6 * D])
    bmodc_d = din("bmod_col", [128, 48])
    bmodr_d = din("bmod_row", [1, 6 * D])
    n1g_d = din("n1g_col", [128, 8])
    n2g_d = din("n2g_col", [128, 8])
    win_d = din("w_in", [D, 3072])
    qkg_d = din("qkg_row", [1, 1024])
    lamv_d = din("lam_row", [1, 256])
    subg_d = din("subg_row", [1, 128])
    cw_d = din("convw", [1, 4608])
    cb_d = din("convb", [1, 1536])
    hw1_d = din("hy_w1", [17, 64])
    hcol_d = din("hy_cols", [64, 4])
    hw2_d = din("hy_w2", [64, 64])
    hw3_d = din("hy_w3", [64, 2048])
    hskip_d = din("hy_skip", [1, 1024])
    hyg_d = din("hyg_row", [1, 512])
    wout_d = din("w_out", [D, D])
    rw_d = din("router_w", [D, NE])
    rb_d = din("router_b", [1, NE])
    ew1_d = din("exp_w1", [NE, D, 2 * D])
    eb1_d = din("eb1_col", [NE, 128, 16])
    ew2_d = din("exp_w2", [NE, D, D])
    eb2_d = din("exp_b2", [NE, D])
    ident_d = din("ident", [128, 128])
    ropec_d = din("rope_c", [128, NKT * 64])
    ropea_d = din("rope_sa", [128, NKT * 32])
    ropeb_d = din("rope_sb", [128, NKT * 32])
    featsT_d = din("featsT", [17, S])
    window_d = din("window", [S, 512])
    ctk_d = din("dft_ctk", [NT, 128, 4096], BF16)
    stk_d = din("dft_stk", [NT, 128, 4096], BF16)
    ckt_d = din("dft_ckt", [NT, 128, 4096], BF16)
    skt_d = din("dft_skt", [NT, 128, 4096], BF16)

    out_d = nc.dram_tensor("out", [S, D], F32, kind="ExternalOutput").ap()

    QT = dscr("QT", [4, 128, S])
    KT = dscr("KT", [4, 128, NKT * 128])
    VV = dscr("VV", [NKT * 128, 512])
    UU = dscr("UU", [S, 1536])
    AT = dscr("AT", [S, 512])
    Z2 = dscr("Z2", [S, 512])
    H2T = dscr("H2T", [8, 128, S])
    GATES = dscr("GATES", [S, NE], F32)

    with ExitStack() as es:
        kb = KB(nc, es)

        def sb(st, name, shape, dt=F32):
            return st.enter_context(nc.sbuf_tensor("sb_" + name, list(shape), dt))

        def ps(st, name, shape, dt=F32):
            return st.enter_context(nc.psum_tensor("ps_" + name, list(shape), dt))

        ident_f = sb(es, "ident_f", [128, 128]); b_identf = kb.buf("identf", dma=True)
        ident_b = sb(es, "ident_b", [128, 128], BF16); b_identb = kb.buf("identb", dma=True)
        ones_f = sb(es, "ones_f", [128, 128]); b_onesf = kb.buf("onesf")
        ones_b = sb(es, "ones_b", [128, 128], BF16); b_onesb = kb.buf("onesb")
        modcol = sb(es, "modcol", [128, 96]); b_modcol = kb.buf("modcol")
        AB = sb(es, "AB", [128, 64]); b_AB = kb.buf("AB")
        G1row = sb(es, "G1row", [128, D]); b_G1 = kb.buf("G1row")
        G2row = sb(es, "G2row", [128, D]); b_G2 = kb.buf("G2row")
        nlam = sb(es, "nlam", [128, 1]); b_nlam = kb.buf("nlam")

        kb.dma("sp", ident_f[:], ident_d, b_identf, writes=[b_identf])
        kb.dma("pool", ident_b[:], ident_d, b_identb, writes=[b_identb])
        kb.op("dve", lambda e: e.memset(ones_f[:], 1.0), writes=[b_onesf])
        kb.op("dve", lambda e: e.memset(ones_b[:], 1.0), writes=[b_onesb])

        with ExitStack() as st:
            csil = sb(st, "csil", [128, 16]); b_csil = kb.buf("csil", dma=True)
            scb = sb(st, "scb", [128, 16], BF16); b_scb = kb.buf("scb")
            bmodc = sb(st, "bmodc", [128, 48]); b_bmodc = kb.buf("bmodc", dma=True)
            bmodr = sb(st, "bmodr", [1, 6 * D]); b_bmodr = kb.buf("bmodr", dma=True)
            n1g = sb(st, "n1g", [128, 8]); b_n1g = kb.buf("n1g", dma=True)
            n2g = sb(st, "n2g", [128, 8]); b_n2g = kb.buf("n2g", dma=True)
            lamv = sb(st, "lamv", [128, 256]); b_lamv = kb.buf("lamv", dma=True)
            lamt = sb(st, "lamt", [128, 128]); b_lamt = kb.buf("lamt")
            lams = sb(st, "lams", [128, 2]); b_lams = kb.buf("lams")
            grow = sb(st, "grow", [1, 2 * D]); b_grow = kb.buf("grow")
            tmp8 = sb(st, "tmp8", [128, 16]); b_tmp8 = kb.buf("tmp8")
            wm = [sb(st, f"wm{i}", [128, 8, D], BF16) for i in range(2)]
            b_wm = [kb.buf(f"wm{i}", dma=True) for i in range(2)]
            p_col = ps(st, "p_col", [128, 512]); b_pcol = kb.buf("pcol")
            p_row = ps(st, "p_row", [128, 512]); b_prow = kb.buf("prow")
            p_bc = [ps(st, f"p_bc{i}", [128, 512]) for i in range(2)]
            b_pbc = [kb.buf(f"pbc{i}") for i in range(2)]

            kb.dma("sp", csil[:], csil_d, b_csil, writes=[b_csil])
            kb.dma("sp", bmodc[:], bmodc_d, b_bmodc, writes=[b_bmodc])
            kb.dma("sp", bmodr[:], bmodr_d, b_bmodr, writes=[b_bmodr])
            kb.dma("sp", n1g[:], n1g_d, b_n1g, writes=[b_n1g])
            kb.dma("sp", n2g[:], n2g_d, b_n2g, writes=[b_n2g])
            kb.dma("sp", lamv[:], lamv_d.broadcast_to([128, 256]), b_lamv, writes=[b_lamv])
            kb.op("act", lambda e: e.activation(out=scb[:], in_=csil[:], func=AF.Silu), reads=[b_csil], writes=[b_scb])
            scv = scb[:].rearrange("p (c w) -> p c w", w=2)
            wmod_v = wmod_d.rearrange("(c p) f -> p c f", p=128)
            for i in range(6):
                sl = i % 2
                kb.dma("pool", wm[sl][:], wmod_v[:, :, i * D:(i + 1) * D], b_wm[sl], writes=[b_wm[sl]])
                if i in (0, 1, 3, 4):
                    for fc in range(8):
                        col = (i * 8 + fc) * 2
                        for c in range(8):
                            kb.op("pe", lambda e, c=c, fc=fc, col=col: e.matmul(
                                p_col[:, col:col + 2], wm[sl][:, c, fc * 128:(fc + 1) * 128], scv[:, c, :],
                                start=(c == 0), stop=(c == 7)),
                                reads=[b_wm[sl], b_scb], writes=[b_pcol], sig=(c == 7))
                else:
                    gi = 0 if i == 2 else 1
                    for hh in range(2):
                        for c in range(8):
                            kb.op("pe", lambda e, c=c, hh=hh: e.matmul(
                                p_row[0:1, :], scv[:, c, 0:1], wm[sl][:, c, hh * 512:(hh + 1) * 512],
                                start=(c == 0), stop=(c == 7)),
                                reads=[b_wm[sl], b_scb], writes=[b_prow], sig=(c == 7))
                        kb.op("dve", lambda e, hh=hh, gi=gi, i=i: e.tensor_tensor(
                            out=grow[0:1, gi * D + hh * 512: gi * D + (hh + 1) * 512], in0=p_row[0:1, :],
                            in1=bmodr[0:1, i * D + hh * 512: i * D + (hh + 1) * 512], op=ALU.add),
                            reads=[b_prow, b_bmodr], writes=[b_grow])
            kb.op("dve", lambda e: e.memset(modcol[:, :], 0.0), writes=[b_modcol])
            for lo in (0, 24):
                kb.op("dve", lambda e, lo=lo: e.tensor_tensor(
                    out=modcol[:, 2 * lo:2 * lo + 32].rearrange("p (a w) -> p a w", w=2),
                    in0=p_col[:, 2 * lo:2 * lo + 32].rearrange("p (a w) -> p a w", w=2),
                    in1=bmodc[:, lo:lo + 16].unsqueeze(2).to_broadcast([128, 16, 2]), op=ALU.add),
                    reads=[b_pcol, b_bmodc], writes=[b_modcol])
            mc = modcol[:, :].rearrange("p (i c w) -> p i c w", i=6, c=8)
            ABv = AB[:, :].rearrange("p (j c) -> p j c", c=8)
            for j, (i, w, g) in enumerate([(1, 0, n1g), (1, 1, n1g), (None, None, None), (None, None, None), (4, 0, n2g)]):
                if i is None:
                    continue
                kb.op("dve", lambda e, i=i, w=w: e.tensor_scalar(tmp8[:, 0:8], mc[:, i, :, w], 1.0, None, ALU.add),
                      reads=[b_modcol], writes=[b_tmp8])
                kb.op("dve", lambda e, j=j, g=g: e.tensor_tensor(out=ABv[:, j, :], in0=tmp8[:, 0:8], in1=g[:, :], op=ALU.mult),
                      reads=[b_tmp8, b_n1g, b_n2g], writes=[b_AB])
            kb.op("dve", lambda e: e.tensor_copy(out=ABv[:, 2, :], in_=mc[:, 0, :, 0]), reads=[b_modcol], writes=[b_AB])
            kb.op("dve", lambda e: e.tensor_copy(out=ABv[:, 3, :], in_=mc[:, 0, :, 1]), reads=[b_modcol], writes=[b_AB])
            kb.op("dve", lambda e: e.tensor_copy(out=ABv[:, 5, :], in_=mc[:, 3, :, 0]), reads=[b_modcol], writes=[b_AB])
            for gi, (G, bG) in enumerate(((G1row, b_G1), (G2row, b_G2))):
                for hh in range(2):
                    k = hh
                    kb.op("pe", lambda e, gi=gi, hh=hh, k=k: e.matmul(
                        p_bc[k][:, :], ones_f[0:1, :], grow[0:1, gi * D + hh * 512: gi * D + (hh + 1) * 512],
                        start=True, stop=True), reads=[b_onesf, b_grow], writes=[b_pbc[k]])
                    kb.op("act", lambda e, G=G, hh=hh, k=k: e.activation(
                        out=G[:, hh * 512:(hh + 1) * 512], in_=p_bc[k][:, :], func=AF.Copy),
                        reads=[b_pbc[k]], writes=[bG])
            for j in range(2):
                kb.op("dve", lambda e, j=j: e.tensor_tensor(
                    out=lamt[:, j * 64:(j + 1) * 64], in0=lamv[:, j * 128: j * 128 + 64],
                    in1=lamv[:, j * 128 + 64: j * 128 + 128], op=ALU.mult), reads=[b_lamv], writes=[b_lamt])
            kb.op("dve", lambda e: e.tensor_reduce(out=lams[:, :], in_=lamt[:, :].rearrange("p (j d) -> p j d", d=64),
                                                   axis=AX.X, op=ALU.add), reads=[b_lamt], writes=[b_lams])
            kb.op("act", lambda e: e.activation(out=lams[:, :], in_=lams[:, :], func=AF.Exp), reads=[b_lams], writes=[b_lams])
            kb.op("dve", lambda e: e.tensor_tensor(out=nlam[:, :], in0=lams[:, 1:2], in1=lams[:, 0:1], op=ALU.subtract),
                  reads=[b_lams], writes=[b_nlam])
            kb.op("dve", lambda e: e.tensor_scalar(nlam[:, :], nlam[:, :], -LAM_INIT, None, ALU.add),
                  reads=[b_nlam], writes=[b_nlam])
            kb.barrier()

        ABv = AB[:, :].rearrange("p (j c) -> p j c", c=8)

        def rms_rstd(e_act_reads, src_ap, junk_ap, ssq_ap, rstd_ap, n, bufs_r, b_junk, b_ssq, b_rstd):
            kb.op("dve", lambda e: e.memset(ssq_ap, 0.0), writes=[b_ssq])
            kb.op("act", lambda e: e.activation(out=junk_ap, in_=src_ap, func=AF.Square, accum_out=ssq_ap),
                  reads=bufs_r, writes=[b_junk, b_ssq])
            kb.op("act", lambda e: e.activation(out=rstd_ap, in_=ssq_ap, func=AF.Sqrt, bias=EPS, scale=1.0 / n),
                  reads=[b_ssq], writes=[b_rstd])
            kb.op("dve", lambda e: e.reciprocal(out=rstd_ap, in_=rstd_ap), reads=[b_rstd], writes=[b_rstd])

        if LV >= 1:
          with ExitStack() as st:
            HW = NKT * 128 + 2
            hT = sb(st, "hT", [128, 8, HW], BF16)
            b_hTa = [kb.buf(f"hTa{i}") for i in range(NKT)]
            b_hTb = [kb.buf(f"hTb{i}") for i in range(NKT)]
            b_hpad = kb.buf("hpad")
            XOFF = LC + 1

            def hcol(i):
                return i * 128 if i < 2 else XOFF + (i - 2) * 128

            kb.op("pool", lambda e: e.memset(hT[:, :, LC:LC + 1], 0.0), writes=[b_hpad])
            kb.op("pool", lambda e: e.memset(hT[:, :, HW - 1:HW], 0.0), writes=[b_hpad])
            with ExitStack() as s2:
                xt = [sb(s2, f"xt{i}", [128, D]) for i in range(3)]
                b_xt = [kb.buf(f"xt{i}", dma=True) for i in range(3)]
                junk = sb(s2, "junkA", [128, D], BF16); b_junk = kb.buf("junkA")
                ssq = [sb(s2, f"ssqA{i}", [128, 2]) for i in range(3)]
                b_ssq = [kb.buf(f"ssqA{i}") for i in range(3)]
                xn = [sb(s2, f"xnA{i}", [128, D], BF16) for i in range(2)]
                b_xn = [kb.buf(f"xnA{i}") for i in range(2)]
                pT = [ps(s2, f"pTA{i}", [128, D], BF16) for i in range(2)]
                b_pT = [kb.buf(f"pTA{i}") for i in range(2)]
                srcs = [ctx_d[0:128, :], ctx_d[128:256, :]] + [x_d[i * 128:(i + 1) * 128, :] for i in range(NT)]
                for i in range(NKT if 't1' not in KDBG else 1):
                    s3 = i % 3
                    s2_ = i % 2
                    kb.dma("sp", xt[s3][:], srcs[i], b_xt[s3], writes=[b_xt[s3]])
                    rms_rstd(None, xt[s3][:], junk[:], ssq[s3][:, 0:1], ssq[s3][:, 1:2], D, [b_xt[s3]], b_junk, b_ssq[s3], b_ssq[s3])
                    kb.op("dve", lambda e, s3=s3, s2_=s2_: e.tensor_scalar(xn[s2_][:], xt[s3][:], ssq[s3][:, 1:2], None, ALU.mult),
                          reads=[b_xt[s3], b_ssq[s3]], writes=[b_xn[s2_]])
                    if 'notr' in KDBG:
                        continue
                    for c in range(8):
                        kb.op("pe", lambda e, c=c, s2_=s2_: e.transpose(pT[s2_][:, c * 128:(c + 1) * 128],
                                                                          xn[s2_][:, c * 128:(c + 1) * 128], ident_b[:]),
                              reads=[b_xn[s2_], b_identb], writes=[b_pT[s2_]], sig=(c == 7))
                    ja, jb = (1, 3) if i < 2 else (0, 2)
                    c0 = hcol(i)
                    if 'noevac' in KDBG:
                        continue
                    for c in range(8):
                        if False:
                            kb.op("act", lambda e, c=c, s2_=s2_, ja=ja, jb=jb, c0=c0: e.activation(
                                out=hT[:, c, c0:c0 + 128], in_=pT[s2_][:, c * 128:(c + 1) * 128], func=AF.Identity,
                                scale=ABv[:, ja, c:c + 1], bias=ABv[:, jb, c:c + 1]),
                                reads=[b_pT[s2_], b_AB], writes=[b_hTa[i]])
                        else:
                            kb.op("dve", lambda e, c=c, s2_=s2_, ja=ja, jb=jb, c0=c0: e.tensor_scalar(
                                hT[:, c, c0:c0 + 128], pT[s2_][:, c * 128:(c + 1) * 128],
                                ABv[:, ja, c:c + 1], ABv[:, jb, c:c + 1], ALU.mult, ALU.add),
                                reads=[b_pT[s2_], b_AB], writes=[b_hTb[i]])
                kb.barrier()

            if LV >= 2:
             with ExitStack() as s2:
                 wq = sb(s2, "wq", [128, 8, 1536], BF16); b_wq = kb.buf("wq", dma=True)
                 qkg = sb(s2, "qkg", [128, 1024]); b_qkg = kb.buf("qkg", dma=True)
                 ropec = sb(s2, "ropec", [128, NKT * 64]); b_ropec = kb.buf("ropec", dma=True)
                 ropea = sb(s2, "ropea", [128, NKT * 32]); b_ropea = kb.buf("ropea", dma=True)
                 ropeb = sb(s2, "ropeb", [128, NKT * 32]); b_ropeb = kb.buf("ropeb", dma=True)
                 sq = sb(s2, "sqB", [128, 1024]); b_sq = kb.buf("sqB")
                 s16 = [sb(s2, f"s16_{i}", [128, 16]) for i in range(2)]
                 b_s16 = [kb.buf(f"s16_{i}") for i in range(2)]
                 t0 = sb(s2, "t0B", [128, 1024]); b_t0 = kb.buf("t0B")
                 t1 = sb(s2, "t1B", [128, 1024]); b_t1 = kb.buf("t1B")
                 t2 = sb(s2, "t2B", [128, 1024]); b_t2 = kb.buf("t2B")
                 qkb = [sb(s2, f"qkb{i}", [128, 1024], BF16) for i in range(2)]
                 b_qkb = [kb.buf(f"qkb{i}") for i in range(2)]
                 qkT = [sb(s2, f"qkT{i}", [128, 1024], BF16) for i in range(2)]
                 b_qkT = [kb.buf(f"qkT{i}", dma=True) for i in range(2)]
                 vt = [sb(s2, f"vt{i}", [128, 512], BF16) for i in range(2)]
                 b_vt = [kb.buf(f"vt{i}", dma=True) for i in range(2)]
                 p_qk = [ps(s2, f"p_qk{i}", [128, 1024]) for i in range(2)]
                 b_pqk = [kb.buf(f"pqk{i}") for i in range(2)]
                 p_v = [ps(s2, f"p_v{i}", [128, 512]) for i in range(2)]
                 b_pv = [kb.buf(f"pv{i}") for i in range(2)]
                 p_t = ps(s2, "p_tB", [128, 1024], BF16); b_pt = kb.buf("ptB")

                 win_v = win_d.rearrange("(c p) f -> p c f", p=128)
                 for c in range(8):
                     kb.dma("pool", wq[:, c, :], win_v[:, c, 0:1536], b_wq, writes=[b_wq])
                 kb.dma("sp", qkg[:], qkg_d.broadcast_to([128, 1024]), b_qkg, writes=[b_qkg])
                 kb.dma("sp", ropec[:], ropec_d, b_ropec, writes=[b_ropec])
                 kb.dma("sp", ropea[:], ropea_d, b_ropea, writes=[b_ropea])
                 kb.dma("sp", ropeb[:], ropeb_d, b_ropeb, writes=[b_ropeb])

                 for i in range(NKT):
                     sl = i % 2
                     c0 = hcol(i)
                     for half in range(2):
                         for c in range(8):
                             kb.op("pe", lambda e, c=c, half=half, sl=sl, c0=c0: e.matmul(
                                 p_qk[sl][:, half * 512:(half + 1) * 512], hT[:, c, c0:c0 + 128],
                                 wq[:, c, half * 512:(half + 1) * 512], start=(c == 0), stop=(c == 7)),
                                 reads=[b_hTa[i], b_hTb[i], b_wq], writes=[b_pqk[sl]], sig=(c == 7 and half == 1))
                     for c in range(8):
                         kb.op("pe", lambda e, c=c, sl=sl, c0=c0: e.matmul(
                             p_v[sl][:, :], hT[:, c, c0:c0 + 128], wq[:, c, 1024:1536], start=(c == 0), stop=(c == 7)),
                             reads=[b_hTa[i], b_hTb[i], b_wq], writes=[b_pv[sl]], sig=(c == 7))
                     kb.op("act", lambda e, sl=sl: e.activation(out=vt[sl][:], in_=p_v[sl][:, :], func=AF.Copy),
                           reads=[b_pv[sl]], writes=[b_vt[sl]])
                     kb.dma("sp", VV[i * 128:(i + 1) * 128, :], vt[sl][:], b_vt[sl], reads=[b_vt[sl]])
                     kb.op("act", lambda e, sl=sl: e.activation(out=sq[:], in_=p_qk[sl][:, :], func=AF.Square),
                           reads=[b_pqk[sl]], writes=[b_sq])
                     kb.op("dve", lambda e, sl=sl: e.tensor_reduce(out=s16[sl][:], in_=sq[:].rearrange("p (g d) -> p g d", d=64),
                                                                   axis=AX.X, op=ALU.add), reads=[b_sq], writes=[b_s16[sl]])
                     kb.op("act", lambda e, sl=sl: e.activation(out=s16[sl][:], in_=s16[sl][:], func=AF.Sqrt, bias=EPS, scale=1.0 / 64),
                           reads=[b_s16[sl]], writes=[b_s16[sl]])
                     kb.op("dve", lambda e, sl=sl: e.reciprocal(out=s16[sl][:], in_=s16[sl][:]), reads=[b_s16[sl]], writes=[b_s16[sl]])
                     kb.op("dve", lambda e, sl=sl: e.tensor_tensor(
                         out=t0[:].rearrange("p (g d) -> p g d", d=64), in0=p_qk[sl][:, :].rearrange("p (g d) -> p g d", d=64),
                         in1=s16[sl][:].unsqueeze(2).to_broadcast([128, 16, 64]), op=ALU.mult),
                         reads=[b_pqk[sl], b_s16[sl]], writes=[b_t0])
                     kb.op("pool", lambda e: e.tensor_tensor(out=t0[:], in0=t0[:], in1=qkg[:], op=ALU.mult),
                           reads=[b_t0, b_qkg], writes=[b_t0])
                     kb.op("dve", lambda e, i=i: e.tensor_tensor(
                         out=t1[:].rearrange("p (g d) -> p g d", d=64), in0=t0[:].rearrange("p (g d) -> p g d", d=64),
                         in1=ropec[:, i * 64:(i + 1) * 64].unsqueeze(1).to_broadcast([128, 16, 64]), op=ALU.mult),
                         reads=[b_t0, b_ropec], writes=[b_t1])
                     t0v = t0[:].rearrange("p (g r h d) -> p g r h d", g=16, r=2, h=2)
                     t2v = t2[:].rearrange("p (g r h d) -> p g r h d", g=16, r=2, h=2)
                     kb.op("pool", lambda e, i=i: e.tensor_tensor(
                         out=t2v[:, :, :, 0, :], in0=t0v[:, :, :, 1, :],
                         in1=ropea[:, i * 32:(i + 1) * 32].rearrange("p (r d) -> p r d", r=2).unsqueeze(1).to_broadcast([128, 16, 2, 16]),
                         op=ALU.mult), reads=[b_t0, b_ropea], writes=[b_t2])
                     kb.op("pool", lambda e, i=i: e.tensor_tensor(
                         out=t2v[:, :, :, 1, :], in0=t0v[:, :, :, 0, :],
                         in1=ropeb[:, i * 32:(i + 1) * 32].rearrange("p (r d) -> p r d", r=2).unsqueeze(1).to_broadcast([128, 16, 2, 16]),
                         op=ALU.mult), reads=[b_t0, b_ropeb], writes=[b_t2])
                     kb.op("dve", lambda e, sl=sl: e.tensor_tensor(out=qkb[sl][:], in0=t1[:], in1=t2[:], op=ALU.add),
                           reads=[b_t1, b_t2], writes=[b_qkb[sl]])
                     for c in range(8):
                         kb.op("pe", lambda e, c=c, sl=sl: e.transpose(p_t[:, c * 128:(c + 1) * 128],
                                                                        qkb[sl][:, c * 128:(c + 1) * 128], ident_b[:]),
                               reads=[b_qkb[sl], b_identb], writes=[b_pt], sig=(c == 7))
                     kb.op("act", lambda e, sl=sl: e.activation(out=qkT[sl][:], in_=p_t[:, :], func=AF.Copy),
                           reads=[b_pt], writes=[b_qkT[sl]])
                     qv = qkT[sl][:].rearrange("p (a h t) -> p a h t", a=2, h=4)
                     if i >= 2:
                         kb.dma("sp", QT[:, :, (i - 2) * 128:(i - 1) * 128].rearrange("h p t -> p h t"), qv[:, 0, :, :],
                                b_qkT[sl], reads=[b_qkT[sl]])
                     kb.dma("sp", KT[:, :, i * 128:(i + 1) * 128].rearrange("h p t -> p h t"), qv[:, 1, :, :],
                            b_qkT[sl], reads=[b_qkT[sl]])
                 kb.barrier()

            if LV >= 3:
              with ExitStack() as s2:
                wu = [sb(s2, f"wu{j}", [128, 8, 1536], BF16) for j in range(3)]
                b_wu = kb.buf("wu")
                wst = [sb(s2, f"wst{i}", [128, 1536]) for i in range(2)]
                b_wst = [kb.buf(f"wst{i}", dma=True) for i in range(2)]
                cw = sb(s2, "cw", [128, 3 * 1536]); b_cw = kb.buf("cw", dma=True)
                cb = sb(s2, "cb", [128, 1536]); b_cb = kb.buf("cb", dma=True)
                ut = [sb(s2, f"ut{i}", [128, 1536], BF16) for i in range(2)]
                b_ut = [kb.buf(f"ut{i}", dma=True) for i in range(2)]
                p_u = [ps(s2, f"p_u{i}", [128, 512]) for i in range(6)]
                b_pu = [kb.buf(f"pu{i}") for i in range(6)]
                win_v = win_d.rearrange("(c p) f -> p c f", p=128)
                kb.dma("sp", cw[:], cw_d.broadcast_to([128, 4608]), b_cw, writes=[b_cw])
                kb.dma("sp", cb[:], cb_d.broadcast_to([128, 1536]), b_cb, writes=[b_cb])
                for c in range(8):
                    sl = c % 2
                    kb.dma("sp", wst[sl][:], win_v[:, c, 1536:3072], b_wst[sl], writes=[b_wst[sl]])
                    for j in range(3):
                        kb.op("dve" if j < 2 else "pool", lambda e, j=j, c=c, sl=sl: e.tensor_tensor(
                            out=wu[j][:, c, :], in0=wst[sl][:], in1=cw[:, j * 1536:(j + 1) * 1536], op=ALU.mult),
                            reads=[b_wst[sl], b_cw], writes=[b_wu])
                for i in range(NT):
                    sl = i % 2
                    c0 = XOFF + i * 128
                    rd = [b_hTa[i + 2], b_hTb[i + 2], b_wu, b_hpad]
                    if i > 0:
                        rd += [b_hTa[i + 1], b_hTb[i + 1]]
                    if i < NT - 1:
                        rd += [b_hTa[i + 3], b_hTb[i + 3]]
                    for g in range(3):
                        pb = sl * 3 + g
                        n = 0
                        for j in range(3):
                            for c in range(8):
                                kb.op("pe", lambda e, c=c, j=j, g=g, pb=pb, c0=c0, n=n: e.matmul(
                                    p_u[pb][:, :], hT[:, c, c0 + j - 1:c0 + j - 1 + 128], wu[j][:, c, g * 512:(g + 1) * 512],
                                    start=(n == 0), stop=(n == 23)), reads=rd, writes=[b_pu[pb]], sig=(n == 23))
                                n += 1
                        kb.op("dve", lambda e, g=g, pb=pb, sl=sl: e.tensor_tensor(
                            out=ut[sl][:, g * 512:(g + 1) * 512], in0=p_u[pb][:, :], in1=cb[:, g * 512:(g + 1) * 512], op=ALU.add),
                            reads=[b_pu[pb], b_cb], writes=[b_ut[sl]])
                    kb.dma("sp", UU[i * 128:(i + 1) * 128, :], ut[sl][:], b_ut[sl], reads=[b_ut[sl]])
                kb.barrier()

        if LV >= 4:
          with ExitStack() as st:
            qh = [sb(st, f"qh{i}", [128, S], BF16) for i in range(2)]
            b_qh = [kb.buf(f"qh{i}", dma=True) for i in range(2)]
            kh = [sb(st, f"kh{i}", [128, NKT * 128], BF16) for i in range(2)]
            b_kh = [kb.buf(f"kh{i}", dma=True) for i in range(2)]
            vh = [sb(st, f"vh{i}", [128, NKT, 130], BF16) for i in range(2)]
            b_vh = [kb.buf(f"vh{i}", dma=True) for i in range(2)]
            subg = sb(st, "subg", [128, 128]); b_subg = kb.buf("subg", dma=True)
            NPT = 4
            pt_ = [sb(st, f"ptile{i}", [128, 512], BF16) for i in range(NPT)]
            b_ptile = [kb.buf(f"ptile{i}") for i in range(NPT)]
            p_s = [ps(st, f"p_s{i}", [128, 512]) for i in range(3)]
            b_ps = [kb.buf(f"ps{i}") for i in range(3)]
            p_o = [ps(st, f"p_o{i}", [128, 512]) for i in range(4)]
            b_po = [kb.buf(f"po{i}") for i in range(4)]
            rr = [sb(st, f"rr{i}", [128, 4]) for i in range(2)]
            b_rr = [kb.buf(f"rr{i}") for i in range(2)]
            ta = [sb(st, f"ta{i}", [128, 128]) for i in range(2)]
            b_ta = [kb.buf(f"ta{i}") for i in range(2)]
            aa = [sb(st, f"aa{i}", [128, 128]) for i in range(2)]
            b_aa = [kb.buf(f"aa{i}") for i in range(2)]
            junkb = sb(st, "junkBB", [128, 128]); b_junkb = kb.buf("junkBB")
            ao = [sb(st, f"ao{i}", [128, 128], BF16) for i in range(2)]
            b_ao = [kb.buf(f"ao{i}", dma=True) for i in range(2)]

            kb.dma("sp", subg[:], subg_d.broadcast_to([128, 128]), b_subg, writes=[b_subg])
            for sl in range(2):
                kb.op("pool", lambda e, sl=sl: e.memset(vh[sl][:, :, 128:130], 1.0), writes=[b_vh[sl]])
            VVv = VV.rearrange("(t p) (h d) -> h p t d", p=128, h=4)
            nps = 0
            npt = 0
            nq = 0
            for h in range(4):
                sl = h % 2
                kb.dma("sp", qh[sl][:], QT[h], b_qh[sl], writes=[b_qh[sl]])
                kb.dma("sp", kh[sl][:], KT[h], b_kh[sl], writes=[b_kh[sl]])
                kb.dma("sp", vh[sl][:, :, 0:128], VVv[h], b_vh[sl], writes=[b_vh[sl]])
                for qc in range(8):
                    for m in range(2):
                        def qk(kt, m=m, qc=qc, sl=sl):
                            nonlocal_a = kt % 3
                            kb.op("pe", lambda e: e.matmul(
                                p_s[nonlocal_a][:, :], kh[sl][m * 64:(m + 1) * 64, kt * 128:(kt + 1) * 128],
                                qh[sl][m * 64:(m + 1) * 64, qc * 512:(qc + 1) * 512], start=True, stop=True),
                                reads=[b_kh[sl], b_qh[sl]], writes=[b_ps[nonlocal_a]])
                        qk(0)
                        qk(1)
                        for kt in range(NKT):
                            a = kt % 3
                            b = npt % NPT
                            npt += 1
                            kb.op("act", lambda e, a=a, b=b: e.activation(out=pt_[b][:], in_=p_s[a][:, :], func=AF.Exp, scale=0.125),
                                  reads=[b_ps[a]], writes=[b_ptile[b]])
                            if kt + 2 < NKT:
                                qk(kt + 2)
                            for qt in range(4):
                                pb = m * 2 + qt // 2
                                o0 = (qt % 2) * 130
                                kb.op("pe", lambda e, b=b, qt=qt, pb=pb, o0=o0, kt=kt, sl=sl: e.matmul(
                                    p_o[pb][:, o0:o0 + 129], pt_[b][:, qt * 128:(qt + 1) * 128], vh[sl][:, kt, 0:129],
                                    start=(kt == 0 and qt % 2 == 0), stop=(kt == NKT - 1)),
                                    reads=[b_ptile[b], b_vh[sl]], writes=[b_po[pb]], sig=(kt == NKT - 1 and qt % 2 == 1) or qt == 3)
                    for qt in range(4):
                        z = nq % 2
                        nq += 1
                        pb0 = qt // 2
                        pb1 = 2 + qt // 2
                        o0 = (qt % 2) * 130
                        kb.op("dve", lambda e, z=z, pb0=pb0, o0=o0: e.reciprocal(out=rr[z][:, 0:1], in_=p_o[pb0][:, o0 + 128:o0 + 129]),
                              reads=[b_po[pb0]], writes=[b_rr[z]])
                        kb.op("dve", lambda e, z=z, pb1=pb1, o0=o0: e.reciprocal(out=rr[z][:, 1:2], in_=p_o[pb1][:, o0 + 128:o0 + 129]),
                              reads=[b_po[pb1]], writes=[b_rr[z]])
                        kb.op("dve", lambda e, z=z: e.tensor_tensor(out=rr[z][:, 1:2], in0=rr[z][:, 1:2], in1=nlam[:, 0:1], op=ALU.mult),
                              reads=[b_rr[z], b_nlam], writes=[b_rr[z]])
                        kb.op("dve", lambda e, z=z, pb1=pb1, o0=o0: e.tensor_scalar(ta[z][:], p_o[pb1][:, o0:o0 + 128], rr[z][:, 1:2], None, ALU.mult),
                              reads=[b_po[pb1], b_rr[z]], writes=[b_ta[z]])
                        kb.op("dve", lambda e, z=z, pb0=pb0, o0=o0: e.scalar_tensor_tensor(
                            out=aa[z][:], in0=p_o[pb0][:, o0:o0 + 128], scalar=rr[z][:, 0:1], in1=ta[z][:], op0=ALU.mult, op1=ALU.add),
                            reads=[b_po[pb0], b_rr[z], b_ta[z]], writes=[b_aa[z]])
                        rms_rstd(None, aa[z][:], junkb[:], rr[z][:, 2:3], rr[z][:, 3:4], 128, [b_aa[z]], b_junkb, b_rr[z], b_rr[z])
                        kb.op("dve", lambda e, z=z: e.tensor_scalar(rr[z][:, 3:4], rr[z][:, 3:4], 1.0 - LAM_INIT, None, ALU.mult),
                              reads=[b_rr[z]], writes=[b_rr[z]])
                        kb.op("dve", lambda e, z=z: e.scalar_tensor_tensor(
                            out=ao[z][:], in0=aa[z][:], scalar=rr[z][:, 3:4], in1=subg[:], op0=ALU.mult, op1=ALU.mult),
                            reads=[b_aa[z], b_rr[z], b_subg], writes=[b_ao[z]])
                        tok0 = qc * 512 + qt * 128
                        kb.dma("sp", AT[tok0:tok0 + 128, h * 128:(h + 1) * 128], ao[z][:], b_ao[z], reads=[b_ao[z]])
            kb.barrier()

        if LV >= 5:
          with ExitStack() as st:
            h2T = sb(st, "h2T_f", [64, S]); b_h2T = kb.buf("h2Tf")
            w3pm = sb(st, "w3pm", [64, 2048]); b_w3pm = kb.buf("w3pm")
            skipb = sb(st, "skipb", [128, 1024]); b_skipb = kb.buf("skipb", dma=True)
            with ExitStack() as s2:
                featsT = sb(s2, "featsT", [17, S]); b_feats = kb.buf("feats", dma=True)
                hw1 = sb(s2, "hw1", [17, 64]); b_hw1 = kb.buf("hw1", dma=True)
                hw2 = sb(s2, "hw2", [64, 64]); b_hw2 = kb.buf("hw2", dma=True)
                hw3 = sb(s2, "hw3", [64, 2048]); b_hw3 = kb.buf("hw3", dma=True)
                hcol_ = sb(s2, "hcols", [64, 4]); b_hcols = kb.buf("hcols", dma=True)
                fb = sb(s2, "fb", [64, 2]); b_fb = kb.buf("fb")
                negpi = sb(s2, "negpi", [64, 1]); b_negpi = kb.buf("negpi")
                h1T = sb(s2, "h1T_f", [64, S]); b_h1T = kb.buf("h1Tf")
                tmpm = [sb(s2, f"tmpm{i}", [64, 512]) for i in range(2)]
                b_tmpm = [kb.buf(f"tmpm{i}") for i in range(2)]
                wrp = [sb(s2, f"wrp{i}", [64, 512]) for i in range(2)]
                b_wrp = [kb.buf(f"wrp{i}") for i in range(2)]
                p_m = [ps(s2, f"p_m{i}", [128, 512]) for i in range(2)]
                b_pm = [kb.buf(f"pm{i}") for i in range(2)]
                kb.dma("sp", featsT[:], featsT_d, b_feats, writes=[b_feats])
                kb.dma("sp", hw1[:], hw1_d, b_hw1, writes=[b_hw1])
                kb.dma("sp", hw2[:], hw2_d, b_hw2, writes=[b_hw2])
                kb.dma("sp", hw3[:], hw3_d, b_hw3, writes=[b_hw3])
                kb.dma("sp", hcol_[:], hcol_d, b_hcols, writes=[b_hcols])
                kb.dma("sp", skipb[:], hskip_d.broadcast_to([128, 1024]), b_skipb, writes=[b_skipb])
                kb.op("dve", lambda e: e.memset(negpi[:], -math.pi), writes=[b_negpi])
                OFFS = 0.0
                for l in range(2):
                    kb.op("dve", lambda e, l=l: e.tensor_tensor(out=fb[:, l:l + 1], in0=hcol_[:, 2 * l:2 * l + 1],
                                                                in1=hcol_[:, 2 * l + 1:2 * l + 2], op=ALU.mult),
                          reads=[b_hcols], writes=[b_fb])
                    kb.op("dve", lambda e, l=l: e.tensor_scalar(fb[:, l:l + 1], fb[:, l:l + 1], OFFS, None, ALU.add),
                          reads=[b_fb], writes=[b_fb])
                for l in range(2):
                    src, b_src = (featsT, b_feats) if l == 0 else (h1T, b_h1T)
                    dst, b_dst = (h1T, b_h1T) if l == 0 else (h2T, b_h2T)
                    wl, b_wl = (hw1, b_hw1) if l == 0 else (hw2, b_hw2)
                    kdim = 17 if l == 0 else 64
                    for tcn in range(8):
                        a = tcn % 2
                        kb.op("pe", lambda e, a=a, tcn=tcn, src=src, wl=wl, kdim=kdim: e.matmul(
                            p_m[a][0:64, :], wl[0:kdim, :], src[0:kdim, tcn * 512:(tcn + 1) * 512], start=True, stop=True),
                            reads=[b_src, b_wl], writes=[b_pm[a]])
                        kb.op("dve", lambda e, a=a, l=l: e.tensor_scalar(
                            tmpm[a][:], p_m[a][0:64, :], hcol_[:, 2 * l + 1:2 * l + 2], fb[:, l:l + 1], ALU.mult, ALU.add),
                            reads=[b_pm[a], b_hcols, b_fb], writes=[b_tmpm[a]])
                        for (thr, per, cmp_) in ((-math.pi, 2.0 * math.pi, ALU.is_lt), (math.pi, -2.0 * math.pi, ALU.is_gt)):
                            kb.op("dve", lambda e, a=a, thr=thr, per=per, cmp_=cmp_: e.tensor_scalar(wrp[a][:], tmpm[a][:], thr, per, cmp_, ALU.mult),
                                  reads=[b_tmpm[a]], writes=[b_wrp[a]])
                            kb.op("dve", lambda e, a=a: e.tensor_tensor(out=tmpm[a][:], in0=tmpm[a][:], in1=wrp[a][:], op=ALU.add),
                                  reads=[b_tmpm[a], b_wrp[a]], writes=[b_tmpm[a]])
                        kb.op("act", lambda e, a=a, tcn=tcn, dst=dst: e.activation(
                            out=dst[:, tcn * 512:(tcn + 1) * 512], in_=tmpm[a][:], func=AF.Sin),
                            reads=[b_tmpm[a]], writes=[b_dst])
                w3v = hw3[:, :].rearrange("j (n r c) -> j n r c", n=2, r=2)
                w3o = w3pm[:, :].rearrange("j (n r c) -> j n r c", n=2, r=2)
                for n_ in range(2):
                    kb.op("dve", lambda e, n_=n_: e.tensor_tensor(out=w3o[:, n_, 0, :], in0=w3v[:, n_, 0, :], in1=w3v[:, n_, 1, :], op=ALU.add),
                          reads=[b_hw3], writes=[b_w3pm])
                    kb.op("dve", lambda e, n_=n_: e.tensor_tensor(out=w3o[:, n_, 1, :], in0=w3v[:, n_, 0, :], in1=w3v[:, n_, 1, :], op=ALU.subtract),
                          reads=[b_hw3], writes=[b_w3pm])
                kb.barrier()
            w3o = w3pm[:, :].rearrange("j (n r c) -> j n r c", n=2, r=2)

            HC = 256
            vz = sb(st, "vz", [128, NT, HC], BF16); b_vz = kb.buf("vz", dma=True)
            gp = sb(st, "gp", [128, NT, HC], BF16); b_gp = kb.buf("gp")
            gm = sb(st, "gm", [128, NT, HC], BF16); b_gm = kb.buf("gm")
            Yr = sb(st, "Yr", [128, NT, HC], BF16); b_Yr = kb.buf("Yr")
            Yn = sb(st, "Yn", [128, NT, HC], BF16); b_Yn = kb.buf("Yn")
            tabC = [sb(st, f"tabC{i}", [128, NT, 128], BF16) for i in range(2)]
            b_tabC = [kb.buf(f"tabC{i}", dma=True) for i in range(2)]
            tabS = [sb(st, f"tabS{i}", [128, NT, 128], BF16) for i in range(2)]
            b_tabS = [kb.buf(f"tabS{i}", dma=True) for i in range(2)]
            wnd = [sb(st, f"wnd{i}", [128, HC]) for i in range(2)]
            b_wnd = [kb.buf(f"wnd{i}", dma=True) for i in range(2)]
            xg = [sb(st, f"xg{i}", [128, HC], BF16) for i in range(2)]
            b_xg = [kb.buf(f"xg{i}", dma=True) for i in range(2)]
            zo = [sb(st, f"zo{i}", [128, HC], BF16) for i in range(2)]
            b_zo = [kb.buf(f"zo{i}", dma=True) for i in range(2)]
            Hr = sb(st, "Hr", [128, HC]); b_Hr = kb.buf("Hr")
            Hs = sb(st, "Hs", [128, HC]); b_Hs = kb.buf("Hs")
            tt = [sb(st, f"ttC{i}", [128, HC]) for i in range(4)]
            b_tt = [kb.buf(f"ttC{i}") for i in range(4)]
            p_g = [ps(st, f"p_g{i}", [128, 512]) for i in range(2)]
            b_pg = [kb.buf(f"pgC{i}") for i in range(2)]
            p_x = [ps(st, f"p_x{i}", [128, 512]) for i in range(4)]
            b_px = [kb.buf(f"pxC{i}") for i in range(4)]
            p_y = [ps(st, f"p_y{i}", [128, 512]) for i in range(2)]
            b_py = [kb.buf(f"pyC{i}") for i in range(2)]
            ntab = 0
            UUv = UU.rearrange("(t p) f -> p t f", p=128)
            windv = window_d.rearrange("(t p) c -> t p c", p=128)
            for half in range(2):
                hc0 = half * HC
                kb.dma("sp", vz[:], UUv[:, :, hc0:hc0 + HC], b_vz, writes=[b_vz])
                for order in range(2):
                    for tt_ in range(NT):
                        a = tt_ % 2
                        kb.dma("sp", wnd[a][:], windv[tt_][:, hc0:hc0 + HC], b_wnd[a], writes=[b_wnd[a]])
                        for r in range(2):
                            kb.op("pe", lambda e, a=a, r=r, tt_=tt_, order=order, hc0=hc0: e.matmul(
                                p_g[a][:, r * HC:(r + 1) * HC], h2T[:, tt_ * 128:(tt_ + 1) * 128], w3o[:, order, r, hc0:hc0 + HC],
                                start=True, stop=True), reads=[b_h2T, b_w3pm], writes=[b_pg[a]], sig=(r == 1))
                        kb.op("dve", lambda e, a=a, tt_=tt_: e.tensor_tensor(out=gp[:, tt_, :], in0=p_g[a][:, 0:HC], in1=wnd[a][:], op=ALU.mult),
                              reads=[b_pg[a], b_wnd[a]], writes=[b_gp])
                        kb.op("dve", lambda e, a=a, tt_=tt_: e.tensor_tensor(out=gm[:, tt_, :], in0=p_g[a][:, HC:2 * HC], in1=wnd[a][:], op=ALU.mult),
                              reads=[b_pg[a], b_wnd[a]], writes=[b_gm])
                    for kt in range(NT):
                        a = ntab % 2
                        ntab += 1
                        kb.dma("sp", tabC[a][:].rearrange("p c k -> p (c k)"), ctk_d[kt], b_tabC[a], writes=[b_tabC[a]])
                        kb.dma("sp", tabS[a][:].rearrange("p c k -> p (c k)"), stk_d[kt], b_tabS[a], writes=[b_tabS[a]])
                        pc, psn = p_x[2 * (kt % 2)], p_x[2 * (kt % 2) + 1]
                        bpc, bps = b_px[2 * (kt % 2)], b_px[2 * (kt % 2) + 1]
                        for c in range(NT):
                            fl = dict(start=(c == 0), stop=(c == NT - 1))
                            kb.op("pe", lambda e, c=c, a=a, pc=pc, fl=fl: e.matmul(pc[:, 0:HC], tabC[a][:, c, :], vz[:, c, :], **fl),
                                  reads=[b_tabC[a], b_vz], writes=[bpc], sig=False)
                            fl = dict(start=False, stop=(c == NT - 1))
                            kb.op("pe", lambda e, c=c, a=a, pc=pc, fl=fl: e.matmul(pc[:, HC:2 * HC], tabC[a][:, c, :], gp[:, c, :], **fl),
                                  reads=[b_tabC[a], b_gp], writes=[bpc], sig=(c == NT - 1))
                            fl = dict(start=(c == 0), stop=(c == NT - 1))
                            kb.op("pe", lambda e, c=c, a=a, psn=psn, fl=fl: e.matmul(psn[:, 0:HC], tabS[a][:, c, :], vz[:, c, :], **fl),
                                  reads=[b_tabS[a], b_vz], writes=[bps], sig=False)
                            fl = dict(start=False, stop=(c == NT - 1))
                            kb.op("pe", lambda e, c=c, a=a, psn=psn, fl=fl: e.matmul(psn[:, HC:2 * HC], tabS[a][:, c, :], gm[:, c, :], **fl),
                                  reads=[b_tabS[a], b_gm], writes=[bps], sig=(c == NT - 1))
                        sk0 = order * 512 + hc0
                        kb.op("dve", lambda e, pc=pc, sk0=sk0: e.tensor_tensor(out=Hr[:], in0=pc[:, HC:2 * HC], in1=skipb[:, sk0:sk0 + HC], op=ALU.add),
                              reads=[bpc, b_skipb], writes=[b_Hr])
                        kb.op("act", lambda e, psn=psn: e.activation(out=Hs[:], in_=psn[:, HC:2 * HC], func=AF.Copy),
                              reads=[bps], writes=[b_Hs])
                        kb.op("dve", lambda e, pc=pc: e.tensor_tensor(out=tt[0][:], in0=pc[:, 0:HC], in1=Hr[:], op=ALU.mult),
                              reads=[bpc, b_Hr], writes=[b_tt[0]])
                        kb.op("dve", lambda e, psn=psn: e.tensor_tensor(out=tt[1][:], in0=psn[:, 0:HC], in1=Hs[:], op=ALU.mult),
                              reads=[bps, b_Hs], writes=[b_tt[1]])
                        kb.op("pool", lambda e, kt=kt: e.tensor_tensor(out=Yr[:, kt, :], in0=tt[0][:], in1=tt[1][:], op=ALU.subtract),
                              reads=[b_tt[0], b_tt[1]], writes=[b_Yr])
                        kb.op("dve", lambda e, pc=pc: e.tensor_tensor(out=tt[2][:], in0=pc[:, 0:HC], in1=Hs[:], op=ALU.mult),
                              reads=[bpc, b_Hs], writes=[b_tt[2]])
                        kb.op("dve", lambda e, psn=psn: e.tensor_tensor(out=tt[3][:], in0=psn[:, 0:HC], in1=Hr[:], op=ALU.mult),
                              reads=[bps, b_Hr], writes=[b_tt[3]])
                        kb.op("pool", lambda e, kt=kt: e.tensor_tensor(out=Yn[:, kt, :], in0=tt[2][:], in1=tt[3][:], op=ALU.add),
                              reads=[b_tt[2], b_tt[3]], writes=[b_Yn])
                    xoff = 512 * (order + 1) + hc0
                    for t_ in range(NT):
                        a = ntab % 2
                        ntab += 1
                        kb.dma("sp", tabC[a][:].rearrange("p c k -> p (c k)"), ckt_d[t_], b_tabC[a], writes=[b_tabC[a]])
                        kb.dma("sp", tabS[a][:].rearrange("p c k -> p (c k)"), skt_d[t_], b_tabS[a], writes=[b_tabS[a]])
                        z = t_ % 2
                        kb.dma("sp", xg[z][:], UU[t_ * 128:(t_ + 1) * 128, xoff:xoff + HC], b_xg[z], writes=[b_xg[z]])
                        for c in range(NT):
                            kb.op("pe", lambda e, c=c, a=a, z=z: e.matmul(p_y[z][:, 0:HC], tabC[a][:, c, :], Yr[:, c, :], start=(c == 0), stop=False),
                                  reads=[b_tabC[a], b_Yr], writes=[b_py[z]], sig=False)
                            kb.op("pe", lambda e, c=c, a=a, z=z: e.matmul(p_y[z][:, 0:HC], tabS[a][:, c, :], Yn[:, c, :], start=False, stop=(c == NT - 1)),
                                  reads=[b_tabS[a], b_Yn], writes=[b_py[z]], sig=(c == NT - 1))
                        if order == 0:
                            kb.op("dve", lambda e, z=z, t_=t_: e.scalar_tensor_tensor(
                                out=vz[:, t_, :], in0=p_y[z][:, 0:HC], scalar=2.0 / NFFT, in1=xg[z][:], op0=ALU.mult, op1=ALU.mult),
                                reads=[b_py[z], b_xg[z]], writes=[b_vz])
                        else:
                            kb.op("dve", lambda e, z=z: e.scalar_tensor_tensor(
                                out=zo[z][:], in0=p_y[z][:, 0:HC], scalar=2.0 / NFFT, in1=xg[z][:], op0=ALU.mult, op1=ALU.mult),
                                reads=[b_py[z], b_xg[z]], writes=[b_zo[z]])
                            kb.dma("sp", Z2[t_ * 128:(t_ + 1) * 128, hc0:hc0 + HC], zo[z][:], b_zo[z], reads=[b_zo[z]])
            kb.barrier()

        if LV >= 6:
          with ExitStack() as st:
            wo = sb(st, "wo", [128, 8, D], BF16); b_wo = kb.buf("wo")
            wost = [sb(st, f"wost{i}", [128, D]) for i in range(2)]
            b_wost = [kb.buf(f"wost{i}", dma=True) for i in range(2)]
            rw = sb(st, "rw", [128, 8, NE], BF16); b_rw = kb.buf("rw", dma=True)
            rbb = sb(st, "rbb", [128, NE]); b_rbb = kb.buf("rbb", dma=True)
            hyg = sb(st, "hyg", [128, 512]); b_hyg = kb.buf("hyg", dma=True)
            cat = [sb(st, f"cat{i}", [128, D], BF16) for i in range(2)]
            b_cat = [kb.buf(f"cat{i}", dma=True) for i in range(2)]
            z2t = [sb(st, f"z2t{i}", [128, 512], BF16) for i in range(2)]
            b_z2t = [kb.buf(f"z2t{i}", dma=True) for i in range(2)]
            xt = [sb(st, f"xtD{i}", [128, D]) for i in range(2)]
            b_xt = [kb.buf(f"xtD{i}", dma=True) for i in range(2)]
            xnw = [sb(st, f"xnw{i}", [128, D]) for i in range(2)]
            b_xnw = [kb.buf(f"xnw{i}", dma=True) for i in range(2)]
            junk = sb(st, "junkD", [128, D], BF16); b_junk = kb.buf("junkD")
            sD = [sb(st, f"sD{i}", [128, 4]) for i in range(2)]
            b_sD = [kb.buf(f"sD{i}") for i in range(2)]
            catT = [sb(st, f"catT{i}", [128, D], BF16) for i in range(2)]
            b_catT = [kb.buf(f"catT{i}") for i in range(2)]
            xn2 = [sb(st, f"xn2{i}", [128, D], BF16) for i in range(2)]
            b_xn2 = [kb.buf(f"xn2{i}") for i in range(2)]
            h2t = [sb(st, f"h2t{i}", [128, D], BF16) for i in range(2)]
            b_h2t = [kb.buf(f"h2t{i}", dma=True) for i in range(2)]
            lg = [sb(st, f"lg{i}", [128, NE]) for i in range(2)]
            b_lg = [kb.buf(f"lg{i}") for i in range(2)]
            m8 = [sb(st, f"m8{i}", [128, 8]) for i in range(2)]
            b_m8 = [kb.buf(f"m8{i}") for i in range(2)]
            msk = [sb(st, f"msk{i}", [128, NE]) for i in range(2)]
            b_msk = [kb.buf(f"msk{i}") for i in range(2)]
            gt = [sb(st, f"gt{i}", [128, NE]) for i in range(2)]
            b_gt = [kb.buf(f"gt{i}", dma=True) for i in range(2)]
            p_t = [ps(st, f"p_tD{i}", [128, D], BF16) for i in range(2)]
            b_pt = [kb.buf(f"ptD{i}") for i in range(2)]
            p_m = [ps(st, f"p_mD{i}", [128, D]) for i in range(2)]
            b_pmx = [kb.buf(f"pmD{i}") for i in range(2)]
            p_l = ps(st, "p_lD", [128, 512]); b_pl = kb.buf("plD")

            wout_v = wout_d.rearrange("(c p) f -> p c f", p=128)
            for c in range(8):
                sl = c % 2
                kb.dma("sp", wost[sl][:], wout_v[:, c, :], b_wost[sl], writes=[b_wost[sl]])
                kb.op("dve", lambda e, c=c, sl=sl: e.tensor_tensor(out=wo[:, c, :], in0=wost[sl][:], in1=G1row[:], op=ALU.mult),
                      reads=[b_wost[sl], b_G1], writes=[b_wo])
            kb.dma("pool", rw[:], rw_d.rearrange("(c p) e -> p c e", p=128), b_rw, writes=[b_rw])
            kb.dma("sp", rbb[:], rb_d.broadcast_to([128, NE]), b_rbb, writes=[b_rbb])
            kb.dma("sp", hyg[:], hyg_d.broadcast_to([128, 512]), b_hyg, writes=[b_hyg])
            for i in range(NT):
                sl = i % 2
                r0, r1 = i * 128, (i + 1) * 128
                kb.dma("sp", cat[sl][:, 0:512], AT[r0:r1, :], b_cat[sl], writes=[b_cat[sl]])
                kb.dma("sp", z2t[sl][:], Z2[r0:r1, :], b_z2t[sl], writes=[b_z2t[sl]])
                kb.dma("sp", xt[sl][:], x_d[r0:r1, :], b_xt[sl], writes=[b_xt[sl]])
                rms_rstd(None, z2t[sl][:], junk[:, 0:512], sD[sl][:, 0:1], sD[sl][:, 1:2], 512, [b_z2t[sl]], b_junk, b_sD[sl], b_sD[sl])
                kb.op("dve", lambda e, sl=sl: e.scalar_tensor_tensor(
                    out=cat[sl][:, 512:1024], in0=z2t[sl][:], scalar=sD[sl][:, 1:2], in1=hyg[:], op0=ALU.mult, op1=ALU.mult),
                    reads=[b_z2t[sl], b_sD[sl], b_hyg], writes=[b_cat[sl]])
                for c in range(8):
                    kb.op("pe", lambda e, c=c, sl=sl: e.transpose(p_t[0][:, c * 128:(c + 1) * 128], cat[sl][:, c * 128:(c + 1) * 128], ident_b[:]),
                          reads=[b_cat[sl], b_identb], writes=[b_pt[0]], sig=(c == 7))
                kb.op("act", lambda e, sl=sl: e.activation(out=catT[sl][:], in_=p_t[0][:, :], func=AF.Copy),
                      reads=[b_pt[0]], writes=[b_catT[sl]])
                for hh in range(2):
                    for c in range(8):
                        kb.op("pe", lambda e, c=c, hh=hh, sl=sl: e.matmul(
                            p_m[sl][:, hh * 512:(hh + 1) * 512], catT[sl][:, c * 128:(c + 1) * 128], wo[:, c, hh * 512:(hh + 1) * 512],
                            start=(c == 0), stop=(c == 7)), reads=[b_catT[sl], b_wo], writes=[b_pmx[sl]], sig=(c == 7 and hh == 1))
                kb.op("dve", lambda e, sl=sl: e.tensor_tensor(out=xnw[sl][:], in0=p_m[sl][:, :], in1=xt[sl][:], op=ALU.add),
                      reads=[b_pmx[sl], b_xt[sl]], writes=[b_xnw[sl]])
                kb.dma("sp", out_d[r0:r1, :], xnw[sl][:], b_xnw[sl], reads=[b_xnw[sl]])
                rms_rstd(None, xnw[sl][:], junk[:], sD[sl][:, 2:3], sD[sl][:, 3:4], D, [b_xnw[sl]], b_junk, b_sD[sl], b_sD[sl])
                kb.op("dve", lambda e, sl=sl: e.tensor_scalar(xn2[sl][:], xnw[sl][:], sD[sl][:, 3:4], None, ALU.mult),
                      reads=[b_xnw[sl], b_sD[sl]], writes=[b_xn2[sl]])
                for c in range(8):
                    kb.op("pe", lambda e, c=c, sl=sl: e.transpose(p_t[1][:, c * 128:(c + 1) * 128], xn2[sl][:, c * 128:(c + 1) * 128], ident_b[:]),
                          reads=[b_xn2[sl], b_identb], writes=[b_pt[1]], sig=(c == 7))
                for c in range(8):
                    if False:
                        kb.op("act", lambda e, c=c, sl=sl: e.activation(
                            out=h2t[sl][:, c * 128:(c + 1) * 128], in_=p_t[1][:, c * 128:(c + 1) * 128], func=AF.Identity,
                            scale=ABv[:, 4, c:c + 1], bias=ABv[:, 5, c:c + 1]), reads=[b_pt[1], b_AB], writes=[b_h2t[sl]])
                    else:
                        kb.op("dve", lambda e, c=c, sl=sl: e.tensor_scalar(
                            h2t[sl][:, c * 128:(c + 1) * 128], p_t[1][:, c * 128:(c + 1) * 128],
                            ABv[:, 4, c:c + 1], ABv[:, 5, c:c + 1], ALU.mult, ALU.add), reads=[b_pt[1], b_AB], writes=[b_h2t[sl]])
                kb.dma("sp", H2T[:, :, r0:r1].rearrange("c p t -> p c t"), h2t[sl][:].rearrange("p (c t) -> p c t", c=8),
                       b_h2t[sl], reads=[b_h2t[sl]])
                for c in range(8):
                    kb.op("pe", lambda e, c=c, sl=sl: e.matmul(p_l[:, 0:NE], h2t[sl][:, c * 128:(c + 1) * 128], rw[:, c, :],
                                                               start=(c == 0), stop=(c == 7)),
                          reads=[b_h2t[sl], b_rw], writes=[b_pl], sig=(c == 7))
                kb.op("dve", lambda e, sl=sl: e.tensor_tensor(out=lg[sl][:], in0=p_l[:, 0:NE], in1=rbb[:], op=ALU.add),
                      reads=[b_pl, b_rbb], writes=[b_lg[sl]])
                kb.op("dve", lambda e, sl=sl: e.max(out=m8[sl][:], in_=lg[sl][:]), reads=[b_lg[sl]], writes=[b_m8[sl]])
                kb.op("dve", lambda e, sl=sl: e.tensor_scalar(msk[sl][:], lg[sl][:], m8[sl][:, 3:4], None, ALU.is_ge),
                      reads=[b_lg[sl], b_m8[sl]], writes=[b_msk[sl]])
                kb.op("dve", lambda e, sl=sl: e.tensor_scalar(lg[sl][:], lg[sl][:], m8[sl][:, 0:1], None, ALU.subtract),
                      reads=[b_lg[sl], b_m8[sl]], writes=[b_lg[sl]])
                kb.op("act", lambda e, sl=sl: e.activation(out=lg[sl][:], in_=lg[sl][:], func=AF.Exp), reads=[b_lg[sl]], writes=[b_lg[sl]])
                kb.op("dve", lambda e, sl=sl: e.tensor_tensor(out=lg[sl][:], in0=lg[sl][:], in1=msk[sl][:], op=ALU.mult),
                      reads=[b_lg[sl], b_msk[sl]], writes=[b_lg[sl]])
                kb.op("dve", lambda e, sl=sl: e.tensor_reduce(out=m8[sl][:, 4:5], in_=lg[sl][:], axis=AX.X, op=ALU.add),
                      reads=[b_lg[sl]], writes=[b_m8[sl]])
                kb.op("dve", lambda e, sl=sl: e.reciprocal(out=m8[sl][:, 4:5], in_=m8[sl][:, 4:5]), reads=[b_m8[sl]], writes=[b_m8[sl]])
                kb.op("dve", lambda e, sl=sl: e.tensor_scalar(gt[sl][:], lg[sl][:], m8[sl][:, 4:5], None, ALU.mult),
                      reads=[b_lg[sl], b_m8[sl]], writes=[b_gt[sl]])
                kb.dma("sp", GATES[r0:r1, :], gt[sl][:], b_gt[sl], reads=[b_gt[sl]])
            kb.barrier()

        if LV >= 7:
          with ExitStack() as st:
            TB = 1024
            NTB = TB // 128
            NS = 3
            gates = sb(st, "gatesE", [128, NT, NE]); b_gates = kb.buf("gatesE", dma=True)
            h2b = sb(st, "h2b", [128, 8, TB], BF16); b_h2b = kb.buf("h2b", dma=True)
            acc = sb(st, "acc", [128, NTB, D]); b_acc = [kb.buf(f"acc{i}") for i in range(NTB)]
            w1b = [sb(st, f"w1b{i}", [128, 8, 2 * D], BF16) for i in range(2)]
            b_w1b = [kb.buf(f"w1b{i}", dma=True) for i in range(2)]
            w2b = [sb(st, f"w2b{i}", [128, 8, D], BF16) for i in range(2)]
            b_w2b = [kb.buf(f"w2b{i}", dma=True) for i in range(2)]
            b1c = [sb(st, f"b1c{i}", [128, 16]) for i in range(2)]
            b_b1c = [kb.buf(f"b1c{i}", dma=True) for i in range(2)]
            b2r = [sb(st, f"b2r{i}", [1, D], BF16) for i in range(2)]
            b_b2r = [kb.buf(f"b2r{i}", dma=True) for i in range(2)]
            actT = [sb(st, f"actT{i}", [128, 8, 512], BF16) for i in range(2)]
            b_actT = [kb.buf(f"actT{i}") for i in range(2)]
            gcl = [sb(st, f"gcl{i}", [128, 512]) for i in range(NS)]
            b_gcl = [kb.buf(f"gcl{i}") for i in range(NS)]
            sgm = [sb(st, f"sgm{i}", [128, 512]) for i in range(NS)]
            b_sgm = [kb.buf(f"sgm{i}") for i in range(NS)]
            lcl = [sb(st, f"lcl{i}", [128, 512]) for i in range(NS)]
            b_lcl = [kb.buf(f"lcl{i}") for i in range(NS)]
            xo = [sb(st, f"xoE{i}", [128, D]) for i in range(2)]
            b_xo = [kb.buf(f"xoE{i}", dma=True) for i in range(2)]
            p_gl = [ps(st, f"p_gl{i}", [128, 512]) for i in range(2 * NS)]
            b_pgl = [kb.buf(f"pgl{i}") for i in range(2 * NS)]
            p_o2 = [ps(st, f"p_o2{i}", [128, 512]) for i in range(2)]
            b_po2 = [kb.buf(f"po2{i}") for i in range(2)]

            kb.dma("sp", gates[:], GATES.rearrange("(t p) e -> p t e", p=128), b_gates, writes=[b_gates])
            ew1_v = ew1_d.rearrange("e (c p) f -> e p c f", p=128)
            ew2_v = ew2_d.rearrange("e (c p) f -> e p c f", p=128)
            NU = (S // TB) * NE

            def load_weights(u):
                ex = u % NE
                ws = u % 2
                for c in range(0, 8, 2):
                    kb.dma("pool", w1b[ws][:, c:c + 2, :], ew1_v[ex][:, c:c + 2, :], b_w1b[ws], writes=[b_w1b[ws]])
                for c in range(0, 8, 4):
                    kb.dma("pool", w2b[ws][:, c:c + 4, :], ew2_v[ex][:, c:c + 4, :], b_w2b[ws], writes=[b_w2b[ws]])
                kb.dma("sp", b1c[ws][:], eb1_d[ex], b_b1c[ws], writes=[b_b1c[ws]])
                kb.dma("pool", b2r[ws][:], eb2_d[ex:ex + 1, :], b_b2r[ws], writes=[b_b2r[ws]])

            state = {"nfc": 0, "no2": 0, "nx": 0, "pend": None}

            def stage2(tb, ex, ws, grp, asl):
                for tq in range(4):
                    tl = grp * 4 + tq
                    tg = tb * NTB + tl
                    for dh in range(2):
                        o = state["no2"] % 2
                        state["no2"] += 1
                        for fc in range(8):
                            kb.op("pe", lambda e, fc=fc: e.matmul(
                                p_o2[o][:, :], actT[asl][:, fc, tq * 128:(tq + 1) * 128], w2b[ws][:, fc, dh * 512:(dh + 1) * 512],
                                start=(fc == 0), stop=False), reads=[b_actT[asl], b_w2b[ws]], writes=[b_po2[o]], sig=False)
                        kb.op("pe", lambda e: e.matmul(
                            p_o2[o][:, :], ones_b[0:1, :], b2r[ws][0:1, dh * 512:(dh + 1) * 512], start=False, stop=True),
                            reads=[b_onesb, b_b2r[ws]], writes=[b_po2[o]])
                        kb.op("dve", lambda e: e.scalar_tensor_tensor(
                            out=acc[:, tl, dh * 512:(dh + 1) * 512], in0=p_o2[o][:, :], scalar=gates[:, tg, ex:ex + 1],
                            in1=acc[:, tl, dh * 512:(dh + 1) * 512], op0=ALU.mult, op1=ALU.add),
                            reads=[b_po2[o], b_gates, b_acc[tl]], writes=[b_acc[tl]])

            def flush():
                if state["pend"] is not None:
                    stage2(*state["pend"])
                    state["pend"] = None

            load_weights(0)
            for tb in range(S // TB):
                t0_ = tb * TB
                kb.dma("sp", h2b[:], H2T[:, :, t0_:t0_ + TB].rearrange("c p t -> p c t"), b_h2b, writes=[b_h2b])
                for tl in range(NTB):
                    kb.op("pool", lambda e, tl=tl: e.memset(acc[:, tl, :], 0.0), writes=[b_acc[tl]])
                for ex in range(NE):
                    u = tb * NE + ex
                    ws = u % 2
                    for grp in range(TB // 512):
                        asl = (state["nfc"] // 8) % 2
                        prev = None
                        for fc in range(8):
                            a = state["nfc"] % NS
                            state["nfc"] += 1
                            pg_, pl_ = p_gl[2 * a], p_gl[2 * a + 1]
                            bpg, bpl = b_pgl[2 * a], b_pgl[2 * a + 1]
                            for c in range(8):
                                kb.op("pe", lambda e, c=c: e.matmul(
                                    pg_[:, :], w1b[ws][:, c, fc * 128:(fc + 1) * 128], h2b[:, c, grp * 512:(grp + 1) * 512],
                                    start=(c == 0), stop=(c == 7)), reads=[b_w1b[ws], b_h2b], writes=[bpg], sig=(c == 7))
                            for c in range(8):
                                kb.op("pe", lambda e, c=c: e.matmul(
                                    pl_[:, :], w1b[ws][:, c, D + fc * 128:D + (fc + 1) * 128], h2b[:, c, grp * 512:(grp + 1) * 512],
                                    start=(c == 0), stop=(c == 7)), reads=[b_w1b[ws], b_h2b], writes=[bpl], sig=(c == 7))
                            kb.op("dve", lambda e: e.tensor_scalar(
                                gcl[a][:], pg_[:, :], b1c[ws][:, fc:fc + 1], 7.0, ALU.add, ALU.min),
                                reads=[bpg, b_b1c[ws]], writes=[b_gcl[a]])
                            kb.op("act", lambda e: e.activation(out=sgm[a][:], in_=gcl[a][:], func=AF.Sigmoid, scale=1.702),
                                  reads=[b_gcl[a]], writes=[b_sgm[a]])
                            kb.op("dve", lambda e: e.tensor_scalar(
                                lcl[a][:], pl_[:, :], b1c[ws][:, 8 + fc:9 + fc], 7.0, ALU.add, ALU.min),
                                reads=[bpl, b_b1c[ws]], writes=[b_lcl[a]])
                            kb.op("dve", lambda e: e.tensor_scalar(lcl[a][:], lcl[a][:], -7.0, 1.0, ALU.max, ALU.add),
                                  reads=[b_lcl[a]], writes=[b_lcl[a]])
                            kb.op("pool", lambda e: e.tensor_tensor(out=gcl[a][:], in0=gcl[a][:], in1=sgm[a][:], op=ALU.mult),
                                  reads=[b_gcl[a], b_sgm[a]], writes=[b_gcl[a]])
                            if prev is not None:
                                pa, pfc = prev
                                kb.op("dve", lambda e: e.tensor_tensor(out=actT[asl][:, pfc, :], in0=gcl[pa][:], in1=lcl[pa][:], op=ALU.mult),
                                      reads=[b_gcl[pa], b_lcl[pa]], writes=[b_actT[asl]])
                            prev = (a, fc)
                            if fc == 3:
                                flush()
                                if grp == 0 and u + 1 < NU:
                                    load_weights(u + 1)
                        pa, pfc = prev
                        kb.op("dve", lambda e: e.tensor_tensor(out=actT[asl][:, pfc, :], in0=gcl[pa][:], in1=lcl[pa][:], op=ALU.mult),
                              reads=[b_gcl[pa], b_lcl[pa]], writes=[b_actT[asl]])
                        state["pend"] = (tb, ex, ws, grp, asl)
                flush()
                for tl in range(NTB):
                    tg = tb * NTB + tl
                    z = state["nx"] % 2
                    state["nx"] += 1
                    kb.dma("sp", xo[z][:], out_d[tg * 128:(tg + 1) * 128, :], b_xo[z], writes=[b_xo[z]])
                    kb.op("dve", lambda e, tl=tl: e.tensor_tensor(out=acc[:, tl, :], in0=acc[:, tl, :], in1=G2row[:], op=ALU.mult),
                          reads=[b_acc[tl], b_G2], writes=[b_acc[tl]])
                    kb.op("dve", lambda e, tl=tl, z=z: e.tensor_tensor(out=xo[z][:], in0=acc[:, tl, :], in1=xo[z][:], op=ALU.add),
                          reads=[b_acc[tl], b_xo[z]], writes=[b_xo[z]])
                    kb.dma("sp", out_d[tg * 128:(tg + 1) * 128, :], xo[z][:], b_xo[z], reads=[b_xo[z]])
            kb.barrier()
        kb.barrier()
    return nc


_CONST = {}


def _constants():
    if _CONST:
        return _CONST
    f32 = np.float32
    pos = np.arange(S)
    row = (pos // 64).astype(f32)
    col = (pos % 64).astype(f32)
    inv = (f32(10000.0) ** (-np.arange(16, dtype=f32) / f32(16))).astype(f32)
    ar = (row[:, None] * inv[None]).astype(f32)
    ac = (col[:, None] * inv[None]).astype(f32)
    cr, sr, cc, sc = np.cos(ar), np.sin(ar), np.cos(ac), np.sin(ac)
    C = np.concatenate([cr, cr, cc, cc], axis=1)
    SA = np.concatenate([-sr, -sc], axis=1)
    SB = np.concatenate([sr, sc], axis=1)
    C = np.concatenate([np.ones((LC, 64), f32), C], 0)
    SA = np.concatenate([np.zeros((LC, 32), f32), SA], 0)
    SB = np.concatenate([np.zeros((LC, 32), f32), SB], 0)

    def pm(a):
        n = a.shape[0] // 128
        return np.ascontiguousarray(a.reshape(n, 128, -1).transpose(1, 0, 2).reshape(128, -1)).astype(f32)

    _CONST["rope_c"], _CONST["rope_sa"], _CONST["rope_sb"] = pm(C), pm(SA), pm(SB)
    posf = np.arange(S, dtype=f32)
    tn = posf / f32(S)
    bands = np.linspace(1e-4, 7, 8, dtype=f32)
    ang = (f32(2.0 * math.pi / S) * posf[:, None] * bands[None]).astype(f32)
    feats = np.concatenate([tn[:, None], np.sin(ang), np.cos(ang)], axis=-1).astype(f32)
    _CONST["featsT"] = np.ascontiguousarray(feats.T)
    deltas = np.abs(np.linspace(math.log(1e-2) / 1.5, math.log(1e-2) / 0.3, 512, dtype=f32))
    _CONST["window"] = np.exp(-tn[:, None] * deltas[None]).astype(f32)
    _CONST["ident"] = np.eye(128, dtype=f32)
    k = np.arange(S, dtype=np.float64)
    t = np.arange(S, dtype=np.float64)
    th = np.pi * (2 * k[:, None] + 1) * t[None, :] / NFFT
    bf = ml_dtypes.bfloat16
    for nm, fn in (("c", np.cos), ("s", np.sin)):
        M = fn(th).astype(f32)
        a = M.reshape(NT, 128, NT, 128).transpose(0, 3, 2, 1)
        _CONST[f"dft_{nm}tk"] = np.ascontiguousarray(a).reshape(NT, 128, S).astype(bf)
        a = M.reshape(NT, 128, NT, 128).transpose(2, 1, 0, 3)
        _CONST[f"dft_{nm}kt"] = np.ascontiguousarray(a).reshape(NT, 128, S).astype(bf)
    return _CONST


def _col(v, n):
    return np.ascontiguousarray(np.asarray(v, np.float32).reshape(n, 128).T)


_PROG = {}


def make_in_maps(inp):
    cst = _constants()
    f32 = np.float32
    g = lambda k: np.asarray(inp[k], f32)
    shared = dict(cst)
    shared["w_mod"] = g("w_mod")[0]
    bm = g("b_mod")[0]
    shared["bmod_col"] = np.ascontiguousarray(bm.reshape(6, 8, 128).transpose(2, 0, 1).reshape(128, 48))
    shared["bmod_row"] = bm.reshape(1, -1)
    shared["n1g_col"] = _col(g("norm1_g")[0], 8)
    shared["n2g_col"] = _col(g("norm2_g")[0], 8)
    shared["w_in"] = g("w_in")[0]
    shared["qkg_row"] = np.concatenate([np.tile(g("q_norm_g")[0], 8), np.tile(g("k_norm_g")[0], 8)]).reshape(1, 1024)
    shared["lam_row"] = np.concatenate([g("lam_q1")[0], g("lam_k1")[0], g("lam_q2")[0], g("lam_k2")[0]]).reshape(1, 256)
    shared["subg_row"] = g("subln_g")[0].reshape(1, 128)
    shared["convw"] = g("hy_conv_w")[0].reshape(1, 4608)
    shared["convb"] = g("hy_conv_b")[0].reshape(1, 1536)
    shared["hy_w1"] = g("hy_w1")[0]
    shared["hy_cols"] = np.ascontiguousarray(np.stack([g("hy_b1")[0], g("hy_f1")[0], g("hy_b2")[0], g("hy_f2")[0]], axis=1))
    shared["hy_w2"] = g("hy_w2")[0]
    shared["hy_w3"] = g("hy_w3")[0]
    shared["hy_skip"] = g("hy_skip")[0].reshape(1, 1024)
    shared["hyg_row"] = g("hy_out_g")[0].reshape(1, 512)
    shared["w_out"] = g("w_out")[0]
    shared["router_w"] = g("router_w")[0]
    shared["router_b"] = g("router_b")[0].reshape(1, NE)
    shared["exp_w1"] = g("exp_w1")[0]
    shared["eb1_col"] = np.ascontiguousarray(g("exp_b1")[0].reshape(NE, 16, 128).transpose(0, 2, 1))
    shared["exp_w2"] = g("exp_w2")[0]
    shared["exp_b2"] = g("exp_b2")[0]
    x = g("x")
    ctx = g("ctx")
    c = g("c")
    cc = g("c_ctx")
    maps = []
    for b in range(8):
        m = dict(shared)
        m["x"] = x[b]
        m["ctx"] = ctx[b]
        cs = np.stack([_col(c[b], 8), _col(cc, 8)], axis=2).reshape(128, 16)
        m["csil"] = np.ascontiguousarray(cs)
        maps.append(m)
    return maps


def kernel(**inputs):
    if "nc" not in _PROG:
        _PROG["nc"] = build_program()
    nc = _PROG["nc"]
    maps = make_in_maps(inputs)
    res = run_bass_kernel_spmd(nc, maps, core_ids=list(range(8)))
    return np.stack([np.asarray(r["out"], np.float32) for r in res.results], axis=0)
```

```python
import math
import os
from contextlib import ExitStack
KDBG = os.environ.get('KDBG', '')

import numpy as np
import ml_dtypes
import concourse.bass as bass
import concourse.mybir as mybir
from concourse.bass_utils import run_bass_kernel_spmd

F32 = mybir.dt.float32
BF16 = mybir.dt.bfloat16
AF = mybir.ActivationFunctionType
ALU = mybir.AluOpType
AX = mybir.AxisListType

D = 1024
S = 4096
LC = 256
NKT = 34
NT = 32
EPS = 1e-6
NE = 32
LAM_INIT = 0.8 - 0.6 * math.exp(0.0)
NFFT = 8192


class Buf:
    __slots__ = ("w", "r", "sem", "dcnt", "name")

    def __init__(self, name):
        self.name = name
        self.w = None
        self.r = {}
        self.sem = None
        self.dcnt = 0


class KB:
    def __init__(self, nc, es):
        self.nc = nc
        self.es = es
        self.eng = {"pe": nc.tensor, "act": nc.scalar, "dve": nc.vector, "pool": nc.gpsimd, "sp": nc.sync}
        self.sem = {n: es.enter_context(nc.semaphore("sem_" + n)) for n in self.eng}
        self.cnt = {n: 0 for n in self.eng}
        self.waited = {n: {} for n in self.eng}
        self.dma_bufs = []

    def buf(self, name, dma=False):
        b = Buf(name)
        if dma:
            b.sem = self.es.enter_context(self.nc.semaphore("d_" + name))
            self.dma_bufs.append(b)
        return b

    def _wait(self, e, ev):
        if ev is None:
            return
        key, sem, val, src = ev
        if src == "pe" and e == "pe":
            return
        if self.waited[e].get(key, 0) >= val:
            return
        self.eng[e].wait_ge(sem, val)
        self.waited[e][key] = val

    def _deps(self, e, reads, writes):
        for b in reads:
            self._wait(e, b.w)
        for b in writes:
            self._wait(e, b.w)
            for ev in list(b.r.values()):
                self._wait(e, ev)

    def op(self, e, fn, reads=(), writes=(), sig=True):
        self._deps(e, reads, writes)
        ins = fn(self.eng[e])
        if sig:
            self.cnt[e] += 1
            ins.then_inc(self.sem[e], 1)
            val = self.cnt[e]
        else:
            val = self.cnt[e] + 1
        ev = (e, self.sem[e], val, e)
        for b in reads:
            b.r[e] = ev
        for b in writes:
            b.w = ev
            b.r = {}
        return ev

    def dma(self, e, out, in_, sb, reads=(), writes=()):
        self._deps(e, reads, writes)
        ins = self.eng[e].dma_start(out=out, in_=in_)
        sb.dcnt += 16
        ins.then_inc(sb.sem, 16)
        ev = (id(sb), sb.sem, sb.dcnt, "dma")
        for b in reads:
            b.r[id(sb)] = ev
        for b in writes:
            b.w = ev
            b.r = {}
        return ev

    def barrier(self):
        evs = [(n, self.sem[n], self.cnt[n], "x") for n in self.eng if self.cnt[n] > 0]
        evs += [(id(b), b.sem, b.dcnt, "dma") for b in self.dma_bufs if b.dcnt > 0]
        for e in self.eng:
            for ev in evs:
                if ev[0] == e:
                    continue
                self._wait(e, ev)


def build_program(stop_after="E", debug=False):
    nc = bass.Bass("TRN2", target_bir_lowering=False)
    LV = {"0": 0, "Ah": 1, "Aq": 2, "A": 3, "B": 4, "C": 5, "D": 6, "E": 7}[stop_after]

    def din(name, shape, dt=F32):
        return nc.dram_tensor(name, list(shape), dt, kind="ExternalInput").ap()

    def dscr(name, shape, dt=BF16):
        return nc.dram_tensor(name, list(shape), dt, kind=("ExternalOutput" if debug else "Internal")).ap()

    x_d = din("x", [S, D])
    ctx_d = din("ctx", [LC, D])
    csil_d = din("csil", [128, 16])
    wmod_d = din("w_mod", [D, 6 * D])
    bmodc_d = din("bmod_col", [128, 48])
    bmodr_d = din("bmod_row", [1, 6 * D])
    n1g_d = din("n1g_col", [128, 8])
    n2g_d = din("n2g_col", [128, 8])
    win_d = din("w_in", [D, 3072])
    qkg_d = din("qkg_row", [1, 1024])
    lamv_d = din("lam_row", [1, 256])
    subg_d = din("subg_row", [1, 128])
    cw_d = din("convw", [1, 4608])
    cb_d = din("convb", [1, 1536])
    hw1_d = din("hy_w1", [17, 64])
    hcol_d = din("hy_cols", [64, 4])
    hw2_d = din("hy_w2", [64, 64])
    hw3_d = din("hy_w3", [64, 2048])
    hskip_d = din("hy_skip", [1, 1024])
    hyg_d = din("hyg_row", [1, 512])
    wout_d = din("w_out", [D, D])
    rw_d = din("router_w", [D, NE])
    rb_d = din("router_b", [1, NE])
    ew1_d = din("exp_w1", [NE, D, 2 * D])
    eb1_d = din("eb1_col", [NE, 128, 16])
    ew2_d = din("exp_w2", [NE, D, D])
    eb2_d = din("exp_b2", [NE, D])
    ident_d = din("ident", [128, 128])
    ropec_d = din("rope_c", [128, NKT * 64])
    ropea_d = din("rope_sa", [128, NKT * 32])
    ropeb_d = din("rope_sb", [128, NKT * 32])
    featsT_d = din("featsT", [17, S])
    window_d = din("window", [S, 512])
    ctk_d = din("dft_ctk", [NT, 128, 4096], BF16)
    stk_d = din("dft_stk", [NT, 128, 4096], BF16)
    ckt_d = din("dft_ckt", [NT, 128, 4096], BF16)
    skt_d = din("dft_skt", [NT, 128, 4096], BF16)

    out_d = nc.dram_tensor("out", [S, D], F32, kind="ExternalOutput").ap()

    QT = dscr("QT", [4, 128, S])
    KT = dscr("KT", [4, 128, NKT * 128])
    VV = dscr("VV", [NKT * 128, 512])
    UU = dscr("UU", [S, 1536])
    AT = dscr("AT", [S, 512])
    Z2 = dscr("Z2", [S, 512])
    H2T = dscr("H2T", [8, 128, S])
    GATES = dscr("GATES", [S, NE], F32)

    with ExitStack() as es:
        kb = KB(nc, es)

        def sb(st, name, shape, dt=F32):
            return st.enter_context(nc.sbuf_tensor("sb_" + name, list(shape), dt))

        def ps(st, name, shape, dt=F32):
            return st.enter_context(nc.psum_tensor("ps_" + name, list(shape), dt))

        ident_f = sb(es, "ident_f", [128, 128]); b_identf = kb.buf("identf", dma=True)
        ident_b = sb(es, "ident_b", [128, 128], BF16); b_identb = kb.buf("identb", dma=True)
        ones_f = sb(es, "ones_f", [128, 128]); b_onesf = kb.buf("onesf")
        ones_b = sb(es, "ones_b", [128, 128], BF16); b_onesb = kb.buf("onesb")
        modcol = sb(es, "modcol", [128, 96]); b_modcol = kb.buf("modcol")
        AB = sb(es, "AB", [128, 64]); b_AB = kb.buf("AB")
        G1row = sb(es, "G1row", [128, D]); b_G1 = kb.buf("G1row")
        G2row = sb(es, "G2row", [128, D]); b_G2 = kb.buf("G2row")
        nlam = sb(es, "nlam", [128, 1]); b_nlam = kb.buf("nlam")

        kb.dma("sp", ident_f[:], ident_d, b_identf, writes=[b_identf])
        kb.dma("pool", ident_b[:], ident_d, b_identb, writes=[b_identb])
        kb.op("dve", lambda e: e.memset(ones_f[:], 1.0), writes=[b_onesf])
        kb.op("dve", lambda e: e.memset(ones_b[:], 1.0), writes=[b_onesb])

        with ExitStack() as st:
            csil = sb(st, "csil", [128, 16]); b_csil = kb.buf("csil", dma=True)
            scb = sb(st, "scb", [128, 16], BF16); b_scb = kb.buf("scb")
            bmodc = sb(st, "bmodc", [128, 48]); b_bmodc = kb.buf("bmodc", dma=True)
            bmodr = sb(st, "bmodr", [1, 6 * D]); b_bmodr = kb.buf("bmodr", dma=True)
            n1g = sb(st, "n1g", [128, 8]); b_n1g = kb.buf("n1g", dma=True)
            n2g = sb(st, "n2g", [128, 8]); b_n2g = kb.buf("n2g", dma=True)
            lamv = sb(st, "lamv", [128, 256]); b_lamv = kb.buf("lamv", dma=True)
            lamt = sb(st, "lamt", [128, 128]); b_lamt = kb.buf("lamt")
            lams = sb(st, "lams", [128, 2]); b_lams = kb.buf("lams")
            grow = sb(st, "grow", [1, 2 * D]); b_grow = kb.buf("grow")
            tmp8 = sb(st, "tmp8", [128, 16]); b_tmp8 = kb.buf("tmp8")
            wm = [sb(st, f"wm{i}", [128, 8, D], BF16) for i in range(2)]
            b_wm = [kb.buf(f"wm{i}", dma=True) for i in range(2)]
            p_col = ps(st, "p_col", [128, 512]); b_pcol = kb.buf("pcol")
            p_row = ps(st, "p_row", [128, 512]); b_prow = kb.buf("prow")
            p_bc = [ps(st, f"p_bc{i}", [128, 512]) for i in range(2)]
            b_pbc = [kb.buf(f"pbc{i}") for i in range(2)]

            kb.dma("sp", csil[:], csil_d, b_csil, writes=[b_csil])
            kb.dma("sp", bmodc[:], bmodc_d, b_bmodc, writes=[b_bmodc])
            kb.dma("sp", bmodr[:], bmodr_d, b_bmodr, writes=[b_bmodr])
            kb.dma("sp", n1g[:], n1g_d, b_n1g, writes=[b_n1g])
            kb.dma("sp", n2g[:], n2g_d, b_n2g, writes=[b_n2g])
            kb.dma("sp", lamv[:], lamv_d.broadcast_to([128, 256]), b_lamv, writes=[b_lamv])
            kb.op("act", lambda e: e.activation(out=scb[:], in_=csil[:], func=AF.Silu), reads=[b_csil], writes=[b_scb])
            scv = scb[:].rearrange("p (c w) -> p c w", w=2)
            wmod_v = wmod_d.rearrange("(c p) f -> p c f", p=128)
            for i in range(6):
                sl = i % 2
                kb.dma("pool", wm[sl][:], wmod_v[:, :, i * D:(i + 1) * D], b_wm[sl], writes=[b_wm[sl]])
                if i in (0, 1, 3, 4):
                    for fc in range(8):
                        col = (i * 8 + fc) * 2
                        for c in range(8):
                            kb.op("pe", lambda e, c=c, fc=fc, col=col: e.matmul(
                                p_col[:, col:col + 2], wm[sl][:, c, fc * 128:(fc + 1) * 128], scv[:, c, :],
                                start=(c == 0), stop=(c == 7)),
                                reads=[b_wm[sl], b_scb], writes=[b_pcol], sig=(c == 7))
                else:
                    gi = 0 if i == 2 else 1
                    for hh in range(2):
                        for c in range(8):
                            kb.op("pe", lambda e, c=c, hh=hh: e.matmul(
                                p_row[0:1, :], scv[:, c, 0:1], wm[sl][:, c, hh * 512:(hh + 1) * 512],
                                start=(c == 0), stop=(c == 7)),
                                reads=[b_wm[sl], b_scb], writes=[b_prow], sig=(c == 7))
                        kb.op("dve", lambda e, hh=hh, gi=gi, i=i: e.tensor_tensor(
                            out=grow[0:1, gi * D + hh * 512: gi * D + (hh + 1) * 512], in0=p_row[0:1, :],
                            in1=bmodr[0:1, i * D + hh * 512: i * D + (hh + 1) * 512], op=ALU.add),
                            reads=[b_prow, b_bmodr], writes=[b_grow])
            kb.op("dve", lambda e: e.memset(modcol[:, :], 0.0), writes=[b_modcol])
            for lo in (0, 24):
                kb.op("dve", lambda e, lo=lo: e.tensor_tensor(
                    out=modcol[:, 2 * lo:2 * lo + 32].rearrange("p (a w) -> p a w", w=2),
                    in0=p_col[:, 2 * lo:2 * lo + 32].rearrange("p (a w) -> p a w", w=2),
                    in1=bmodc[:, lo:lo + 16].unsqueeze(2).to_broadcast([128, 16, 2]), op=ALU.add),
                    reads=[b_pcol, b_bmodc], writes=[b_modcol])
            mc = modcol[:, :].rearrange("p (i c w) -> p i c w", i=6, c=8)
            ABv = AB[:, :].rearrange("p (j c) -> p j c", c=8)
            for j, (i, w, g) in enumerate([(1, 0, n1g), (1, 1, n1g), (None, None, None), (None, None, None), (4, 0, n2g)]):
                if i is None:
                    continue
                kb.op("dve", lambda e, i=i, w=w: e.tensor_scalar(tmp8[:, 0:8], mc[:, i, :, w], 1.0, None, ALU.add),
                      reads=[b_modcol], writes=[b_tmp8])
                kb.op("dve", lambda e, j=j, g=g: e.tensor_tensor(out=ABv[:, j, :], in0=tmp8[:, 0:8], in1=g[:, :], op=ALU.mult),
                      reads=[b_tmp8, b_n1g, b_n2g], writes=[b_AB])
            kb.op("dve", lambda e: e.tensor_copy(out=ABv[:, 2, :], in_=mc[:, 0, :, 0]), reads=[b_modcol], writes=[b_AB])
            kb.op("dve", lambda e: e.tensor_copy(out=ABv[:, 3, :], in_=mc[:, 0, :, 1]), reads=[b_modcol], writes=[b_AB])
            kb.op("dve", lambda e: e.tensor_copy(out=ABv[:, 5, :], in_=mc[:, 3, :, 0]), reads=[b_modcol], writes=[b_AB])
            for gi, (G, bG) in enumerate(((G1row, b_G1), (G2row, b_G2))):
                for hh in range(2):
                    k = hh
                    kb.op("pe", lambda e, gi=gi, hh=hh, k=k: e.matmul(
                        p_bc[k][:, :], ones_f[0:1, :], grow[0:1, gi * D + hh * 512: gi * D + (hh + 1) * 512],
                        start=True, stop=True), reads=[b_onesf, b_grow], writes=[b_pbc[k]])
                    kb.op("act", lambda e, G=G, hh=hh, k=k: e.activation(
                        out=G[:, hh * 512:(hh + 1) * 512], in_=p_bc[k][:, :], func=AF.Copy),
                        reads=[b_pbc[k]], writes=[bG])
            for j in range(2):
                kb.op("dve", lambda e, j=j: e.tensor_tensor(
                    out=lamt[:, j * 64:(j + 1) * 64], in0=lamv[:, j * 128: j * 128 + 64],
                    in1=lamv[:, j * 128 + 64: j * 128 + 128], op=ALU.mult), reads=[b_lamv], writes=[b_lamt])
            kb.op("dve", lambda e: e.tensor_reduce(out=lams[:, :], in_=lamt[:, :].rearrange("p (j d) -> p j d", d=64),
                                                   axis=AX.X, op=ALU.add), reads=[b_lamt], writes=[b_lams])
            kb.op("act", lambda e: e.activation(out=lams[:, :], in_=lams[:, :], func=AF.Exp), reads=[b_lams], writes=[b_lams])
            kb.op("dve", lambda e: e.tensor_tensor(out=nlam[:, :], in0=lams[:, 1:2], in1=lams[:, 0:1], op=ALU.subtract),
                  reads=[b_lams], writes=[b_nlam])
            kb.op("dve", lambda e: e.tensor_scalar(nlam[:, :], nlam[:, :], -LAM_INIT, None, ALU.add),
                  reads=[b_nlam], writes=[b_nlam])
            kb.barrier()

        ABv = AB[:, :].rearrange("p (j c) -> p j c", c=8)

        def rms_rstd(e_act_reads, src_ap, junk_ap, ssq_ap, rstd_ap, n, bufs_r, b_junk, b_ssq, b_rstd):
            kb.op("dve", lambda e: e.memset(ssq_ap, 0.0), writes=[b_ssq])
            kb.op("act", lambda e: e.activation(out=junk_ap, in_=src_ap, func=AF.Square, accum_out=ssq_ap),
                  reads=bufs_r, writes=[b_junk, b_ssq])
            kb.op("act", lambda e: e.activation(out=rstd_ap, in_=ssq_ap, func=AF.Sqrt, bias=EPS, scale=1.0 / n),
                  reads=[b_ssq], writes=[b_rstd])
            kb.op("dve", lambda e: e.reciprocal(out=rstd_ap, in_=rstd_ap), reads=[b_rstd], writes=[b_rstd])

        if LV >= 1:
          with ExitStack() as st:
            HW = NKT * 128 + 2
            hT = sb(st, "hT", [128, 8, HW], BF16)
            b_hTa = [kb.buf(f"hTa{i}") for i in range(NKT)]
            b_hTb = [kb.buf(f"hTb{i}") for i in range(NKT)]
            b_hpad = kb.buf("hpad")
            XOFF = LC + 1

            def hcol(i):
                return i * 128 if i < 2 else XOFF + (i - 2) * 128

            kb.op("pool", lambda e: e.memset(hT[:, :, LC:LC + 1], 0.0), writes=[b_hpad])
            kb.op("pool", lambda e: e.memset(hT[:, :, HW - 1:HW], 0.0), writes=[b_hpad])
            with ExitStack() as s2:
                xt = [sb(s2, f"xt{i}", [128, D]) for i in range(3)]
                b_xt = [kb.buf(f"xt{i}", dma=True) for i in range(3)]
                junk = sb(s2, "junkA", [128, D], BF16); b_junk = kb.buf("junkA")
                ssq = [sb(s2, f"ssqA{i}", [128, 2]) for i in range(3)]
                b_ssq = [kb.buf(f"ssqA{i}") for i in range(3)]
                xn = [sb(s2, f"xnA{i}", [128, D], BF16) for i in range(2)]
                b_xn = [kb.buf(f"xnA{i}") for i in range(2)]
                pT = [ps(s2, f"pTA{i}", [128, D], BF16) for i in range(2)]
                b_pT = [kb.buf(f"pTA{i}") for i in range(2)]
                srcs = [ctx_d[0:128, :], ctx_d[128:256, :]] + [x_d[i * 128:(i + 1) * 128, :] for i in range(NT)]
                for i in range(NKT if 't1' not in KDBG else 1):
                    s3 = i % 3
                    s2_ = i % 2
                    kb.dma("sp", xt[s3][:], srcs[i], b_xt[s3], writes=[b_xt[s3]])
                    rms_rstd(None, xt[s3][:], junk[:], ssq[s3][:, 0:1], ssq[s3][:, 1:2], D, [b_xt[s3]], b_junk, b_ssq[s3], b_ssq[s3])
                    kb.op("dve", lambda e, s3=s3, s2_=s2_: e.tensor_scalar(xn[s2_][:], xt[s3][:], ssq[s3][:, 1:2], None, ALU.mult),
                          reads=[b_xt[s3], b_ssq[s3]], writes=[b_xn[s2_]])
                    if 'notr' in KDBG:
                        continue
                    for c in range(8):
                        kb.op("pe", lambda e, c=c, s2_=s2_: e.transpose(pT[s2_][:, c * 128:(c + 1) * 128],
                                                                          xn[s2_][:, c * 128:(c + 1) * 128], ident_b[:]),
                              reads=[b_xn[s2_], b_identb], writes=[b_pT[s2_]], sig=(c == 7))
                    ja, jb = (1, 3) if i < 2 else (0, 2)
                    c0 = hcol(i)
                    if 'noevac' in KDBG:
                        continue
                    for c in range(8):
                        if False:
                            kb.op("act", lambda e, c=c, s2_=s2_, ja=ja, jb=jb, c0=c0: e.activation(
                                out=hT[:, c, c0:c0 + 128], in_=pT[s2_][:, c * 128:(c + 1) * 128], func=AF.Identity,
                                scale=ABv[:, ja, c:c + 1], bias=ABv[:, jb, c:c + 1]),
                                reads=[b_pT[s2_], b_AB], writes=[b_hTa[i]])
                        else:
                            kb.op("dve", lambda e, c=c, s2_=s2_, ja=ja, jb=jb, c0=c0: e.tensor_scalar(
                                hT[:, c, c0:c0 + 128], pT[s2_][:, c * 128:(c + 1) * 128],
                                ABv[:, ja, c:c + 1], ABv[:, jb, c:c + 1], ALU.mult, ALU.add),
                                reads=[b_pT[s2_], b_AB], writes=[b_hTb[i]])
                kb.barrier()

            if LV >= 2:
             with ExitStack() as s2:
                 wq = sb(s2, "wq", [128, 8, 1536], BF16); b_wq = kb.buf("wq", dma=True)
                 qkg = sb(s2, "qkg", [128, 1024]); b_qkg = kb.buf("qkg", dma=True)
                 ropec = sb(s2, "ropec", [128, NKT * 64]); b_ropec = kb.buf("ropec", dma=True)
                 ropea = sb(s2, "ropea", [128, NKT * 32]); b_ropea = kb.buf("ropea", dma=True)
                 ropeb = sb(s2, "ropeb", [128, NKT * 32]); b_ropeb = kb.buf("ropeb", dma=True)
                 sq_ = [sb(s2, f"sqB{i}", [128, 1024]) for i in range(2)]; b_sq_ = [kb.buf(f"sqB{i}") for i in range(2)]
                 s16 = [sb(s2, f"s16_{i}", [128, 16]) for i in range(2)]
                 b_s16 = [kb.buf(f"s16_{i}") for i in range(2)]
                 t0_ = [sb(s2, f"t0B{i}", [128, 1024]) for i in range(2)]; b_t0_ = [kb.buf(f"t0B{i}") for i in range(2)]
                 t1_ = [sb(s2, f"t1B{i}", [128, 1024]) for i in range(2)]; b_t1_ = [kb.buf(f"t1B{i}") for i in range(2)]
                 t2_ = [sb(s2, f"t2B{i}", [128, 1024]) for i in range(2)]; b_t2_ = [kb.buf(f"t2B{i}") for i in range(2)]
                 qkb = [sb(s2, f"qkb{i}", [128, 1024], BF16) for i in range(2)]
                 b_qkb = [kb.buf(f"qkb{i}") for i in range(2)]
                 qkT = [sb(s2, f"qkT{i}", [128, 1024], BF16) for i in range(2)]
                 b_qkT = [kb.buf(f"qkT{i}", dma=True) for i in range(2)]
                 vt = [sb(s2, f"vt{i}", [128, 512], BF16) for i in range(2)]
                 b_vt = [kb.buf(f"vt{i}", dma=True) for i in range(2)]
                 p_qk = [ps(s2, f"p_qk{i}", [128, 1024]) for i in range(2)]
                 b_pqk = [kb.buf(f"pqk{i}") for i in range(2)]
                 p_v = [ps(s2, f"p_v{i}", [128, 512]) for i in range(2)]
                 b_pv = [kb.buf(f"pv{i}") for i in range(2)]
                 p_t = ps(s2, "p_tB", [128, 1024], BF16); b_pt = kb.buf("ptB")

                 win_v = win_d.rearrange("(c p) f -> p c f", p=128)
                 for c in range(8):
                     kb.dma("pool", wq[:, c, :], win_v[:, c, 0:1536], b_wq, writes=[b_wq])
                 kb.dma("sp", qkg[:], qkg_d.broadcast_to([128, 1024]), b_qkg, writes=[b_qkg])
                 kb.dma("sp", ropec[:], ropec_d, b_ropec, writes=[b_ropec])
                 kb.dma("sp", ropea[:], ropea_d, b_ropea, writes=[b_ropea])
                 kb.dma("sp", ropeb[:], ropeb_d, b_ropeb, writes=[b_ropeb])

                 for i in range(NKT):
                     sl = i % 2
                     sq, t0, t1, t2 = sq_[sl], t0_[sl], t1_[sl], t2_[sl]
                     b_sq, b_t0, b_t1, b_t2 = b_sq_[sl], b_t0_[sl], b_t1_[sl], b_t2_[sl]
                     c0 = hcol(i)
                     for half in range(2):
                         for c in range(8):
                             kb.op("pe", lambda e, c=c, half=half, sl=sl, c0=c0: e.matmul(
                                 p_qk[sl][:, half * 512:(half + 1) * 512], hT[:, c, c0:c0 + 128],
                                 wq[:, c, half * 512:(half + 1) * 512], start=(c == 0), stop=(c == 7)),
                                 reads=[b_hTa[i], b_hTb[i], b_wq], writes=[b_pqk[sl]], sig=(c == 7 and half == 1))
                     for c in range(8):
                         kb.op("pe", lambda e, c=c, sl=sl, c0=c0: e.matmul(
                             p_v[sl][:, :], hT[:, c, c0:c0 + 128], wq[:, c, 1024:1536], start=(c == 0), stop=(c == 7)),
                             reads=[b_hTa[i], b_hTb[i], b_wq], writes=[b_pv[sl]], sig=(c == 7))
                     kb.op("act", lambda e, sl=sl: e.activation(out=vt[sl][:], in_=p_v[sl][:, :], func=AF.Copy),
                           reads=[b_pv[sl]], writes=[b_vt[sl]])
                     kb.dma("sp", VV[i * 128:(i + 1) * 128, :], vt[sl][:], b_vt[sl], reads=[b_vt[sl]])
                     kb.op("act", lambda e, sl=sl: e.activation(out=sq[:], in_=p_qk[sl][:, :], func=AF.Square),
                           reads=[b_pqk[sl]], writes=[b_sq])
                     kb.op("dve", lambda e, sl=sl: e.tensor_reduce(out=s16[sl][:], in_=sq[:].rearrange("p (g d) -> p g d", d=64),
                                                                   axis=AX.X, op=ALU.add), reads=[b_sq], writes=[b_s16[sl]])
                     kb.op("act", lambda e, sl=sl: e.activation(out=s16[sl][:], in_=s16[sl][:], func=AF.Sqrt, bias=EPS, scale=1.0 / 64),
                           reads=[b_s16[sl]], writes=[b_s16[sl]])
                     kb.op("dve", lambda e, sl=sl: e.reciprocal(out=s16[sl][:], in_=s16[sl][:]), reads=[b_s16[sl]], writes=[b_s16[sl]])
                     kb.op("dve", lambda e, sl=sl: e.tensor_tensor(
                         out=t0[:].rearrange("p (g d) -> p g d", d=64), in0=p_qk[sl][:, :].rearrange("p (g d) -> p g d", d=64),
                         in1=s16[sl][:].unsqueeze(2).to_broadcast([128, 16, 64]), op=ALU.mult),
                         reads=[b_pqk[sl], b_s16[sl]], writes=[b_t0])
                     kb.op("pool", lambda e: e.tensor_tensor(out=t0[:], in0=t0[:], in1=qkg[:], op=ALU.mult),
                           reads=[b_t0, b_qkg], writes=[b_t0])
                     kb.op("dve", lambda e, i=i: e.tensor_tensor(
                         out=t1[:].rearrange("p (g d) -> p g d", d=64), in0=t0[:].rearrange("p (g d) -> p g d", d=64),
                         in1=ropec[:, i * 64:(i + 1) * 64].unsqueeze(1).to_broadcast([128, 16, 64]), op=ALU.mult),
                         reads=[b_t0, b_ropec], writes=[b_t1])
                     t0v = t0[:].rearrange("p (g r h d) -> p g r h d", g=16, r=2, h=2)
                     t2v = t2[:].rearrange("p (g r h d) -> p g r h d", g=16, r=2, h=2)
                     kb.op("pool", lambda e, i=i: e.tensor_tensor(
                         out=t2v[:, :, :, 0, :], in0=t0v[:, :, :, 1, :],
                         in1=ropea[:, i * 32:(i + 1) * 32].rearrange("p (r d) -> p r d", r=2).unsqueeze(1).to_broadcast([128, 16, 2, 16]),
                         op=ALU.mult), reads=[b_t0, b_ropea], writes=[b_t2])
                     kb.op("pool", lambda e, i=i: e.tensor_tensor(
                         out=t2v[:, :, :, 1, :], in0=t0v[:, :, :, 0, :],
                         in1=ropeb[:, i * 32:(i + 1) * 32].rearrange("p (r d) -> p r d", r=2).unsqueeze(1).to_broadcast([128, 16, 2, 16]),
                         op=ALU.mult), reads=[b_t0, b_ropeb], writes=[b_t2])
                     kb.op("dve", lambda e, sl=sl: e.tensor_tensor(out=qkb[sl][:], in0=t1[:], in1=t2[:], op=ALU.add),
                           reads=[b_t1, b_t2], writes=[b_qkb[sl]])
                     for c in range(8):
                         kb.op("pe", lambda e, c=c, sl=sl: e.transpose(p_t[:, c * 128:(c + 1) * 128],
                                                                        qkb[sl][:, c * 128:(c + 1) * 128], ident_b[:]),
                               reads=[b_qkb[sl], b_identb], writes=[b_pt], sig=(c == 7))
                     kb.op("act", lambda e, sl=sl: e.activation(out=qkT[sl][:], in_=p_t[:, :], func=AF.Copy),
                           reads=[b_pt], writes=[b_qkT[sl]])
                     qv = qkT[sl][:].rearrange("p (a h t) -> p a h t", a=2, h=4)
                     if i >= 2:
                         kb.dma("sp", QT[:, :, (i - 2) * 128:(i - 1) * 128].rearrange("h p t -> p h t"), qv[:, 0, :, :],
                                b_qkT[sl], reads=[b_qkT[sl]])
                     kb.dma("sp", KT[:, :, i * 128:(i + 1) * 128].rearrange("h p t -> p h t"), qv[:, 1, :, :],
                            b_qkT[sl], reads=[b_qkT[sl]])
                 kb.barrier()

            if LV >= 3:
              with ExitStack() as s2:
                wu = [sb(s2, f"wu{j}", [128, 8, 1536], BF16) for j in range(3)]
                b_wu = kb.buf("wu")
                wst = [sb(s2, f"wst{i}", [128, 1536]) for i in range(2)]
                b_wst = [kb.buf(f"wst{i}", dma=True) for i in range(2)]
                cw = sb(s2, "cw", [128, 3 * 1536]); b_cw = kb.buf("cw", dma=True)
                cb = sb(s2, "cb", [128, 1536]); b_cb = kb.buf("cb", dma=True)
                ut = [sb(s2, f"ut{i}", [128, 1536], BF16) for i in range(2)]
                b_ut = [kb.buf(f"ut{i}", dma=True) for i in range(2)]
                p_u = [ps(s2, f"p_u{i}", [128, 512]) for i in range(6)]
                b_pu = [kb.buf(f"pu{i}") for i in range(6)]
                win_v = win_d.rearrange("(c p) f -> p c f", p=128)
                kb.dma("sp", cw[:], cw_d.broadcast_to([128, 4608]), b_cw, writes=[b_cw])
                kb.dma("sp", cb[:], cb_d.broadcast_to([128, 1536]), b_cb, writes=[b_cb])
                for c in range(8):
                    sl = c % 2
                    kb.dma("sp", wst[sl][:], win_v[:, c, 1536:3072], b_wst[sl], writes=[b_wst[sl]])
                    for j in range(3):
                        kb.op("dve" if j < 2 else "pool", lambda e, j=j, c=c, sl=sl: e.tensor_tensor(
                            out=wu[j][:, c, :], in0=wst[sl][:], in1=cw[:, j * 1536:(j + 1) * 1536], op=ALU.mult),
                            reads=[b_wst[sl], b_cw], writes=[b_wu])
                for i in range(NT):
                    sl = i % 2
                    c0 = XOFF + i * 128
                    rd = [b_hTa[i + 2], b_hTb[i + 2], b_wu, b_hpad]
                    if i > 0:
                        rd += [b_hTa[i + 1], b_hTb[i + 1]]
                    if i < NT - 1:
                        rd += [b_hTa[i + 3], b_hTb[i + 3]]
                    for g in range(3):
                        pb = sl * 3 + g
                        n = 0
                        for j in range(3):
                            for c in range(8):
                                kb.op("pe", lambda e, c=c, j=j, g=g, pb=pb, c0=c0, n=n: e.matmul(
                                    p_u[pb][:, :], hT[:, c, c0 + j - 1:c0 + j - 1 + 128], wu[j][:, c, g * 512:(g + 1) * 512],
                                    start=(n == 0), stop=(n == 23)), reads=rd, writes=[b_pu[pb]], sig=(n == 23))
                                n += 1
                        kb.op("dve", lambda e, g=g, pb=pb, sl=sl: e.tensor_tensor(
                            out=ut[sl][:, g * 512:(g + 1) * 512], in0=p_u[pb][:, :], in1=cb[:, g * 512:(g + 1) * 512], op=ALU.add),
                            reads=[b_pu[pb], b_cb], writes=[b_ut[sl]])
                    kb.dma("sp", UU[i * 128:(i + 1) * 128, :], ut[sl][:], b_ut[sl], reads=[b_ut[sl]])
                kb.barrier()

        if LV >= 4:
          with ExitStack() as st:
            qz = [[sb(st, f"qz{m}_{i}", [128, S], BF16) for i in range(2)] for m in range(2)]
            b_qz = [[kb.buf(f"qz{m}_{i}", dma=True) for i in range(2)] for m in range(2)]
            kh = [sb(st, f"kh{i}", [128, NKT * 128], BF16) for i in range(2)]
            b_kh = [kb.buf(f"kh{i}", dma=True) for i in range(2)]
            vh = [sb(st, f"vh{i}", [128, NKT, 130], BF16) for i in range(2)]
            b_vh = [kb.buf(f"vh{i}", dma=True) for i in range(2)]
            subg = sb(st, "subg", [128, 128]); b_subg = kb.buf("subg", dma=True)
            NPT = 4
            pt_ = [sb(st, f"ptile{i}", [128, 512], BF16) for i in range(NPT)]
            b_ptile = [kb.buf(f"ptile{i}") for i in range(NPT)]
            p_s = [ps(st, f"p_s{i}", [128, 512]) for i in range(2)]
            b_ps = [kb.buf(f"ps{i}") for i in range(2)]
            p_o = [ps(st, f"p_o{i}", [128, 512]) for i in range(6)]
            b_po = [kb.buf(f"po{i}") for i in range(6)]
            NZ = 2
            rq = [sb(st, f"rq{i}", [128, 16]) for i in range(NZ)]
            b_rq = [kb.buf(f"rq{i}") for i in range(NZ)]
            ta = [sb(st, f"ta{i}", [128, 128]) for i in range(2)]
            b_ta = [kb.buf(f"ta{i}") for i in range(2)]
            aa = [sb(st, f"aa{i}", [128, 512]) for i in range(NZ)]
            b_aa = [kb.buf(f"aa{i}") for i in range(NZ)]
            junkb = sb(st, "junkBB", [128, 128]); b_junkb = kb.buf("junkBB")
            ao = [sb(st, f"ao{i}", [128, 512], BF16) for i in range(2)]
            b_ao = [kb.buf(f"ao{i}", dma=True) for i in range(2)]

            kb.dma("sp", subg[:], subg_d.broadcast_to([128, 128]), b_subg, writes=[b_subg])
            for sl in range(2):
                kb.op("pool", lambda e, sl=sl: e.memset(vh[sl][:, :, 128:130], 1.0), writes=[b_vh[sl]])
                kb.op("pool", lambda e, sl=sl: e.memset(qz[0][sl][64:128, :], 0.0), writes=[b_qz[0][sl]])
                kb.op("dve", lambda e, sl=sl: e.memset(qz[1][sl][0:64, :], 0.0), writes=[b_qz[1][sl]])
            VVv = VV.rearrange("(t p) (h d) -> h p t d", p=128, h=4)
            stB = {"npt": 0, "gq": 0, "pend": None, "nta": 0}
            C0 = 1.0 - LAM_INIT

            def post_phase1(h, qc, sets, z):
                for qt in range(4):
                    o0 = (qt % 2) * 130
                    pb0 = 2 * sets[0] + qt // 2
                    pb1 = 2 * sets[1] + qt // 2
                    kb.op("dve", lambda e: e.reciprocal(out=rq[z][:, qt:qt + 1], in_=p_o[pb0][:, o0 + 128:o0 + 129]),
                          reads=[b_po[pb0]], writes=[b_rq[z]])
                    kb.op("dve", lambda e: e.reciprocal(out=rq[z][:, 4 + qt:5 + qt], in_=p_o[pb1][:, o0 + 128:o0 + 129]),
                          reads=[b_po[pb1]], writes=[b_rq[z]])
                    kb.op("dve", lambda e: e.tensor_tensor(out=rq[z][:, 4 + qt:5 + qt], in0=rq[z][:, 4 + qt:5 + qt], in1=nlam[:, 0:1], op=ALU.mult),
                          reads=[b_rq[z], b_nlam], writes=[b_rq[z]])
                    y = stB["nta"] % 2
                    stB["nta"] += 1
                    kb.op("dve", lambda e: e.tensor_scalar(ta[y][:], p_o[pb1][:, o0:o0 + 128], rq[z][:, 4 + qt:5 + qt], None, ALU.mult),
                          reads=[b_po[pb1], b_rq[z]], writes=[b_ta[y]])
                    kb.op("dve", lambda e: e.scalar_tensor_tensor(
                        out=aa[z][:, qt * 128:(qt + 1) * 128], in0=p_o[pb0][:, o0:o0 + 128], scalar=rq[z][:, qt:qt + 1], in1=ta[y][:],
                        op0=ALU.mult, op1=ALU.add), reads=[b_po[pb0], b_rq[z], b_ta[y]], writes=[b_aa[z]])
                    kb.op("dve", lambda e: e.tensor_tensor(out=junkb[:], in0=aa[z][:, qt * 128:(qt + 1) * 128],
                                                           in1=aa[z][:, qt * 128:(qt + 1) * 128], op=ALU.mult),
                          reads=[b_aa[z]], writes=[b_junkb])
                    kb.op("dve", lambda e: e.tensor_reduce(out=rq[z][:, 8 + qt:9 + qt], in_=junkb[:], axis=AX.X, op=ALU.add),
                          reads=[b_junkb], writes=[b_rq[z]])

            def post_phase2(h, qc, z):
                kb.op("act", lambda e: e.activation(out=rq[z][:, 12:16], in_=rq[z][:, 8:12], func=AF.Sqrt,
                                                    bias=EPS / (C0 * C0), scale=1.0 / (128.0 * C0 * C0)),
                      reads=[b_rq[z]], writes=[b_rq[z]])
                kb.op("dve", lambda e: e.reciprocal(out=rq[z][:, 12:16], in_=rq[z][:, 12:16]), reads=[b_rq[z]], writes=[b_rq[z]])
                y = z % 2
                for qt in range(4):
                    kb.op("dve", lambda e: e.scalar_tensor_tensor(
                        out=ao[y][:, qt * 128:(qt + 1) * 128], in0=aa[z][:, qt * 128:(qt + 1) * 128], scalar=rq[z][:, 12 + qt:13 + qt],
                        in1=subg[:], op0=ALU.mult, op1=ALU.mult), reads=[b_aa[z], b_rq[z], b_subg], writes=[b_ao[y]])
                for qt in range(4):
                    tok0 = qc * 512 + qt * 128
                    kb.dma("sp", AT[tok0:tok0 + 128, h * 128:(h + 1) * 128], ao[y][:, qt * 128:(qt + 1) * 128], b_ao[y], reads=[b_ao[y]])

            def flushB():
                if stB["pend"] is not None:
                    post_phase2(*stB["pend"])
                    stB["pend"] = None

            for h in range(4):
                sl = h % 2
                kb.dma("sp", qz[0][sl][0:64, :], QT[h][0:64, :], b_qz[0][sl], writes=[b_qz[0][sl]])
                kb.dma("sp", qz[1][sl][64:128, :], QT[h][64:128, :], b_qz[1][sl], writes=[b_qz[1][sl]])
                kb.dma("sp", kh[sl][:], KT[h], b_kh[sl], writes=[b_kh[sl]])
                kb.dma("sp", vh[sl][:, :, 0:128], VVv[h], b_vh[sl], writes=[b_vh[sl]])
                for qc in range(8):
                    gq = stB["gq"]
                    stB["gq"] += 1
                    sets = [(2 * gq + m) % 3 for m in range(2)]
                    for m in range(2):
                        sset = sets[m]

                        def qk(kt):
                            a_ = kt % 2
                            kb.op("pe", lambda e: e.matmul(
                                p_s[a_][:, :], kh[sl][:, kt * 128:(kt + 1) * 128],
                                qz[m][sl][:, qc * 512:(qc + 1) * 512], start=True, stop=True),
                                reads=[b_kh[sl], b_qz[m][sl]], writes=[b_ps[a_]])
                        qk(0)
                        for kt in range(NKT):
                            a = kt % 2
                            b = stB["npt"] % NPT
                            stB["npt"] += 1
                            kb.op("act", lambda e: e.activation(out=pt_[b][:], in_=p_s[a][:, :], func=AF.Exp, scale=0.125),
                                  reads=[b_ps[a]], writes=[b_ptile[b]])
                            if kt + 1 < NKT:
                                qk(kt + 1)
                            for qt in range(4):
                                pb = 2 * sset + qt // 2
                                o0 = (qt % 2) * 130
                                kb.op("pe", lambda e: e.matmul(
                                    p_o[pb][:, o0:o0 + 129], pt_[b][:, qt * 128:(qt + 1) * 128], vh[sl][:, kt, 0:129],
                                    start=(kt == 0 and qt % 2 == 0), stop=(kt == NKT - 1)),
                                    reads=[b_ptile[b], b_vh[sl]], writes=[b_po[pb]], sig=(kt == NKT - 1 and qt % 2 == 1) or qt == 3)
                            if kt == 6 and m == 0:
                                flushB()
                    z = gq % NZ
                    post_phase1(h, qc, sets, z)
                    stB["pend"] = (h, qc, z)
            flushB()
            kb.barrier()

        if LV >= 5:
          with ExitStack() as st:
            h2T = sb(st, "h2T_f", [64, S]); b_h2T = kb.buf("h2Tf")
            w3pm = sb(st, "w3pm", [64, 2048]); b_w3pm = kb.buf("w3pm")
            skipb = sb(st, "skipb", [128, 1024]); b_skipb = kb.buf("skipb", dma=True)
            with ExitStack() as s2:
                featsT = sb(s2, "featsT", [17, S]); b_feats = kb.buf("feats", dma=True)
                hw1 = sb(s2, "hw1", [17, 64]); b_hw1 = kb.buf("hw1", dma=True)
                hw2 = sb(s2, "hw2", [64, 64]); b_hw2 = kb.buf("hw2", dma=True)
                hw3 = sb(s2, "hw3", [64, 2048]); b_hw3 = kb.buf("hw3", dma=True)
                hcol_ = sb(s2, "hcols", [64, 4]); b_hcols = kb.buf("hcols", dma=True)
                fb = sb(s2, "fb", [64, 2]); b_fb = kb.buf("fb")
                negpi = sb(s2, "negpi", [64, 1]); b_negpi = kb.buf("negpi")
                h1T = sb(s2, "h1T_f", [64, S]); b_h1T = kb.buf("h1Tf")
                tmpm = [sb(s2, f"tmpm{i}", [64, 512]) for i in range(2)]
                b_tmpm = [kb.buf(f"tmpm{i}") for i in range(2)]
                wrp = [sb(s2, f"wrp{i}", [64, 512]) for i in range(2)]
                b_wrp = [kb.buf(f"wrp{i}") for i in range(2)]
                p_m = [ps(s2, f"p_m{i}", [128, 512]) for i in range(2)]
                b_pm = [kb.buf(f"pm{i}") for i in range(2)]
                kb.dma("sp", featsT[:], featsT_d, b_feats, writes=[b_feats])
                kb.dma("sp", hw1[:], hw1_d, b_hw1, writes=[b_hw1])
                kb.dma("sp", hw2[:], hw2_d, b_hw2, writes=[b_hw2])
                kb.dma("sp", hw3[:], hw3_d, b_hw3, writes=[b_hw3])
                kb.dma("sp", hcol_[:], hcol_d, b_hcols, writes=[b_hcols])
                kb.dma("sp", skipb[:], hskip_d.broadcast_to([128, 1024]), b_skipb, writes=[b_skipb])
                kb.op("dve", lambda e: e.memset(negpi[:], -math.pi), writes=[b_negpi])
                OFFS = 0.0
                for l in range(2):
                    kb.op("dve", lambda e, l=l: e.tensor_tensor(out=fb[:, l:l + 1], in0=hcol_[:, 2 * l:2 * l + 1],
                                                                in1=hcol_[:, 2 * l + 1:2 * l + 2], op=ALU.mult),
                          reads=[b_hcols], writes=[b_fb])
                    kb.op("dve", lambda e, l=l: e.tensor_scalar(fb[:, l:l + 1], fb[:, l:l + 1], OFFS, None, ALU.add),
                          reads=[b_fb], writes=[b_fb])
                for l in range(2):
                    src, b_src = (featsT, b_feats) if l == 0 else (h1T, b_h1T)
                    dst, b_dst = (h1T, b_h1T) if l == 0 else (h2T, b_h2T)
                    wl, b_wl = (hw1, b_hw1) if l == 0 else (hw2, b_hw2)
                    kdim = 17 if l == 0 else 64
                    for tcn in range(8):
                        a = tcn % 2
                        kb.op("pe", lambda e, a=a, tcn=tcn, src=src, wl=wl, kdim=kdim: e.matmul(
                            p_m[a][0:64, :], wl[0:kdim, :], src[0:kdim, tcn * 512:(tcn + 1) * 512], start=True, stop=True),
                            reads=[b_src, b_wl], writes=[b_pm[a]])
                        kb.op("dve", lambda e, a=a, l=l: e.tensor_scalar(
                            tmpm[a][:], p_m[a][0:64, :], hcol_[:, 2 * l + 1:2 * l + 2], fb[:, l:l + 1], ALU.mult, ALU.add),
                            reads=[b_pm[a], b_hcols, b_fb], writes=[b_tmpm[a]])
                        for (thr, per, cmp_) in ((-math.pi, 2.0 * math.pi, ALU.is_lt), (math.pi, -2.0 * math.pi, ALU.is_gt)):
                            kb.op("dve", lambda e, a=a, thr=thr, per=per, cmp_=cmp_: e.tensor_scalar(wrp[a][:], tmpm[a][:], thr, per, cmp_, ALU.mult),
                                  reads=[b_tmpm[a]], writes=[b_wrp[a]])
                            kb.op("dve", lambda e, a=a: e.tensor_tensor(out=tmpm[a][:], in0=tmpm[a][:], in1=wrp[a][:], op=ALU.add),
                                  reads=[b_tmpm[a], b_wrp[a]], writes=[b_tmpm[a]])
                        kb.op("act", lambda e, a=a, tcn=tcn, dst=dst: e.activation(
                            out=dst[:, tcn * 512:(tcn + 1) * 512], in_=tmpm[a][:], func=AF.Sin),
                            reads=[b_tmpm[a]], writes=[b_dst])
                w3v = hw3[:, :].rearrange("j (n r c) -> j n r c", n=2, r=2)
                w3o = w3pm[:, :].rearrange("j (n r c) -> j n r c", n=2, r=2)
                for n_ in range(2):
                    kb.op("dve", lambda e, n_=n_: e.tensor_tensor(out=w3o[:, n_, 0, :], in0=w3v[:, n_, 0, :], in1=w3v[:, n_, 1, :], op=ALU.add),
                          reads=[b_hw3], writes=[b_w3pm])
                    kb.op("dve", lambda e, n_=n_: e.tensor_tensor(out=w3o[:, n_, 1, :], in0=w3v[:, n_, 0, :], in1=w3v[:, n_, 1, :], op=ALU.subtract),
                          reads=[b_hw3], writes=[b_w3pm])
                kb.barrier()
            w3o = w3pm[:, :].rearrange("j (n r c) -> j n r c", n=2, r=2)

            HC = 256
            vz = sb(st, "vz", [128, NT, HC], BF16); b_vz = kb.buf("vz", dma=True)
            gp = sb(st, "gp", [128, NT, HC], BF16); b_gp = kb.buf("gp")
            gm = sb(st, "gm", [128, NT, HC], BF16); b_gm = kb.buf("gm")
            Yr = sb(st, "Yr", [128, NT, HC], BF16); b_Yr = kb.buf("Yr")
            Yn = sb(st, "Yn", [128, NT, HC], BF16); b_Yn = kb.buf("Yn")
            tabC = [sb(st, f"tabC{i}", [128, NT, 128], BF16) for i in range(2)]
            b_tabC = [kb.buf(f"tabC{i}", dma=True) for i in range(2)]
            tabS = [sb(st, f"tabS{i}", [128, NT, 128], BF16) for i in range(2)]
            b_tabS = [kb.buf(f"tabS{i}", dma=True) for i in range(2)]
            wnd = [sb(st, f"wnd{i}", [128, HC]) for i in range(2)]
            b_wnd = [kb.buf(f"wnd{i}", dma=True) for i in range(2)]
            xg = [sb(st, f"xg{i}", [128, HC], BF16) for i in range(2)]
            b_xg = [kb.buf(f"xg{i}", dma=True) for i in range(2)]
            zo = [sb(st, f"zo{i}", [128, HC], BF16) for i in range(2)]
            b_zo = [kb.buf(f"zo{i}", dma=True) for i in range(2)]
            Hr = sb(st, "Hr", [128, HC]); b_Hr = kb.buf("Hr")
            Hs = sb(st, "Hs", [128, HC]); b_Hs = kb.buf("Hs")
            tt = [sb(st, f"ttC{i}", [128, HC]) for i in range(4)]
            b_tt = [kb.buf(f"ttC{i}") for i in range(4)]
            p_g = [ps(st, f"p_g{i}", [128, 512]) for i in range(2)]
            b_pg = [kb.buf(f"pgC{i}") for i in range(2)]
            p_x = [ps(st, f"p_x{i}", [128, 512]) for i in range(4)]
            b_px = [kb.buf(f"pxC{i}") for i in range(4)]
            p_y = [ps(st, f"p_y{i}", [128, 512]) for i in range(2)]
            b_py = [kb.buf(f"pyC{i}") for i in range(2)]
            ntab = 0
            UUv = UU.rearrange("(t p) f -> p t f", p=128)
            windv = window_d.rearrange("(t p) c -> t p c", p=128)
            for half in range(2):
                hc0 = half * HC
                kb.dma("sp", vz[:], UUv[:, :, hc0:hc0 + HC], b_vz, writes=[b_vz])
                for order in range(2):
                    for tt_ in range(NT):
                        a = tt_ % 2
                        kb.dma("sp", wnd[a][:], windv[tt_][:, hc0:hc0 + HC], b_wnd[a], writes=[b_wnd[a]])
                        for r in range(2):
                            kb.op("pe", lambda e, a=a, r=r, tt_=tt_, order=order, hc0=hc0: e.matmul(
                                p_g[a][:, r * HC:(r + 1) * HC], h2T[:, tt_ * 128:(tt_ + 1) * 128], w3o[:, order, r, hc0:hc0 + HC],
                                start=True, stop=True), reads=[b_h2T, b_w3pm], writes=[b_pg[a]], sig=(r == 1))
                        kb.op("dve", lambda e, a=a, tt_=tt_: e.tensor_tensor(out=gp[:, tt_, :], in0=p_g[a][:, 0:HC], in1=wnd[a][:], op=ALU.mult),
                              reads=[b_pg[a], b_wnd[a]], writes=[b_gp])
                        kb.op("dve", lambda e, a=a, tt_=tt_: e.tensor_tensor(out=gm[:, tt_, :], in0=p_g[a][:, HC:2 * HC], in1=wnd[a][:], op=ALU.mult),
                              reads=[b_pg[a], b_wnd[a]], writes=[b_gm])
                    for kt in range(NT):
                        a = ntab % 2
                        ntab += 1
                        kb.dma("sp", tabC[a][:].rearrange("p c k -> p (c k)"), ctk_d[kt], b_tabC[a], writes=[b_tabC[a]])
                        kb.dma("sp", tabS[a][:].rearrange("p c k -> p (c k)"), stk_d[kt], b_tabS[a], writes=[b_tabS[a]])
                        pc, psn = p_x[2 * (kt % 2)], p_x[2 * (kt % 2) + 1]
                        bpc, bps = b_px[2 * (kt % 2)], b_px[2 * (kt % 2) + 1]
                        for c in range(NT):
                            fl = dict(start=(c == 0), stop=(c == NT - 1))
                            kb.op("pe", lambda e, c=c, a=a, pc=pc, fl=fl: e.matmul(pc[:, 0:HC], tabC[a][:, c, :], vz[:, c, :], **fl),
                                  reads=[b_tabC[a], b_vz], writes=[bpc], sig=False)
                            fl = dict(start=False, stop=(c == NT - 1))
                            kb.op("pe", lambda e, c=c, a=a, pc=pc, fl=fl: e.matmul(pc[:, HC:2 * HC], tabC[a][:, c, :], gp[:, c, :], **fl),
                                  reads=[b_tabC[a], b_gp], writes=[bpc], sig=(c == NT - 1))
                            fl = dict(start=(c == 0), stop=(c == NT - 1))
                            kb.op("pe", lambda e, c=c, a=a, psn=psn, fl=fl: e.matmul(psn[:, 0:HC], tabS[a][:, c, :], vz[:, c, :], **fl),
                                  reads=[b_tabS[a], b_vz], writes=[bps], sig=False)
                            fl = dict(start=False, stop=(c == NT - 1))
                            kb.op("pe", lambda e, c=c, a=a, psn=psn, fl=fl: e.matmul(psn[:, HC:2 * HC], tabS[a][:, c, :], gm[:, c, :], **fl),
                                  reads=[b_tabS[a], b_gm], writes=[bps], sig=(c == NT - 1))
                        sk0 = order * 512 + hc0
                        kb.op("dve", lambda e, pc=pc, sk0=sk0: e.tensor_tensor(out=Hr[:], in0=pc[:, HC:2 * HC], in1=skipb[:, sk0:sk0 + HC], op=ALU.add),
                              reads=[bpc, b_skipb], writes=[b_Hr])
                        kb.op("act", lambda e, psn=psn: e.activation(out=Hs[:], in_=psn[:, HC:2 * HC], func=AF.Copy),
                              reads=[bps], writes=[b_Hs])
                        kb.op("dve", lambda e, pc=pc: e.tensor_tensor(out=tt[0][:], in0=pc[:, 0:HC], in1=Hr[:], op=ALU.mult),
                              reads=[bpc, b_Hr], writes=[b_tt[0]])
                        kb.op("dve", lambda e, psn=psn: e.tensor_tensor(out=tt[1][:], in0=psn[:, 0:HC], in1=Hs[:], op=ALU.mult),
                              reads=[bps, b_Hs], writes=[b_tt[1]])
                        kb.op("pool", lambda e, kt=kt: e.tensor_tensor(out=Yr[:, kt, :], in0=tt[0][:], in1=tt[1][:], op=ALU.subtract),
                              reads=[b_tt[0], b_tt[1]], writes=[b_Yr])
                        kb.op("dve", lambda e, pc=pc: e.tensor_tensor(out=tt[2][:], in0=pc[:, 0:HC], in1=Hs[:], op=ALU.mult),
                              reads=[bpc, b_Hs], writes=[b_tt[2]])
                        kb.op("dve", lambda e, psn=psn: e.tensor_tensor(out=tt[3][:], in0=psn[:, 0:HC], in1=Hr[:], op=ALU.mult),
                              reads=[bps, b_Hr], writes=[b_tt[3]])
                        kb.op("pool", lambda e, kt=kt: e.tensor_tensor(out=Yn[:, kt, :], in0=tt[2][:], in1=tt[3][:], op=ALU.add),
                              reads=[b_tt[2], b_tt[3]], writes=[b_Yn])
                    xoff = 512 * (order + 1) + hc0
                    for t_ in range(NT):
                        a = ntab % 2
                        ntab += 1
                        kb.dma("sp", tabC[a][:].rearrange("p c k -> p (c k)"), ckt_d[t_], b_tabC[a], writes=[b_tabC[a]])
                        kb.dma("sp", tabS[a][:].rearrange("p c k -> p (c k)"), skt_d[t_], b_tabS[a], writes=[b_tabS[a]])
                        z = t_ % 2
                        kb.dma("sp", xg[z][:], UU[t_ * 128:(t_ + 1) * 128, xoff:xoff + HC], b_xg[z], writes=[b_xg[z]])
                        for c in range(NT):
                            kb.op("pe", lambda e, c=c, a=a, z=z: e.matmul(p_y[z][:, 0:HC], tabC[a][:, c, :], Yr[:, c, :], start=(c == 0), stop=False),
                                  reads=[b_tabC[a], b_Yr], writes=[b_py[z]], sig=False)
                            kb.op("pe", lambda e, c=c, a=a, z=z: e.matmul(p_y[z][:, 0:HC], tabS[a][:, c, :], Yn[:, c, :], start=False, stop=(c == NT - 1)),
                                  reads=[b_tabS[a], b_Yn], writes=[b_py[z]], sig=(c == NT - 1))
                        if order == 0:
                            kb.op("dve", lambda e, z=z, t_=t_: e.scalar_tensor_tensor(
                                out=vz[:, t_, :], in0=p_y[z][:, 0:HC], scalar=2.0 / NFFT, in1=xg[z][:], op0=ALU.mult, op1=ALU.mult),
                                reads=[b_py[z], b_xg[z]], writes=[b_vz])
                        else:
                            kb.op("dve", lambda e, z=z: e.scalar_tensor_tensor(
                                out=zo[z][:], in0=p_y[z][:, 0:HC], scalar=2.0 / NFFT, in1=xg[z][:], op0=ALU.mult, op1=ALU.mult),
                                reads=[b_py[z], b_xg[z]], writes=[b_zo[z]])
                            kb.dma("sp", Z2[t_ * 128:(t_ + 1) * 128, hc0:hc0 + HC], zo[z][:], b_zo[z], reads=[b_zo[z]])
            kb.barrier()

        if LV >= 6:
          with ExitStack() as st:
            wo = sb(st, "wo", [128, 8, D], BF16); b_wo = kb.buf("wo")
            wost = [sb(st, f"wost{i}", [128, D]) for i in range(2)]
            b_wost = [kb.buf(f"wost{i}", dma=True) for i in range(2)]
            rw = sb(st, "rw", [128, 8, NE], BF16); b_rw = kb.buf("rw", dma=True)
            rbb = sb(st, "rbb", [128, NE]); b_rbb = kb.buf("rbb", dma=True)
            hyg = sb(st, "hyg", [128, 512]); b_hyg = kb.buf("hyg", dma=True)
            cat = [sb(st, f"cat{i}", [128, D], BF16) for i in range(2)]
            b_cat = [kb.buf(f"cat{i}", dma=True) for i in range(2)]
            z2t = [sb(st, f"z2t{i}", [128, 512], BF16) for i in range(2)]
            b_z2t = [kb.buf(f"z2t{i}", dma=True) for i in range(2)]
            xt = [sb(st, f"xtD{i}", [128, D]) for i in range(2)]
            b_xt = [kb.buf(f"xtD{i}", dma=True) for i in range(2)]
            xnw = [sb(st, f"xnw{i}", [128, D]) for i in range(2)]
            b_xnw = [kb.buf(f"xnw{i}", dma=True) for i in range(2)]
            junk = sb(st, "junkD", [128, D], BF16); b_junk = kb.buf("junkD")
            sD = [sb(st, f"sD{i}", [128, 4]) for i in range(2)]
            b_sD = [kb.buf(f"sD{i}") for i in range(2)]
            catT = [sb(st, f"catT{i}", [128, D], BF16) for i in range(2)]
            b_catT = [kb.buf(f"catT{i}") for i in range(2)]
            xn2 = [sb(st, f"xn2{i}", [128, D], BF16) for i in range(2)]
            b_xn2 = [kb.buf(f"xn2{i}") for i in range(2)]
            h2t = [sb(st, f"h2t{i}", [128, D], BF16) for i in range(2)]
            b_h2t = [kb.buf(f"h2t{i}", dma=True) for i in range(2)]
            lg = [sb(st, f"lg{i}", [128, NE]) for i in range(2)]
            b_lg = [kb.buf(f"lg{i}") for i in range(2)]
            m8 = [sb(st, f"m8{i}", [128, 8]) for i in range(2)]
            b_m8 = [kb.buf(f"m8{i}") for i in range(2)]
            msk = [sb(st, f"msk{i}", [128, NE]) for i in range(2)]
            b_msk = [kb.buf(f"msk{i}") for i in range(2)]
            gt = [sb(st, f"gt{i}", [128, NE]) for i in range(2)]
            b_gt = [kb.buf(f"gt{i}", dma=True) for i in range(2)]
            p_t = [ps(st, f"p_tD{i}", [128, D], BF16) for i in range(2)]
            b_pt = [kb.buf(f"ptD{i}") for i in range(2)]
            p_m = [ps(st, f"p_mD{i}", [128, D]) for i in range(2)]
            b_pmx = [kb.buf(f"pmD{i}") for i in range(2)]
            p_l = ps(st, "p_lD", [128, 512]); b_pl = kb.buf("plD")

            wout_v = wout_d.rearrange("(c p) f -> p c f", p=128)
            for c in range(8):
                sl = c % 2
                kb.dma("sp", wost[sl][:], wout_v[:, c, :], b_wost[sl], writes=[b_wost[sl]])
                kb.op("dve", lambda e, c=c, sl=sl: e.tensor_tensor(out=wo[:, c, :], in0=wost[sl][:], in1=G1row[:], op=ALU.mult),
                      reads=[b_wost[sl], b_G1], writes=[b_wo])
            kb.dma("pool", rw[:], rw_d.rearrange("(c p) e -> p c e", p=128), b_rw, writes=[b_rw])
            kb.dma("sp", rbb[:], rb_d.broadcast_to([128, NE]), b_rbb, writes=[b_rbb])
            kb.dma("sp", hyg[:], hyg_d.broadcast_to([128, 512]), b_hyg, writes=[b_hyg])
            for i in range(NT):
                sl = i % 2
                r0, r1 = i * 128, (i + 1) * 128
                kb.dma("sp", cat[sl][:, 0:512], AT[r0:r1, :], b_cat[sl], writes=[b_cat[sl]])
                kb.dma("sp", z2t[sl][:], Z2[r0:r1, :], b_z2t[sl], writes=[b_z2t[sl]])
                kb.dma("sp", xt[sl][:], x_d[r0:r1, :], b_xt[sl], writes=[b_xt[sl]])
                rms_rstd(None, z2t[sl][:], junk[:, 0:512], sD[sl][:, 0:1], sD[sl][:, 1:2], 512, [b_z2t[sl]], b_junk, b_sD[sl], b_sD[sl])
                kb.op("dve", lambda e, sl=sl: e.scalar_tensor_tensor(
                    out=cat[sl][:, 512:1024], in0=z2t[sl][:], scalar=sD[sl][:, 1:2], in1=hyg[:], op0=ALU.mult, op1=ALU.mult),
                    reads=[b_z2t[sl], b_sD[sl], b_hyg], writes=[b_cat[sl]])
                for c in range(8):
                    kb.op("pe", lambda e, c=c, sl=sl: e.transpose(p_t[0][:, c * 128:(c + 1) * 128], cat[sl][:, c * 128:(c + 1) * 128], ident_b[:]),
                          reads=[b_cat[sl], b_identb], writes=[b_pt[0]], sig=(c == 7))
                kb.op("act", lambda e, sl=sl: e.activation(out=catT[sl][:], in_=p_t[0][:, :], func=AF.Copy),
                      reads=[b_pt[0]], writes=[b_catT[sl]])
                for hh in range(2):
                    for c in range(8):
                        kb.op("pe", lambda e, c=c, hh=hh, sl=sl: e.matmul(
                            p_m[sl][:, hh * 512:(hh + 1) * 512], catT[sl][:, c * 128:(c + 1) * 128], wo[:, c, hh * 512:(hh + 1) * 512],
                            start=(c == 0), stop=(c == 7)), reads=[b_catT[sl], b_wo], writes=[b_pmx[sl]], sig=(c == 7 and hh == 1))
                kb.op("dve", lambda e, sl=sl: e.tensor_tensor(out=xnw[sl][:], in0=p_m[sl][:, :], in1=xt[sl][:], op=ALU.add),
                      reads=[b_pmx[sl], b_xt[sl]], writes=[b_xnw[sl]])
                kb.dma("sp", out_d[r0:r1, :], xnw[sl][:], b_xnw[sl], reads=[b_xnw[sl]])
                rms_rstd(None, xnw[sl][:], junk[:], sD[sl][:, 2:3], sD[sl][:, 3:4], D, [b_xnw[sl]], b_junk, b_sD[sl], b_sD[sl])
                kb.op("dve", lambda e, sl=sl: e.tensor_scalar(xn2[sl][:], xnw[sl][:], sD[sl][:, 3:4], None, ALU.mult),
                      reads=[b_xnw[sl], b_sD[sl]], writes=[b_xn2[sl]])
                for c in range(8):
                    kb.op("pe", lambda e, c=c, sl=sl: e.transpose(p_t[1][:, c * 128:(c + 1) * 128], xn2[sl][:, c * 128:(c + 1) * 128], ident_b[:]),
                          reads=[b_xn2[sl], b_identb], writes=[b_pt[1]], sig=(c == 7))
                for c in range(8):
                    if False:
                        kb.op("act", lambda e, c=c, sl=sl: e.activation(
                            out=h2t[sl][:, c * 128:(c + 1) * 128], in_=p_t[1][:, c * 128:(c + 1) * 128], func=AF.Identity,
                            scale=ABv[:, 4, c:c + 1], bias=ABv[:, 5, c:c + 1]), reads=[b_pt[1], b_AB], writes=[b_h2t[sl]])
                    else:
                        kb.op("dve", lambda e, c=c, sl=sl: e.tensor_scalar(
                            h2t[sl][:, c * 128:(c + 1) * 128], p_t[1][:, c * 128:(c + 1) * 128],
                            ABv[:, 4, c:c + 1], ABv[:, 5, c:c + 1], ALU.mult, ALU.add), reads=[b_pt[1], b_AB], writes=[b_h2t[sl]])
                kb.dma("sp", H2T[:, :, r0:r1].rearrange("c p t -> p c t"), h2t[sl][:].rearrange("p (c t) -> p c t", c=8),
                       b_h2t[sl], reads=[b_h2t[sl]])
                for c in range(8):
                    kb.op("pe", lambda e, c=c, sl=sl: e.matmul(p_l[:, 0:NE], h2t[sl][:, c * 128:(c + 1) * 128], rw[:, c, :],
                                                               start=(c == 0), stop=(c == 7)),
                          reads=[b_h2t[sl], b_rw], writes=[b_pl], sig=(c == 7))
                kb.op("dve", lambda e, sl=sl: e.tensor_tensor(out=lg[sl][:], in0=p_l[:, 0:NE], in1=rbb[:], op=ALU.add),
                      reads=[b_pl, b_rbb], writes=[b_lg[sl]])
                kb.op("dve", lambda e, sl=sl: e.max(out=m8[sl][:], in_=lg[sl][:]), reads=[b_lg[sl]], writes=[b_m8[sl]])
                kb.op("dve", lambda e, sl=sl: e.tensor_scalar(msk[sl][:], lg[sl][:], m8[sl][:, 3:4], None, ALU.is_ge),
                      reads=[b_lg[sl], b_m8[sl]], writes=[b_msk[sl]])
                kb.op("dve", lambda e, sl=sl: e.tensor_scalar(lg[sl][:], lg[sl][:], m8[sl][:, 0:1], None, ALU.subtract),
                      reads=[b_lg[sl], b_m8[sl]], writes=[b_lg[sl]])
                kb.op("act", lambda e, sl=sl: e.activation(out=lg[sl][:], in_=lg[sl][:], func=AF.Exp), reads=[b_lg[sl]], writes=[b_lg[sl]])
                kb.op("dve", lambda e, sl=sl: e.tensor_tensor(out=lg[sl][:], in0=lg[sl][:], in1=msk[sl][:], op=ALU.mult),
                      reads=[b_lg[sl], b_msk[sl]], writes=[b_lg[sl]])
                kb.op("dve", lambda e, sl=sl: e.tensor_reduce(out=m8[sl][:, 4:5], in_=lg[sl][:], axis=AX.X, op=ALU.add),
                      reads=[b_lg[sl]], writes=[b_m8[sl]])
                kb.op("dve", lambda e, sl=sl: e.reciprocal(out=m8[sl][:, 4:5], in_=m8[sl][:, 4:5]), reads=[b_m8[sl]], writes=[b_m8[sl]])
                kb.op("dve", lambda e, sl=sl: e.tensor_scalar(gt[sl][:], lg[sl][:], m8[sl][:, 4:5], None, ALU.mult),
                      reads=[b_lg[sl], b_m8[sl]], writes=[b_gt[sl]])
                kb.dma("sp", GATES[r0:r1, :], gt[sl][:], b_gt[sl], reads=[b_gt[sl]])
            kb.barrier()

        if LV >= 7:
          with ExitStack() as st:
            TB = 1024
            NTB = TB // 128
            NS = 3
            gates = sb(st, "gatesE", [128, NT, NE]); b_gates = kb.buf("gatesE", dma=True)
            h2b = sb(st, "h2b", [128, 8, TB], BF16); b_h2b = kb.buf("h2b", dma=True)
            acc = sb(st, "acc", [128, NTB, D]); b_acc = [kb.buf(f"acc{i}") for i in range(NTB)]
            w1b = [sb(st, f"w1b{i}", [128, 8, 2 * D], BF16) for i in range(2)]
            b_w1b = [kb.buf(f"w1b{i}", dma=True) for i in range(2)]
            w2b = [sb(st, f"w2b{i}", [128, 8, D], BF16) for i in range(2)]
            b_w2b = [kb.buf(f"w2b{i}", dma=True) for i in range(2)]
            b1c = [sb(st, f"b1c{i}", [128, 16]) for i in range(2)]
            b_b1c = [kb.buf(f"b1c{i}", dma=True) for i in range(2)]
            b2r = [sb(st, f"b2r{i}", [1, D], BF16) for i in range(2)]
            b_b2r = [kb.buf(f"b2r{i}", dma=True) for i in range(2)]
            actT = [sb(st, f"actT{i}", [128, 8, 512], BF16) for i in range(2)]
            b_actT = [kb.buf(f"actT{i}") for i in range(2)]
            gcl = [sb(st, f"gcl{i}", [128, 512]) for i in range(NS)]
            b_gcl = [kb.buf(f"gcl{i}") for i in range(NS)]
            sgm = [sb(st, f"sgm{i}", [128, 512]) for i in range(NS)]
            b_sgm = [kb.buf(f"sgm{i}") for i in range(NS)]
            lcl = [sb(st, f"lcl{i}", [128, 512]) for i in range(NS)]
            b_lcl = [kb.buf(f"lcl{i}") for i in range(NS)]
            xo = [sb(st, f"xoE{i}", [128, D]) for i in range(2)]
            b_xo = [kb.buf(f"xoE{i}", dma=True) for i in range(2)]
            p_gl = [ps(st, f"p_gl{i}", [128, 512]) for i in range(2 * NS)]
            b_pgl = [kb.buf(f"pgl{i}") for i in range(2 * NS)]
            p_o2 = [ps(st, f"p_o2{i}", [128, 512]) for i in range(2)]
            b_po2 = [kb.buf(f"po2{i}") for i in range(2)]

            kb.dma("sp", gates[:], GATES.rearrange("(t p) e -> p t e", p=128), b_gates, writes=[b_gates])
            b2all = sb(st, "b2all", [NE, D], BF16); b_b2all = kb.buf("b2all", dma=True)
            gT = sb(st, "gT", [NE, 128], BF16); b_gT = kb.buf("gT")
            kb.dma("pool", b2all[:], eb2_d, b_b2all, writes=[b_b2all])
            p_gT, b_pgT = p_gl[0], b_pgl[0]
            ew1_v = ew1_d.rearrange("e (c p) f -> e p c f", p=128)
            ew2_v = ew2_d.rearrange("e (c p) f -> e p c f", p=128)
            NU = (S // TB) * NE

            def load_weights(u):
                ex = u % NE
                ws = u % 2
                for c in range(0, 8, 2):
                    kb.dma("pool", w1b[ws][:, c:c + 2, :], ew1_v[ex][:, c:c + 2, :], b_w1b[ws], writes=[b_w1b[ws]])
                for c in range(0, 8, 4):
                    kb.dma("pool", w2b[ws][:, c:c + 4, :], ew2_v[ex][:, c:c + 4, :], b_w2b[ws], writes=[b_w2b[ws]])
                kb.dma("sp", b1c[ws][:], eb1_d[ex], b_b1c[ws], writes=[b_b1c[ws]])
                kb.op("dve", lambda e: e.tensor_scalar(b1c[ws][:, 8:16], b1c[ws][:, 8:16], 1.0, None, ALU.add),
                      reads=[b_b1c[ws]], writes=[b_b1c[ws]])

            state = {"nfc": 0, "no2": 0, "nx": 0, "pend": None}

            def stage2(tb, ex, ws, grp, asl):
                for tq in range(4):
                    tl = grp * 4 + tq
                    tg = tb * NTB + tl
                    for dh in range(2):
                        o = state["no2"] % 2
                        state["no2"] += 1
                        for fc in range(8):
                            kb.op("pe", lambda e, fc=fc: e.matmul(
                                p_o2[o][:, :], actT[asl][:, fc, tq * 128:(tq + 1) * 128], w2b[ws][:, fc, dh * 512:(dh + 1) * 512],
                                start=(fc == 0), stop=(fc == 7)), reads=[b_actT[asl], b_w2b[ws]], writes=[b_po2[o]], sig=(fc == 7))
                        kb.op("dve", lambda e: e.scalar_tensor_tensor(
                            out=acc[:, tl, dh * 512:(dh + 1) * 512], in0=p_o2[o][:, :], scalar=gates[:, tg, ex:ex + 1],
                            in1=acc[:, tl, dh * 512:(dh + 1) * 512], op0=ALU.mult, op1=ALU.add),
                            reads=[b_po2[o], b_gates, b_acc[tl]], writes=[b_acc[tl]])

            def finish(pa, pfc, asl):
                kb.op("dve", lambda e: e.tensor_tensor(out=gcl[pa][:], in0=gcl[pa][:], in1=sgm[pa][:], op=ALU.mult),
                      reads=[b_gcl[pa], b_sgm[pa]], writes=[b_gcl[pa]])
                kb.op("dve", lambda e: e.scalar_tensor_tensor(
                    out=actT[asl][:, pfc, :], in0=lcl[pa][:], scalar=-6.0, in1=gcl[pa][:], op0=ALU.max, op1=ALU.mult),
                    reads=[b_gcl[pa], b_lcl[pa]], writes=[b_actT[asl]])

            def flush():
                if state["pend"] is not None:
                    stage2(*state["pend"])
                    state["pend"] = None

            load_weights(0)
            for tb in range(S // TB):
                t0_ = tb * TB
                kb.dma("sp", h2b[:], H2T[:, :, t0_:t0_ + TB].rearrange("c p t -> p c t"), b_h2b, writes=[b_h2b])
                for tl in range(NTB):
                    tg = tb * NTB + tl
                    kb.op("pe", lambda e, tg=tg: e.transpose(p_gT[0:NE, 0:128], gates[:, tg, :], ident_f[:]),
                          reads=[b_gates, b_identf], writes=[b_pgT])
                    kb.op("act", lambda e: e.activation(out=gT[:, :], in_=p_gT[0:NE, 0:128], func=AF.Copy), reads=[b_pgT], writes=[b_gT])
                    for dh in range(2):
                        o = state["no2"] % 2
                        state["no2"] += 1
                        kb.op("pe", lambda e, dh=dh, o=o: e.matmul(p_o2[o][:, :], gT[:, :], b2all[:, dh * 512:(dh + 1) * 512], start=True, stop=True),
                              reads=[b_gT, b_b2all], writes=[b_po2[o]])
                        kb.op("act", lambda e, dh=dh, o=o, tl=tl: e.activation(out=acc[:, tl, dh * 512:(dh + 1) * 512], in_=p_o2[o][:, :], func=AF.Copy),
                              reads=[b_po2[o]], writes=[b_acc[tl]])
                for ex in range(NE):
                    u = tb * NE + ex
                    ws = u % 2
                    for grp in range(TB // 512):
                        asl = (state["nfc"] // 8) % 2
                        prev = None
                        for fc in range(8):
                            a = state["nfc"] % NS
                            state["nfc"] += 1
                            pg_, pl_ = p_gl[2 * a], p_gl[2 * a + 1]
                            bpg, bpl = b_pgl[2 * a], b_pgl[2 * a + 1]
                            for c in range(8):
                                kb.op("pe", lambda e, c=c: e.matmul(
                                    pg_[:, :], w1b[ws][:, c, fc * 128:(fc + 1) * 128], h2b[:, c, grp * 512:(grp + 1) * 512],
                                    start=(c == 0), stop=(c == 7)), reads=[b_w1b[ws], b_h2b], writes=[bpg], sig=(c == 7))
                            for c in range(8):
                                kb.op("pe", lambda e, c=c: e.matmul(
                                    pl_[:, :], w1b[ws][:, c, D + fc * 128:D + (fc + 1) * 128], h2b[:, c, grp * 512:(grp + 1) * 512],
                                    start=(c == 0), stop=(c == 7)), reads=[b_w1b[ws], b_h2b], writes=[bpl], sig=(c == 7))
                            kb.op("dve", lambda e: e.tensor_scalar(
                                gcl[a][:], pg_[:, :], b1c[ws][:, fc:fc + 1], 7.0, ALU.add, ALU.min),
                                reads=[bpg, b_b1c[ws]], writes=[b_gcl[a]])
                            kb.op("act", lambda e: e.activation(out=sgm[a][:], in_=gcl[a][:], func=AF.Sigmoid, scale=1.702),
                                  reads=[b_gcl[a]], writes=[b_sgm[a]])
                            kb.op("dve", lambda e: e.tensor_scalar(
                                lcl[a][:], pl_[:, :], b1c[ws][:, 8 + fc:9 + fc], 8.0, ALU.add, ALU.min),
                                reads=[bpl, b_b1c[ws]], writes=[b_lcl[a]])
                            if prev is not None:
                                pa, pfc = prev
                                finish(pa, pfc, asl)
                            prev = (a, fc)
                            if fc == 3:
                                flush()
                                if grp == 0 and u + 1 < NU:
                                    load_weights(u + 1)
                        pa, pfc = prev
                        finish(pa, pfc, asl)
                        state["pend"] = (tb, ex, ws, grp, asl)
                flush()
                for tl in range(NTB):
                    tg = tb * NTB + tl
                    z = state["nx"] % 2
                    state["nx"] += 1
                    kb.dma("sp", xo[z][:], out_d[tg * 128:(tg + 1) * 128, :], b_xo[z], writes=[b_xo[z]])
                    kb.op("dve", lambda e, tl=tl: e.tensor_tensor(out=acc[:, tl, :], in0=acc[:, tl, :], in1=G2row[:], op=ALU.mult),
                          reads=[b_acc[tl], b_G2], writes=[b_acc[tl]])
                    kb.op("dve", lambda e, tl=tl, z=z: e.tensor_tensor(out=xo[z][:], in0=acc[:, tl, :], in1=xo[z][:], op=ALU.add),
                          reads=[b_acc[tl], b_xo[z]], writes=[b_xo[z]])
                    kb.dma("sp", out_d[tg * 128:(tg + 1) * 128, :], xo[z][:], b_xo[z], reads=[b_xo[z]])
            kb.barrier()
        kb.barrier()
    return nc


_CONST = {}


def _constants():
    if _CONST:
        return _CONST
    f32 = np.float32
    pos = np.arange(S)
    row = (pos // 64).astype(f32)
    col = (pos % 64).astype(f32)
    inv = (f32(10000.0) ** (-np.arange(16, dtype=f32) / f32(16))).astype(f32)
    ar = (row[:, None] * inv[None]).astype(f32)
    ac = (col[:, None] * inv[None]).astype(f32)
    cr, sr, cc, sc = np.cos(ar), np.sin(ar), np.cos(ac), np.sin(ac)
    C = np.concatenate([cr, cr, cc, cc], axis=1)
    SA = np.concatenate([-sr, -sc], axis=1)
    SB = np.concatenate([sr, sc], axis=1)
    C = np.concatenate([np.ones((LC, 64), f32), C], 0)
    SA = np.concatenate([np.zeros((LC, 32), f32), SA], 0)
    SB = np.concatenate([np.zeros((LC, 32), f32), SB], 0)

    def pm(a):
        n = a.shape[0] // 128
        return np.ascontiguousarray(a.reshape(n, 128, -1).transpose(1, 0, 2).reshape(128, -1)).astype(f32)

    _CONST["rope_c"], _CONST["rope_sa"], _CONST["rope_sb"] = pm(C), pm(SA), pm(SB)
    posf = np.arange(S, dtype=f32)
    tn = posf / f32(S)
    bands = np.linspace(1e-4, 7, 8, dtype=f32)
    ang = (f32(2.0 * math.pi / S) * posf[:, None] * bands[None]).astype(f32)
    feats = np.concatenate([tn[:, None], np.sin(ang), np.cos(ang)], axis=-1).astype(f32)
    _CONST["featsT"] = np.ascontiguousarray(feats.T)
    deltas = np.abs(np.linspace(math.log(1e-2) / 1.5, math.log(1e-2) / 0.3, 512, dtype=f32))
    _CONST["window"] = np.exp(-tn[:, None] * deltas[None]).astype(f32)
    _CONST["ident"] = np.eye(128, dtype=f32)
    k = np.arange(S, dtype=np.float64)
    t = np.arange(S, dtype=np.float64)
    th = np.pi * (2 * k[:, None] + 1) * t[None, :] / NFFT
    bf = ml_dtypes.bfloat16
    for nm, fn in (("c", np.cos), ("s", np.sin)):
        M = fn(th).astype(f32)
        a = M.reshape(NT, 128, NT, 128).transpose(0, 3, 2, 1)
        _CONST[f"dft_{nm}tk"] = np.ascontiguousarray(a).reshape(NT, 128, S).astype(bf)
        a = M.reshape(NT, 128, NT, 128).transpose(2, 1, 0, 3)
        _CONST[f"dft_{nm}kt"] = np.ascontiguousarray(a).reshape(NT, 128, S).astype(bf)
    return _CONST


def _col(v, n):
    return np.ascontiguousarray(np.asarray(v, np.float32).reshape(n, 128).T)


_PROG = {}


def make_in_maps(inp):
    cst = _constants()
    f32 = np.float32
    g = lambda k: np.asarray(inp[k], f32)
    shared = dict(cst)
    shared["w_mod"] = g("w_mod")[0]
    bm = g("b_mod")[0]
    shared["bmod_col"] = np.ascontiguousarray(bm.reshape(6, 8, 128).transpose(2, 0, 1).reshape(128, 48))
    shared["bmod_row"] = bm.reshape(1, -1)
    shared["n1g_col"] = _col(g("norm1_g")[0], 8)
    shared["n2g_col"] = _col(g("norm2_g")[0], 8)
    shared["w_in"] = g("w_in")[0]
    shared["qkg_row"] = np.concatenate([np.tile(g("q_norm_g")[0], 8), np.tile(g("k_norm_g")[0], 8)]).reshape(1, 1024)
    shared["lam_row"] = np.concatenate([g("lam_q1")[0], g("lam_k1")[0], g("lam_q2")[0], g("lam_k2")[0]]).reshape(1, 256)
    shared["subg_row"] = g("subln_g")[0].reshape(1, 128)
    shared["convw"] = g("hy_conv_w")[0].reshape(1, 4608)
    shared["convb"] = g("hy_conv_b")[0].reshape(1, 1536)
    shared["hy_w1"] = g("hy_w1")[0]
    shared["hy_cols"] = np.ascontiguousarray(np.stack([g("hy_b1")[0], g("hy_f1")[0], g("hy_b2")[0], g("hy_f2")[0]], axis=1))
    shared["hy_w2"] = g("hy_w2")[0]
    shared["hy_w3"] = g("hy_w3")[0]
    shared["hy_skip"] = g("hy_skip")[0].reshape(1, 1024)
    shared["hyg_row"] = g("hy_out_g")[0].reshape(1, 512)
    shared["w_out"] = g("w_out")[0]
    shared["router_w"] = g("router_w")[0]
    shared["router_b"] = g("router_b")[0].reshape(1, NE)
    shared["exp_w1"] = g("exp_w1")[0]
    shared["eb1_col"] = np.ascontiguousarray(g("exp_b1")[0].reshape(NE, 16, 128).transpose(0, 2, 1))
    shared["exp_w2"] = g("exp_w2")[0]
    shared["exp_b2"] = g("exp_b2")[0]
    x = g("x")
    ctx = g("ctx")
    c = g("c")
    cc = g("c_ctx")
    maps = []
    for b in range(8):
        m = dict(shared)
        m["x"] = x[b]
        m["ctx"] = ctx[b]
        cs = np.stack([_col(c[b], 8), _col(cc, 8)], axis=2).reshape(128, 16)
        m["csil"] = np.ascontiguousarray(cs)
        maps.append(m)
    return maps


def kernel(**inputs):
    if "nc" not in _PROG:
        _PROG["nc"] = build_program()
    nc = _PROG["nc"]
    maps = make_in_maps(inputs)
    res = run_bass_kernel_spmd(nc, maps, core_ids=list(range(8)))
    return np.stack([np.asarray(r["out"], np.float32) for r in res.results], axis=0)
```
